# Optimizing a Trainium2 kernel written in Bass

```python
import math
import jax
import jax.numpy as jnp
from jax import lax
import numpy as np

D_MODEL = 1024
BATCH = 4
SEQ = 8192
DEPTH = 2

CHUNK = 64
Q_BLOCK = 128
PLE_DIM = 256
RMS_EPS = 1e-6

SSM_GROUP_CH = 16
SSM_GROUPS = 16
SSM_WIDTH = SSM_GROUPS * SSM_GROUP_CH
SSM_STATE = 64

FOX_HEADS = 4
FOX_HEAD_DIM = 64

MLA_HEADS = 4
MLA_NOPE_DIM = 64
MLA_ROPE_DIM = 32
MLA_V_DIM = 64
MLA_Q_LORA = 192
MLA_KV_LORA = 128
ROPE_BASE = 10000.0

CHK_HEADS = 4
CHK_HEAD_DIM = 64
LEFT_CHUNKS = 8
REL_CLIP = 256

N_BRANCHES = 4
BRANCH_WIDTH = 256

IN_SIZES = (SSM_WIDTH,
            FOX_HEADS * FOX_HEAD_DIM, FOX_HEADS * FOX_HEAD_DIM, FOX_HEADS * FOX_HEAD_DIM, FOX_HEADS,
            MLA_Q_LORA, MLA_KV_LORA, MLA_ROPE_DIM,
            CHK_HEADS * CHK_HEAD_DIM, CHK_HEADS * CHK_HEAD_DIM, CHK_HEADS * CHK_HEAD_DIM)
D_IN = sum(IN_SIZES)

D_FF = 2816
N_EXPERTS = 8
TOP_K = 2
D_FF_EXPERT = 1408
MOE_BLOCK = 256
N_DENSE = (DEPTH + 1) // 2
N_MOE = DEPTH // 2

kernel_name = "hybrid_streaming_encoder_block"


def rms_norm(x, g):
    xf = x.astype(jnp.float32)
    y = xf * lax.rsqrt(jnp.mean(xf * xf, axis=-1, keepdims=True) + RMS_EPS)
    return (y * g.astype(jnp.float32)).astype(x.dtype)


def rope_tables(positions):
    inv_freq = ROPE_BASE ** (-jnp.arange(0, MLA_ROPE_DIM, 2, dtype=jnp.float32) / MLA_ROPE_DIM)
    ang = positions.astype(jnp.float32)[..., None] * inv_freq
    return jnp.cos(ang)[:, :, None, :], jnp.sin(ang)[:, :, None, :]


def apply_rope(t, cos, sin):
    tf = t.astype(jnp.float32)
    t1, t2 = jnp.split(tf, 2, axis=-1)
    return jnp.concatenate([t1 * cos - t2 * sin, t2 * cos + t1 * sin], axis=-1).astype(t.dtype)


def s5_mixer(u, a_re, a_im, log_dt, b_re, b_im, c_re, c_im, d_skip, w_glu):
    bsz, seq, _ = u.shape
    f32 = jnp.float32
    uf = u.astype(f32).reshape(bsz, seq, SSM_GROUPS, SSM_GROUP_CH)
    ar, ai = a_re.astype(f32), a_im.astype(f32)
    dt = jnp.exp(log_dt.astype(f32))[:, None]
    mag = jnp.exp(ar * dt)
    abar_r, abar_i = mag * jnp.cos(ai * dt), mag * jnp.sin(ai * dt)
    den = ar * ar + ai * ai
    nr, ni = abar_r - 1.0, abar_i
    coef_r = (nr * ar + ni * ai) / den
    coef_i = (ni * ar - nr * ai) / den
    br, bi = b_re.astype(f32), b_im.astype(f32)
    bbar_r = coef_r[..., None] * br - coef_i[..., None] * bi
    bbar_i = coef_r[..., None] * bi + coef_i[..., None] * br
    bu_r = jnp.einsum("blgh,gph->blgp", uf, bbar_r)
    bu_i = jnp.einsum("blgh,gph->blgp", uf, bbar_i)
    a_r = jnp.broadcast_to(abar_r, bu_r.shape)
    a_i = jnp.broadcast_to(abar_i, bu_i.shape)

    def combine(left, right):
        lar, lai, lbr, lbi = left
        rar, rai, rbr, rbi = right
        return (rar * lar - rai * lai, rar * lai + rai * lar,
                rar * lbr - rai * lbi + rbr, rar * lbi + rai * lbr + rbi)

    _, _, xr, xi = lax.associative_scan(combine, (a_r, a_i, bu_r, bu_i), axis=1)
    y = (jnp.einsum("blgp,ghp->blgh", xr, c_re.astype(f32))
         - jnp.einsum("blgp,ghp->blgh", xi, c_im.astype(f32))
         + d_skip.astype(f32) * uf)
    y = jax.nn.gelu(y.reshape(bsz, seq, SSM_WIDTH)).astype(u.dtype)
    return y * jax.nn.sigmoid(y @ w_glu)


def blocked_attention(q, k, v, frame_causal, decay_cum=None):
    bsz, seq, nh, dq = q.shape
    dv = v.shape[-1]
    nb = seq // Q_BLOCK
    q_blocks = (q * (dq ** -0.5)).reshape(bsz, nb, Q_BLOCK, nh, dq).transpose(1, 0, 2, 3, 4)
    kpos = jnp.arange(seq)
    xs = (q_blocks, jnp.arange(nb))
    if decay_cum is not None:
        xs = xs + (decay_cum.reshape(bsz, nb, Q_BLOCK, nh).transpose(1, 0, 3, 2),)
        decay_k = decay_cum.transpose(0, 2, 1)

    def one_block(args):
        q_blk, blk = args[0], args[1]
        qpos = blk * Q_BLOCK + jnp.arange(Q_BLOCK)
        if frame_causal:
            mask = kpos[None, :] <= qpos[:, None]
        else:
            mask = (kpos[None, :] // CHUNK) <= (qpos[:, None] // CHUNK)
        s = jnp.einsum("bqhd,bkhd->bhqk", q_blk, k, preferred_element_type=jnp.float32)
        if decay_cum is not None:
            s = s + (args[2][..., None] - decay_k[:, :, None, :])
        s = jnp.where(mask, s, -jnp.inf)
        prob = jax.nn.softmax(s, axis=-1).astype(v.dtype)
        return jnp.einsum("bhqk,bkhd->bqhd", prob, v)

    out = lax.map(one_block, xs)
    return out.transpose(1, 0, 2, 3, 4).reshape(bsz, seq, nh * dv)


def fox_mixer(q, k, v, f_logit, b_f):
    bsz, seq, _ = q.shape
    split = lambda t: t.reshape(bsz, seq, FOX_HEADS, FOX_HEAD_DIM)
    log_f = jax.nn.log_sigmoid(f_logit.astype(jnp.float32) + b_f.astype(jnp.float32))
    decay_cum = jnp.cumsum(log_f, axis=1)
    return blocked_attention(split(q), split(k), split(v), True, decay_cum)


def mla_mixer(c_q, c_kv, k_pe, cos, sin, g_q, w_uq, g_kv, w_ukv):
    bsz, seq, _ = c_q.shape
    q = (rms_norm(c_q, g_q) @ w_uq).reshape(bsz, seq, MLA_HEADS, MLA_NOPE_DIM + MLA_ROPE_DIM)
    q_nope, q_pe = q[..., :MLA_NOPE_DIM], q[..., MLA_NOPE_DIM:]
    kv = (rms_norm(c_kv, g_kv) @ w_ukv).reshape(bsz, seq, MLA_HEADS, MLA_NOPE_DIM + MLA_V_DIM)
    k_nope, v = kv[..., :MLA_NOPE_DIM], kv[..., MLA_NOPE_DIM:]
    q_pe = apply_rope(q_pe, cos, sin)
    k_pe = jnp.broadcast_to(apply_rope(k_pe[:, :, None, :], cos, sin), (bsz, seq, MLA_HEADS, MLA_ROPE_DIM))
    q = jnp.concatenate([q_nope, q_pe], axis=-1)
    k = jnp.concatenate([k_nope, k_pe], axis=-1)
    return blocked_attention(q, k, v, False)


def chunk_band_attention(q, k, v, rel_bias):
    bsz, seq, nh, dh = q.shape
    nc = seq // CHUNK
    band = (LEFT_CHUNKS + 1) * CHUNK
    qc = (q * (dh ** -0.5)).reshape(bsz, nc, CHUNK, nh, dh)

    def gather_band(t):
        tc = t.reshape(bsz, nc, CHUNK, nh, dh)
        tp = jnp.pad(tc, ((0, 0), (LEFT_CHUNKS, 0), (0, 0), (0, 0), (0, 0)))
        return jnp.concatenate([tp[:, j:j + nc] for j in range(LEFT_CHUNKS + 1)], axis=2)

    kb, vb = gather_band(k), gather_band(v)
    s = jnp.einsum("bnqhd,bnkhd->bnhqk", qc, kb, preferred_element_type=jnp.float32)
    qi = jnp.arange(CHUNK)
    kj = jnp.arange(band)
    rel = qi[:, None] + LEFT_CHUNKS * CHUNK - kj[None, :]
    idx = jnp.clip(rel, -REL_CLIP, REL_CLIP) + REL_CLIP
    bias = rel_bias.astype(jnp.float32)[:, idx]
    valid = (jnp.arange(nc)[:, None] - LEFT_CHUNKS + kj[None, :] // CHUNK) >= 0
    s = jnp.where(valid[None, :, None, None, :], s + bias[None, None], -jnp.inf)
    prob = jax.nn.softmax(s, axis=-1).astype(v.dtype)
    out = jnp.einsum("bnhqk,bnkhd->bnqhd", prob, vb)
    return out.reshape(bsz, seq, nh * dh)


def hybrid_mixer(h, cos, sin, w_in, ssm_a_re, ssm_a_im, ssm_log_dt, ssm_b_re, ssm_b_im, ssm_c_re,
                 ssm_c_im, ssm_d, ssm_w_glu, fox_b_f, mla_g_q, mla_w_uq, mla_g_kv, mla_w_ukv,
                 chk_rel_bias, w_branch, w_gate, b_gate, w_o):
    bsz, seq, _ = h.shape
    splits = np.cumsum(IN_SIZES)[:-1].tolist()
    (u, fq, fk, fv, ff, cq, ckv, kpe, dq, dk, dv) = jnp.split(h @ w_in, splits, axis=-1)
    y_a = s5_mixer(u, ssm_a_re, ssm_a_im, ssm_log_dt, ssm_b_re, ssm_b_im, ssm_c_re, ssm_c_im, ssm_d, ssm_w_glu)
    y_b = fox_mixer(fq, fk, fv, ff, fox_b_f)
    y_c = mla_mixer(cq, ckv, kpe, cos, sin, mla_g_q, mla_w_uq, mla_g_kv, mla_w_ukv)
    heads = lambda t: t.reshape(bsz, seq, CHK_HEADS, CHK_HEAD_DIM)
    y_d = chunk_band_attention(heads(dq), heads(dk), heads(dv), chk_rel_bias)
    merged = None
    for bi, y in enumerate((y_a, y_b, y_c, y_d)):
        term = jax.nn.sigmoid(h @ w_gate[bi] + b_gate[bi]) * (y @ w_branch[bi])
        merged = term if merged is None else merged + term
    return merged @ w_o


def swiglu(h, w1, w3, w2):
    return (jax.nn.silu(h @ w1) * (h @ w3)) @ w2


def moe_swiglu(h, w_router, b_router, w1, w3, w2):
    bsz, seq, d = h.shape
    n = bsz * seq
    hf = h.reshape(n, d)
    logits = jnp.dot(hf.astype(jnp.float32), w_router.astype(jnp.float32)) + b_router.astype(jnp.float32)
    top_logit, top_idx = lax.top_k(logits, TOP_K)
    gate = jax.nn.softmax(top_logit, axis=-1)
    nk = n * TOP_K
    e_flat = top_idx.reshape(nk)
    tok_flat = jnp.repeat(jnp.arange(n, dtype=jnp.int32), TOP_K)
    gate_flat = gate.reshape(nk)
    order = jnp.argsort(e_flat)
    e_s, tok_s, gate_s = e_flat[order], tok_flat[order], gate_flat[order]
    counts = jnp.bincount(e_flat, length=N_EXPERTS)
    padded = (counts + MOE_BLOCK - 1) // MOE_BLOCK * MOE_BLOCK
    pad_end = jnp.cumsum(padded)
    pad_start = pad_end - padded
    start = jnp.cumsum(counts) - counts
    dest = pad_start[e_s] + jnp.arange(nk) - start[e_s]
    n_blocks = -(-nk // MOE_BLOCK) + N_EXPERTS
    m = n_blocks * MOE_BLOCK
    slot_tok = jnp.full((m,), n, jnp.int32).at[dest].set(tok_s)
    h_pad = jnp.concatenate([hf, jnp.zeros((1, d), hf.dtype)], axis=0)
    xb = h_pad[slot_tok].reshape(n_blocks, MOE_BLOCK, d)
    block_expert = jnp.minimum(
        jnp.searchsorted(pad_end, jnp.arange(n_blocks) * MOE_BLOCK, side="right"), N_EXPERTS - 1)

    def expert_block(args):
        x_blk, e = args
        return (jax.nn.silu(x_blk @ w1[e]) * (x_blk @ w3[e])) @ w2[e]

    yb = lax.map(expert_block, (xb, block_expert)).reshape(m, d)
    y_s = yb[dest] * gate_s[:, None].astype(h.dtype)
    out = jnp.zeros((n, d), h.dtype).at[tok_s].add(y_s)
    return out.reshape(bsz, seq, d)


def setup_inputs(seed: int = 0) -> dict:
    key = jax.random.key(seed)
    ks = iter(jax.random.split(key, 48))
    f32 = jnp.float32

    def nrm(shape, scale):
        return jax.random.normal(next(ks), shape, f32) * scale

    def gain(shape):
        return 1.0 + nrm(shape, 0.02)

    L, G, P, Hg = DEPTH, SSM_GROUPS, SSM_STATE, SSM_GROUP_CH
    x = nrm((BATCH, SEQ, D_MODEL), 1.0)
    p = nrm((DEPTH, BATCH, SEQ, PLE_DIM), 1.0)
    offset = jax.random.randint(next(ks), (BATCH, 1), 0, 256, dtype=jnp.int32) * CHUNK
    positions = (offset + jnp.arange(SEQ, dtype=jnp.int32)[None, :]).astype(jnp.int32)
    w_in = nrm((L, D_MODEL, D_IN), D_MODEL ** -0.5)
    ssm_a_re = -0.5 + nrm((L, G, P), 0.01)
    ssm_a_im = math.pi * jnp.arange(P, dtype=f32)[None, None, :] + nrm((L, G, P), 0.01)
    ssm_log_dt = jax.random.uniform(next(ks), (L, G), f32, math.log(1e-3), math.log(1e-1))
    ssm_b_re = nrm((L, G, P, Hg), (2 * Hg) ** -0.5)
    ssm_b_im = nrm((L, G, P, Hg), (2 * Hg) ** -0.5)
    ssm_c_re = nrm((L, G, Hg, P), (2 * P) ** -0.5)
    ssm_c_im = nrm((L, G, Hg, P), (2 * P) ** -0.5)
    ssm_d = nrm((L, G, Hg), 0.5)
    ssm_w_glu = nrm((L, SSM_WIDTH, SSM_WIDTH), SSM_WIDTH ** -0.5)
    fox_b_f = nrm((L, FOX_HEADS), 0.1)
    mla_g_q = gain((L, MLA_Q_LORA))
    mla_w_uq = nrm((L, MLA_Q_LORA, MLA_HEADS * (MLA_NOPE_DIM + MLA_ROPE_DIM)), MLA_Q_LORA ** -0.5)
    mla_g_kv = gain((L, MLA_KV_LORA))
    mla_w_ukv = nrm((L, MLA_KV_LORA, MLA_HEADS * (MLA_NOPE_DIM + MLA_V_DIM)), MLA_KV_LORA ** -0.5)
    chk_rel_bias = nrm((L, CHK_HEADS, 2 * REL_CLIP + 1), 0.2)
    w_branch = nrm((L, N_BRANCHES, BRANCH_WIDTH, D_MODEL), BRANCH_WIDTH ** -0.5)
    w_gate = nrm((L, N_BRANCHES, D_MODEL, D_MODEL), D_MODEL ** -0.5)
    b_gate = nrm((L, N_BRANCHES, D_MODEL), 0.01)
    w_o = nrm((L, D_MODEL, D_MODEL), D_MODEL ** -0.5)
    g_mix = gain((L, D_MODEL))
    g_ffn = gain((L, D_MODEL))
    ffn_w1 = nrm((N_DENSE, D_MODEL, D_FF), D_MODEL ** -0.5)
    ffn_w3 = nrm((N_DENSE, D_MODEL, D_FF), D_MODEL ** -0.5)
    ffn_w2 = nrm((N_DENSE, D_FF, D_MODEL), D_FF ** -0.5)
    moe_w_router = nrm((N_MOE, D_MODEL, N_EXPERTS), D_MODEL ** -0.5)
    moe_b_router = nrm((N_MOE, N_EXPERTS), 0.01)
    moe_w1 = nrm((N_MOE, N_EXPERTS, D_MODEL, D_FF_EXPERT), D_MODEL ** -0.5)
    moe_w3 = nrm((N_MOE, N_EXPERTS, D_MODEL, D_FF_EXPERT), D_MODEL ** -0.5)
    moe_w2 = nrm((N_MOE, N_EXPERTS, D_FF_EXPERT, D_MODEL), D_FF_EXPERT ** -0.5)
    g_ple = gain((L, D_MODEL))
    ple_w_gate = nrm((L, D_MODEL, D_MODEL), D_MODEL ** -0.5)
    ple_w_up = nrm((L, PLE_DIM, D_MODEL), PLE_DIM ** -0.5)
    g_final = gain((D_MODEL,))
    return {"x": x, "p": p, "positions": positions, "w_in": w_in,
            "ssm_a_re": ssm_a_re, "ssm_a_im": ssm_a_im, "ssm_log_dt": ssm_log_dt,
            "ssm_b_re": ssm_b_re, "ssm_b_im": ssm_b_im, "ssm_c_re": ssm_c_re, "ssm_c_im": ssm_c_im,
            "ssm_d": ssm_d, "ssm_w_glu": ssm_w_glu, "fox_b_f": fox_b_f,
            "mla_g_q": mla_g_q, "mla_w_uq": mla_w_uq, "mla_g_kv": mla_g_kv, "mla_w_ukv": mla_w_ukv,
            "chk_rel_bias": chk_rel_bias, "w_branch": w_branch, "w_gate": w_gate, "b_gate": b_gate,
            "w_o": w_o, "g_mix": g_mix, "g_ffn": g_ffn,
            "ffn_w1": ffn_w1, "ffn_w3": ffn_w3, "ffn_w2": ffn_w2,
            "moe_w_router": moe_w_router, "moe_b_router": moe_b_router,
            "moe_w1": moe_w1, "moe_w3": moe_w3, "moe_w2": moe_w2,
            "g_ple": g_ple, "ple_w_gate": ple_w_gate, "ple_w_up": ple_w_up, "g_final": g_final}


def reference(x, p, positions, w_in, ssm_a_re, ssm_a_im, ssm_log_dt, ssm_b_re, ssm_b_im, ssm_c_re,
              ssm_c_im, ssm_d, ssm_w_glu, fox_b_f, mla_g_q, mla_w_uq, mla_g_kv, mla_w_ukv,
              chk_rel_bias, w_branch, w_gate, b_gate, w_o, g_mix, g_ffn, ffn_w1, ffn_w3, ffn_w2,
              moe_w_router, moe_b_router, moe_w1, moe_w3, moe_w2, g_ple, ple_w_gate, ple_w_up, g_final):
    cos, sin = rope_tables(positions)
    for i in range(DEPTH):
        h = rms_norm(x, g_mix[i])
        x = x + hybrid_mixer(h, cos, sin, w_in[i], ssm_a_re[i], ssm_a_im[i], ssm_log_dt[i], ssm_b_re[i],
                             ssm_b_im[i], ssm_c_re[i], ssm_c_im[i], ssm_d[i], ssm_w_glu[i], fox_b_f[i],
                             mla_g_q[i], mla_w_uq[i], mla_g_kv[i], mla_w_ukv[i], chk_rel_bias[i],
                             w_branch[i], w_gate[i], b_gate[i], w_o[i])
        h = rms_norm(x, g_ffn[i])
        if i % 2 == 0:
            x = x + swiglu(h, ffn_w1[i // 2], ffn_w3[i // 2], ffn_w2[i // 2])
        else:
            j = i // 2
            x = x + moe_swiglu(h, moe_w_router[j], moe_b_router[j], moe_w1[j], moe_w3[j], moe_w2[j])
        ple_gate = jax.nn.sigmoid(rms_norm(x, g_ple[i]) @ ple_w_gate[i])
        x = x + (p[i] @ ple_w_up[i]) * ple_gate
    return rms_norm(x, g_final)
```

```python
import contextlib
import math
import numpy as np
import ml_dtypes
import concourse.bass as bass
import concourse.mybir as mybir
from concourse.bass_utils import run_bass_kernel_spmd

F32 = mybir.dt.float32
BF16 = mybir.dt.bfloat16
I32 = mybir.dt.int32
AF = mybir.ActivationFunctionType
ALU = mybir.AluOpType
AX = mybir.AxisListType

ENGS = ("pe", "act", "dve", "pool", "sp")
CH = 30000
NEG = -30000.0


class Op:
    __slots__ = ("eng", "fn", "waits", "dma", "dsem", "dval", "signal", "sigval", "sigsem", "bg")

    def __init__(self, eng, fn, dma):
        self.eng = eng
        self.fn = fn
        self.dma = dma
        self.waits = []
        self.signal = False
        self.sigval = 0
        self.sigsem = 0
        self.dsem = None
        self.dval = 0
        self.bg = False


class Prog:
    def __init__(self, nc):
        self.nc = nc
        self.ops = {e: [] for e in ENGS}
        self.lastw = {}
        self.readers = {}
        self.dma_sems = {}
        self.last_dma = {}
        self.bg_mode = False
        self.selfwait = {"pe": False, "act": True, "dve": True, "pool": True, "sp": False}

    def op(self, eng, fn, reads=(), writes=(), dma_key=None):
        o = Op(eng, fn, dma_key is not None)
        o.bg = self.bg_mode
        deps = []
        for t in reads:
            w = self.lastw.get(t)
            if w is not None:
                deps.append((w, 0))
        for t in writes:
            w = self.lastw.get(t)
            if w is not None:
                deps.append((w, 1))
            for r in self.readers.get(t, ()):
                deps.append((r, 2))
        seen = set()
        for d, kind in deps:
            if d is o or id(d) in seen:
                continue
            if (not d.dma) and d.eng == eng:
                if kind == 2 or not self.selfwait[eng]:
                    continue
            seen.add(id(d))
            o.waits.append(d)
        for t in reads:
            self.readers.setdefault(t, []).append(o)
        for t in writes:
            self.lastw[t] = o
            self.readers[t] = []
        if o.dma:
            ent = self.dma_sems.setdefault(dma_key, [0])
            ent[0] += 16
            o.dsem = dma_key
            o.dval = ent[0]
        self.ops[eng].append(o)
        if o.dma:
            self.last_dma[dma_key] = o
        return o

    def barrier(self):
        lasts = []
        for e in ENGS:
            for o in reversed(self.ops[e]):
                if not o.dma and not o.bg:
                    lasts.append(o)
                    break
        dl = [o for o in self.last_dma.values() if not o.bg]
        for e in ENGS:
            o = Op(e, lambda eng: eng.nop(), False)
            o.waits = [d for d in lasts if d.eng != e] + dl
            self.ops[e].append(o)
        self.lastw = {t: o for t, o in self.lastw.items() if o.bg}
        self.readers = {t: [r for r in rs if r.bg] for t, rs in self.readers.items()}

    def emit(self):
        nc = self.nc
        for e in ENGS:
            for o in self.ops[e]:
                for d in o.waits:
                    if not d.dma:
                        d.signal = True
        nsig = {}
        for e in ENGS:
            c = 0
            for o in self.ops[e]:
                if o.signal and not o.dma:
                    o.sigsem = c // CH
                    o.sigval = c % CH + 1
                    c += 1
            nsig[e] = c
        with contextlib.ExitStack() as st:
            esem = {e: [st.enter_context(nc.semaphore("s_%s%d" % (e, j)))
                        for j in range(nsig[e] // CH + 1)] for e in ENGS}
            dsem = {}
            for i, k in enumerate(self.dma_sems):
                dsem[k] = st.enter_context(nc.semaphore("d%d" % i))
            block = st.enter_context(nc.Block())
            hooks = {"pe": block.tensor, "act": block.scalar, "dve": block.vector,
                     "pool": block.gpsimd, "sp": block.sync}

            def make(e):
                def body(eng):
                    waited = {}
                    for o in self.ops[e]:
                        need = {}
                        for d in o.waits:
                            if d.dma:
                                s, v = dsem[d.dsem], d.dval
                                if isinstance(d.dsem, str) and d.dsem.startswith("G:"):
                                    v = self.dma_sems[d.dsem][0]
                            else:
                                s, v = esem[d.eng][d.sigsem], d.sigval
                            key = id(s)
                            if waited.get(key, 0) >= v:
                                continue
                            if key not in need or need[key][1] < v:
                                need[key] = (s, v)
                        for key, (s, v) in need.items():
                            eng.wait_ge(s, v)
                            waited[key] = v
                        inst = o.fn(eng)
                        if o.dma:
                            inst.then_inc(dsem[o.dsem], 16)
                        elif o.signal:
                            inst.then_inc(esem[e][o.sigsem], 1)
                    if e == "sp":
                        for k, ent in self.dma_sems.items():
                            eng.wait_ge(dsem[k], ent[0])
                return body

            for e in ENGS:
                if self.ops[e] or e == "sp":
                    hooks[e](make(e))


class V:
    __slots__ = ("ap", "tok")

    def __init__(self, ap, tok):
        self.ap = ap
        self.tok = tok

    def __getitem__(self, idx):
        return V(self.ap[idx], self.tok)

    def re(self, s, **kw):
        return V(self.ap.rearrange(s, **kw), self.tok)

    def sub(self, tok):
        return V(self.ap, tok)


def _ap(x):
    return x.ap if isinstance(x, V) else x


def _toks(*xs):
    return [x.tok for x in xs if isinstance(x, V)]


class KB:
    def __init__(self):
        self.nc = bass.Bass("TRN2", target_bir_lowering=False)
        self.P = Prog(self.nc)
        self.st = contextlib.ExitStack()
        self.nps = 0
        self.dq = 0

    @contextlib.contextmanager
    def phase(self):
        old = self.st
        self.st = contextlib.ExitStack()
        try:
            yield
        finally:
            self.P.barrier()
            self.st.close()
            self.st = old

    def dram(self, name, shape, dt, kind):
        return V(self.nc.dram_tensor(name, list(shape), dt, kind=kind).ap(), "D:" + name)

    def sb(self, name, shape, dt):
        t = self.st.enter_context(self.nc.sbuf_tensor(name, list(shape), dt))
        return V(t[:], "S:" + name)

    def ps(self, name, shape, dt):
        t = self.st.enter_context(self.nc.psum_tensor(name, list(shape), dt))
        return V(t[:], "P:" + name)

    def dma(self, out, in_, q="sp", key=None, **kw):
        o, i = _ap(out), _ap(in_)
        self.P.op(q, lambda e: e.dma_start(out=o, in_=i, **kw), reads=_toks(in_), writes=_toks(out),
                  dma_key=key or out.tok)

    def mm(self, out, lhsT, rhs, start=True, stop=True, extra_reads=()):
        o, l, r = _ap(out), _ap(lhsT), _ap(rhs)
        self.P.op("pe", lambda e: e.matmul(o, l, r, start=start, stop=stop),
                  reads=_toks(lhsT, rhs) + list(extra_reads), writes=_toks(out))

    def tr(self, out, in_, ident):
        o, i, d = _ap(out), _ap(in_), _ap(ident)
        self.P.op("pe", lambda e: e.transpose(o, i, d), reads=_toks(in_, ident), writes=_toks(out))

    def act(self, out, in_, func, bias=None, scale=1.0, accum_out=None, eng="act"):
        o, i = _ap(out), _ap(in_)
        b = _ap(bias)
        s = _ap(scale)
        a = _ap(accum_out)
        kw = {}
        if b is not None:
            kw["bias"] = b
        if a is not None:
            kw["accum_out"] = a
        self.P.op("act", lambda e: e.activation(out=o, in_=i, func=func, scale=s, **kw),
                  reads=_toks(in_, bias, scale), writes=_toks(out, accum_out))

    def tt(self, out, in0, in1, op, eng="dve"):
        o, a, b = _ap(out), _ap(in0), _ap(in1)
        self.P.op(eng, lambda e: e.tensor_tensor(out=o, in0=a, in1=b, op=op),
                  reads=_toks(in0, in1), writes=_toks(out))

    def ts(self, out, in0, s1, op0, s2=None, op1=None, eng="dve", accum_out=None):
        o, a = _ap(out), _ap(in0)
        x1, x2 = _ap(s1), _ap(s2)
        acc = _ap(accum_out)
        kw = {}
        if op1 is not None:
            kw["op1"] = op1
        if acc is not None:
            kw["accum_out"] = acc
        self.P.op(eng, lambda e: e.tensor_scalar(out=o, in0=a, scalar1=x1, scalar2=x2, op0=op0, **kw),
                  reads=_toks(in0, s1, s2), writes=_toks(out, accum_out))

    def stt(self, out, in0, scalar, in1, op0, op1, eng="dve"):
        o, a, s, b = _ap(out), _ap(in0), _ap(scalar), _ap(in1)
        self.P.op(eng, lambda e: e.scalar_tensor_tensor(out=o, in0=a, scalar=s, in1=b, op0=op0, op1=op1),
                  reads=_toks(in0, scalar, in1), writes=_toks(out))

    def copy(self, out, in_, eng="dve"):
        o, i = _ap(out), _ap(in_)
        if eng == "act":
            self.P.op("act", lambda e: e.copy(out=o, in_=i), reads=_toks(in_), writes=_toks(out))
        else:
            self.P.op(eng, lambda e: e.tensor_copy(out=o, in_=i), reads=_toks(in_), writes=_toks(out))

    def recip(self, out, in_):
        o, i = _ap(out), _ap(in_)
        self.P.op("dve", lambda e: e.reciprocal(out=o, in_=i), reads=_toks(in_), writes=_toks(out))

    def memset(self, out, val, eng="pool"):
        o = _ap(out)
        self.P.op(eng, lambda e: e.memset(o, val), writes=_toks(out))

    def scan(self, out, d0, d1, init, op0=ALU.mult, op1=ALU.add):
        o, a, b, i = _ap(out), _ap(d0), _ap(d1), _ap(init)
        self.P.op("dve", lambda e: e.tensor_tensor_scan(out=o, data0=a, data1=b, initial=i, op0=op0, op1=op1),
                  reads=_toks(d0, d1, init), writes=_toks(out))

    def reduce(self, out, in_, op, axis=AX.X):
        o, i = _ap(out), _ap(in_)
        self.P.op("dve", lambda e: e.tensor_reduce(out=o, in_=i, axis=axis, op=op),
                  reads=_toks(in_), writes=_toks(out))

    def finish(self):
        self.P.emit()
        self.st.close()
        return self.nc


D = 1024
RMS_EPS = 1e-6
C_U, C_FQ, C_FK, C_FV, C_FF, C_CQ, C_CKV, C_KPE, C_DQ, C_DK, C_DV = (
    0, 256, 512, 768, 1024, 1028, 1220, 1348, 1380, 1636, 1892)
D_IN = 2148
WA_COLS = np.concatenate([np.arange(C_U, C_U + 256), np.arange(C_FK, C_FK + 256), np.arange(C_DK, C_DK + 256),
                          np.arange(C_CKV, C_CKV + 128), np.arange(C_KPE, C_KPE + 32),
                          np.arange(C_FV, C_FV + 256), np.arange(C_DV, C_DV + 256), np.arange(C_FF, C_FF + 4)])
NWA = 1444
R_U, R_FK, R_DK, R_CKV, R_KPE, R_KSW, NFEAT = 0, 256, 512, 768, 896, 928, 960


class Scr:
    pass


def alloc_norm_scratch(k, tag=""):
    s = Scr()
    s.junk = k.sb("njunk" + tag, [128, 1024], BF16)
    s.ssq = k.sb("nssq" + tag, [128, 4], F32)
    s.rstd = k.sb("nrstd" + tag, [128, 4], F32)
    s.xn = k.sb("nxn" + tag, [128, 4, 1024], BF16)
    return s


def norm_hT(k, xg, gt, hT, s, pst, ident, flip=0, raw_out=None):
    for b in range(4):
        k.act(s.junk.ap, xg[:, b, :], AF.Square, accum_out=s.ssq[:, b:b + 1])
    k.ts(s.rstd, s.ssq, 1.0 / 1024, ALU.mult, RMS_EPS, ALU.add)
    k.act(s.rstd, s.rstd, AF.Sqrt)
    k.recip(s.rstd, s.rstd)
    for b in range(4):
        k.ts(s.xn[:, b, :], xg[:, b, :], s.rstd[:, b:b + 1], ALU.mult, eng=("dve" if b % 2 == 0 else "pool"))
    for c in range(8):
        pt = pst[c % len(pst)]
        for b in range(4):
            k.tr(pt[:, b * 128:(b + 1) * 128], s.xn[:, b, c * 128:(c + 1) * 128], ident)
        if raw_out is not None:
            k.copy(raw_out[:, c, :], pt[:, 0:512], eng=("act" if (c + flip) % 2 == 0 else "dve"))
            k.ts(hT[:, c, :], raw_out[:, c, :], gt[:, c:c + 1], ALU.mult, eng="pool")
        elif (c + flip) % 2 == 0:
            k.act(hT[:, c, :], pt[:, 0:512], AF.Copy, scale=gt[:, c:c + 1])
        else:
            k.ts(hT[:, c, :], pt[:, 0:512], gt[:, c:c + 1], ALU.mult)


S96 = 96 ** -0.5
TWO_PI = 2.0 * math.pi
RC1 = 6.28125
RC2 = float(np.float32(TWO_PI - 6.28125))
RC3 = float(TWO_PI - 6.28125 - np.float64(np.float32(TWO_PI - 6.28125)))
PI_LO = 3.1415925


def range_reduce(k, out, in_, tmp_i, tmp_f):
    k.ts(tmp_f, in_, 1.0 / TWO_PI, ALU.mult, 0.5, ALU.add)
    k.copy(tmp_i, tmp_f)
    k.copy(tmp_f, tmp_i)
    k.stt(out, tmp_f, -RC1, in_, ALU.mult, ALU.add)
    k.stt(out, tmp_f, -RC2, out, ALU.mult, ALU.add)
    k.stt(out, tmp_f, -RC3, out, ALU.mult, ALU.add)
    k.ts(tmp_f, out, -math.pi, ALU.is_lt)
    k.stt(out, tmp_f, TWO_PI, out, ALU.mult, ALU.add)
    k.ts(tmp_f, out, math.pi, ALU.is_gt)
    k.stt(out, tmp_f, -TWO_PI, out, ALU.mult, ALU.add)
    k.ts(out, out, -PI_LO, ALU.max, PI_LO, ALU.min)


FR_U, FR_FK, FR_DK, FR_MK, NFR = 0, 256, 512, 768, 1152
TC_FV, TC_DV, TC_MV, NTC = 0, 256, 512, 768
NWB = 2180


def build_A(NG):
    T = NG * 512
    k = KB()
    x = k.dram("x", [T, D], F32, "ExternalInput")
    g = k.dram("g", [128, 8], F32, "ExternalInput")
    win = k.dram("win", [D, D_IN], F32, "ExternalInput")
    wuq = k.dram("wuq", [192, 384], F32, "ExternalInput")
    wukv = k.dram("wukv", [128, 512], F32, "ExternalInput")
    gq = k.dram("gq", [128, 2], F32, "ExternalInput")
    gkv = k.dram("gkv", [128, 1], F32, "ExternalInput")
    pos = k.dram("pos", [1, T], I32, "ExternalInput")
    cst = k.dram("cst", [128, 2], F32, "ExternalInput")
    idn = k.dram("identd", [128, 128], BF16, "ExternalInput")
    featT = k.dram("featT", [NFR, T], BF16, "ExternalOutput")
    tokM = k.dram("tokM", [T, NTC], BF16, "ExternalOutput")
    ffM = k.dram("ffM", [T, 4], F32, "ExternalOutput")
    qfT = k.dram("qfT", [256, T], BF16, "ExternalOutput")
    dqT = k.dram("dqT", [256, T], BF16, "ExternalOutput")
    qmT = k.dram("qmT", [384, T], BF16, "ExternalOutput")
    build_A_body(k, NG, x, g, win, wuq, wukv, gq, gkv, pos, cst, idn, featT, tokM, ffM, qfT, dqT, qmT)
    return k.finish()


def build_A_body(k, NG, x, g, win, wuq, wukv, gq, gkv, pos, cst, idn, featT, tokM, ffM, qfT, dqT, qmT):
    ident = k.sb("ident", [128, 128], BF16)
    ones = k.sb("ones", [128, 128], BF16)
    gt = k.sb("gt", [128, 8], F32)
    gqt = k.sb("gqt", [128, 2], F32)
    gkvt = k.sb("gkvt", [128, 1], F32)
    cstt = k.sb("cstt", [128, 2], F32)
    wb = k.sb("wb", [128, 8, NWB], BF16)
    k.dma(ident, idn)
    k.dma(gt, g)
    k.dma(gqt, gq)
    k.dma(gkvt, gkv)
    k.dma(cstt, cst)
    k.memset(ones, 1.0)
    xgs = [k.sb("xg%d" % i, [128, 4, 1024], F32) for i in range(2)]
    k.dma(xgs[0], x[0:512, :].re("(b p) f -> p b f", p=128))
    wst = [k.sb("wst%d" % i, [128, D_IN], F32) for i in range(2)]
    wbt = ["S:wb%d" % kc for kc in range(8)]
    for kc in range(8):
        src = wst[kc % 2]
        k.dma(src, win[kc * 128:(kc + 1) * 128, :], q="pool" if kc % 2 else "sp")
        dst = wb[:, kc, :].sub(wbt[kc])
        e = ("pool", "dve")[kc % 2]
        k.copy(dst[:, 0:D_IN], src, eng=e)
        k.ts(dst[:, D_IN:D_IN + 16], src[:, C_KPE + 16:C_KPE + 32], -1.0, ALU.mult, eng=e)
        k.copy(dst[:, D_IN + 16:D_IN + 32], src[:, C_KPE:C_KPE + 16], eng=e)
    wuq_s = k.sb("wuq_s", [128, 2, 384], F32)
    wukv_s = k.sb("wukv_s", [128, 512], F32)
    k.dma(wuq_s[:, 0, :], wuq[0:128, :])
    k.dma(wuq_s[0:64, 1, :], wuq[128:192, :])
    k.dma(wukv_s, wukv)
    wuq_b = k.sb("wuq_b", [128, 2, 384], BF16)
    wuqsw_b = k.sb("wuqsw_b", [128, 2, 384], BF16)
    wkn_b = k.sb("wkn_b", [128, 4, 64], BF16)
    wv_b = k.sb("wv_b", [128, 4, 64], BF16)
    k.copy(wuq_b[:, 0, :], wuq_s[:, 0, :])
    k.copy(wuq_b[0:64, 1, :], wuq_s[0:64, 1, :])
    k.copy(wuqsw_b[:, 0, :], wuq_s[:, 0, :])
    k.copy(wuqsw_b[0:64, 1, :], wuq_s[0:64, 1, :])
    for c, np_ in ((0, 128), (1, 64)):
        for h in range(4):
            b0 = h * 96 + 64
            k.ts(wuqsw_b[0:np_, c, b0:b0 + 16], wuq_s[0:np_, c, b0 + 16:b0 + 32], -1.0, ALU.mult)
            k.copy(wuqsw_b[0:np_, c, b0 + 16:b0 + 32], wuq_s[0:np_, c, b0:b0 + 16])
    wk4 = wukv_s.re("k (h two d) -> k h two d", h=4, two=2)
    k.copy(wkn_b, wk4[:, :, 0, :])
    k.copy(wv_b, wk4[:, :, 1, :])

    hTs = [k.sb("hT%d" % i, [128, 8, 512], BF16) for i in range(2)]
    scr = alloc_norm_scratch(k)
    pst = [k.ps("pst%d" % i, [128, 1024], BF16) for i in range(2)]
    pss = [k.ps("ps%d" % i, [128, 512], F32) for i in range(6)]
    fst = [k.sb("fst%d" % i, [128, 6, 512], BF16) for i in range(2)]
    qst = [k.sb("qst%d" % i, [128, 4, 512], BF16) for i in range(2)]
    tst = [k.sb("tst%d" % i, [128, 4, NTC], BF16) for i in range(2)]
    ffs = [k.sb("ffs%d" % i, [128, 4, 4], F32) for i in range(2)]
    mkn = [k.sb("mkn%d" % i, [64, 4, 512], BF16) for i in range(2)]
    rk = [k.sb("rk%d" % i, [96, 512], BF16) for i in range(2)]
    qms = [k.sb("qms%d" % i, [96, 4, 512], BF16) for i in range(2)]
    posi = k.sb("posi", [96, 512], I32)
    posf = k.sb("posf", [96, 512], F32)
    ang = k.sb("ang", [96, 512], F32)
    ang2 = k.sb("ang2", [96, 512], F32)
    rtf = k.sb("rtf", [96, 512], F32)
    rti = k.sb("rti", [96, 512], I32)
    Ct = k.sb("Ct", [96, 512], F32)
    St = k.sb("St", [96, 512], F32)
    kpe_s = k.sb("kpe_s", [96, 512], F32)
    ksw_s = k.sb("ksw_s", [96, 512], F32)
    rt1 = k.sb("rt1", [96, 512], F32)
    rt2 = k.sb("rt2", [96, 512], F32)
    ckv_s = k.sb("ckv_s", [128, 512], F32)
    cq_s = k.sb("cq_s", [128, 2, 512], F32)
    sq = k.sb("sq", [128, 2, 512], BF16)
    rstd = k.sb("rstd", [128, 512], F32)
    ckvn = k.sb("ckvn", [128, 512], BF16)
    cqn = k.sb("cqn", [128, 2, 512], BF16)
    R = slice(64, 96)
    pc = [0]

    def nps():
        p_ = pss[pc[0] % 6]
        pc[0] += 1
        return p_

    def proj(c0, m, hT):
        ps = nps()
        for kc in range(8):
            k.mm(ps[0:m, :], wb[:, kc, c0:c0 + m].sub(wbt[kc]), hT[:, kc, :], start=(kc == 0), stop=(kc == 7))
        return ps

    for gi in range(NG):
        xg = xgs[gi % 2]
        if gi + 1 < NG:
            k.dma(xgs[(gi + 1) % 2], x[(gi + 1) * 512:(gi + 2) * 512, :].re("(b p) f -> p b f", p=128))
        hT = hTs[gi % 2]
        norm_hT(k, xg, gt, hT, scr, pst, ident)
        cols = slice(gi * 512, (gi + 1) * 512)
        fs, qs, tsb, ff = fst[gi % 2], qst[gi % 2], tst[gi % 2], ffs[gi % 2]
        k.dma(posi[R, :], pos[0:1, cols].re("o t -> (o) t").ap.partition_broadcast(32)
              if False else V(pos.ap[0:1, cols].partition_broadcast(32), pos.tok))
        k.copy(posf[R, :], posi[R, :])
        k.ts(ang[R, :], posf[R, :], cstt[R, 0:1], ALU.mult)
        k.ts(ang2[R, :], ang[R, :], math.pi / 2, ALU.add)
        range_reduce(k, ang[R, :], ang[R, :], rti[R, :], rtf[R, :])
        range_reduce(k, ang2[R, :], ang2[R, :], rti[R, :], rtf[R, :])
        k.act(St[R, :], ang[R, :], AF.Sin)
        k.act(Ct[R, :], ang2[R, :], AF.Sin)
        for ci, c0 in enumerate((C_U, C_U + 128, C_FK, C_FK + 128, C_DK, C_DK + 128)):
            ps = proj(c0, 128, hT)
            k.copy(fs[:, ci, :], ps, eng=("act" if ci % 2 else "dve"))
        k.dma(featT[0:768, cols].re("(c p) t -> p c t", p=128), fs, key="featT")
        for ci, c0 in enumerate((C_FQ, C_FQ + 128, C_DQ, C_DQ + 128)):
            ps = proj(c0, 128, hT)
            if ci % 2:
                k.act(qs[:, ci, :], ps, AF.Copy, scale=0.125)
            else:
                k.ts(qs[:, ci, :], ps, 0.125, ALU.mult)
        k.dma(qfT[:, cols].re("(c p) t -> p c t", p=128), qs[:, 0:2, :], key="qfT")
        k.dma(dqT[:, cols].re("(c p) t -> p c t", p=128), qs[:, 2:4, :], key="dqT")
        for tb in range(4):
            for (c0, n, o0) in ((C_FV, 256, TC_FV), (C_DV, 256, TC_DV)):
                ps = nps()
                for kc in range(8):
                    k.mm(ps[:, 0:n], hT[:, kc, tb * 128:(tb + 1) * 128], wb[:, kc, c0:c0 + n].sub(wbt[kc]),
                         start=(kc == 0), stop=(kc == 7))
                k.copy(tsb[:, tb, o0:o0 + n], ps[:, 0:n], eng=("act" if tb % 2 else "dve"))
            ps = nps()
            for kc in range(8):
                k.mm(ps[:, 0:4], hT[:, kc, tb * 128:(tb + 1) * 128], wb[:, kc, C_FF:C_FF + 4].sub(wbt[kc]),
                     start=(kc == 0), stop=(kc == 7))
            k.copy(ff[:, tb, :], ps[:, 0:4])
        k.dma(ffM[cols, :].re("(b p) c -> p b c", p=128), ff, key="ffM")
        ps = proj(C_CKV, 128, hT)
        k.copy(ckv_s, ps, eng="act")
        k.act(sq[:, 0, :], ckv_s, AF.Square)
        ps = nps()
        k.mm(ps, ones, sq[:, 0, :])
        k.ts(rstd, ps, 1.0 / 128, ALU.mult, RMS_EPS, ALU.add)
        k.act(rstd, rstd, AF.Sqrt)
        k.recip(rstd, rstd)
        k.stt(ckvn, ckv_s, gkvt[:, 0:1], rstd, ALU.mult, ALU.mult)
        mk = mkn[gi % 2]
        for h in range(4):
            ps = nps()
            k.mm(ps[0:64, :], wkn_b[:, h, :], ckvn)
            k.copy(mk[:, h, :], ps[0:64, :], eng=("act" if h % 2 else "dve"))
        for tb in range(4):
            ps = nps()
            k.mm(ps[:, 0:256], ckvn[:, tb * 128:(tb + 1) * 128], wv_b.re("k h d -> k (h d)"))
            k.copy(tsb[:, tb, TC_MV:TC_MV + 256], ps[:, 0:256], eng=("act" if tb % 2 else "dve"))
        k.dma(tokM[cols, :].re("(b p) c -> p b c", p=128), tsb, key="tokM")
        psA = proj(C_KPE - 64, 96, hT)
        psB = proj(D_IN - 64, 96, hT)
        k.tt(rt1[R, :], psA[R, :], Ct[R, :], ALU.mult)
        k.tt(rt2[R, :], psB[R, :], St[R, :], ALU.mult)
        rkk = rk[gi % 2]
        k.tt(rkk[R, :], rt1[R, :], rt2[R, :], ALU.add)
        for h in range(4):
            r0 = FR_MK + h * 96
            k.dma(featT[r0:r0 + 64, cols], mk[:, h, :], key="featT")
            k.dma(featT[r0 + 64:r0 + 96, cols], rkk[R, :], key="featT")
        ps0 = proj(C_CQ, 128, hT)
        ps1 = proj(C_CQ + 128, 64, hT)
        k.copy(cq_s[:, 0, :], ps0, eng="act")
        k.copy(cq_s[0:64, 1, :], ps1[0:64, :], eng="act")
        k.act(sq[:, 0, :], cq_s[:, 0, :], AF.Square)
        k.act(sq[0:64, 1, :], cq_s[0:64, 1, :], AF.Square)
        ps = nps()
        k.mm(ps, ones, sq[:, 0, :], start=True, stop=False)
        k.mm(ps, ones[0:64, :], sq[0:64, 1, :], start=False, stop=True)
        k.ts(rstd, ps, 1.0 / 192, ALU.mult, RMS_EPS, ALU.add)
        k.act(rstd, rstd, AF.Sqrt)
        k.recip(rstd, rstd)
        k.stt(cqn[:, 0, :], cq_s[:, 0, :], gqt[:, 0:1], rstd, ALU.mult, ALU.mult)
        k.stt(cqn[0:64, 1, :], cq_s[0:64, 1, :], gqt[0:64, 1:2], rstd[0:64, :], ALU.mult, ALU.mult)
        qm = qms[gi % 2]
        for h in range(4):
            pa = nps()
            k.mm(pa[0:96, :], wuq_b[:, 0, h * 96:(h + 1) * 96], cqn[:, 0, :], start=True, stop=False)
            k.mm(pa[0:96, :], wuq_b[0:64, 1, h * 96:(h + 1) * 96], cqn[0:64, 1, :], start=False, stop=True)
            pb = nps()
            k.mm(pb[0:96, :], wuqsw_b[:, 0, h * 96:(h + 1) * 96], cqn[:, 0, :], start=True, stop=False)
            k.mm(pb[0:96, :], wuqsw_b[0:64, 1, h * 96:(h + 1) * 96], cqn[0:64, 1, :], start=False, stop=True)
            k.ts(qm[0:64, h, :], pa[0:64, :], S96, ALU.mult)
            k.stt(rt1[R, :], pa[R, :], S96, Ct[R, :], ALU.mult, ALU.mult)
            k.stt(rt2[R, :], pb[R, :], S96, St[R, :], ALU.mult, ALU.mult)
            k.tt(qm[R, h, :], rt1[R, :], rt2[R, :], ALU.add)
        k.dma(qmT[:, cols].re("(h r) t -> r h t", r=96), qm, key="qmT")


INV_FREQ = (10000.0 ** (-np.arange(0, 32, 2, dtype=np.float32) / 32)).astype(np.float32)
IDENT = np.eye(128, dtype=np.float32).astype(ml_dtypes.bfloat16)


def host_A_inputs(x_own, g, w_in, wuq, wukv, gq, gkv, pos_own):
    cst = np.zeros((128, 2), np.float32)
    cst[64:80, 0] = INV_FREQ
    cst[80:96, 0] = INV_FREQ
    gqp = np.zeros((256,), np.float32)
    gqp[:192] = gq
    return {"x": np.ascontiguousarray(x_own, dtype=np.float32),
            "g": np.ascontiguousarray(g.reshape(8, 128).T),
            "win": np.ascontiguousarray(w_in), "wuq": np.ascontiguousarray(wuq),
            "wukv": np.ascontiguousarray(wukv),
            "gq": np.ascontiguousarray(gqp.reshape(2, 128).T), "gkv": np.ascontiguousarray(gkv.reshape(128, 1)),
            "pos": np.ascontiguousarray(pos_own.reshape(1, -1).astype(np.int32)),
            "cst": cst, "identd": IDENT}


def attn_head(k, nkb, kt_fn, qt, v_fn, bias_fn, mask_fn, pss, pts, yrow_out, rec_t, rows, srows):
    O = pss[3]
    nsc = (0, 1, 2, 4)
    prev = None
    for idx in range(nkb):
        sc = pss[nsc[idx % 4]]
        m = mask_fn(idx)
        k.mm(sc, kt_fn(idx), qt, start=True, stop=(m is None))
        if m is not None:
            k.mm(sc, m[0], m[1], start=False, stop=True)
        pt = pts[idx % len(pts)]
        b = bias_fn(idx)
        if b is None:
            k.act(pt, sc, AF.Exp)
        else:
            k.act(pt, sc, AF.Exp, bias=b)
        if prev is not None:
            pi, pp = prev
            k.mm(O, v_fn(pi), pp, start=(pi == 0), stop=False)
        prev = (idx, pt)
    pi, pp = prev
    k.mm(O, v_fn(pi), pp, start=(pi == 0), stop=True)
    k.recip(rec_t[rows, :], O[srows, :])
    k.tt(yrow_out, O[rows, :], rec_t[rows, :], ALU.mult)


def load_vpad(k, Vp, src, c0, nkb):
    for h in range(4):
        hp = h % 2
        tok = Vp.tok + "h%d" % h
        k.dma(Vp[:, :, h, hp * 64:(hp + 1) * 64].sub(tok),
              src[:, c0 + h * 64:c0 + (h + 1) * 64].re("(kb p) c -> p kb c", p=128), key=tok)
        k.memset(Vp[:, :, h, (1 - hp) * 64:(2 - hp) * 64].sub(tok), 1.0, eng="dve")


def vhead(Vp, kb, h):
    return Vp[:, kb, h, :].sub(Vp.tok + "h%d" % h)


def fox_phase(k, NG, C, kfeat, ktok, kff, qfT, yT_out):
    NS, NK = 2 * NG, 2 * NG * 512
    NKB = NK // 128
    T = NG * 512
    KT = [k.sb("fKT%d" % h, [67, NK], BF16) for h in range(4)]
    QT = [k.sb("fQT%d" % h, [67, T], BF16) for h in range(4)]
    Vt = k.sb("fV", [128, NKB, 4, 128], BF16)
    load_vpad(k, Vt, ktok, TC_FV, NKB)
    for h in range(4):
        k.dma(KT[h][0:64, :], kfeat[FR_FK + h * 64:FR_FK + (h + 1) * 64, :])
        k.memset(KT[h][64:67, :], 1.0, eng="dve")
        k.dma(QT[h][0:64, :], qfT[h * 64:(h + 1) * 64, :])
    ff = k.sb("f_ff", [128, NKB, 4], F32)
    t1 = k.sb("f_t1", [128, NKB, 4], F32)
    t2 = k.sb("f_t2", [128, NKB, 4], F32)
    lf = k.sb("f_lf", [128, NKB, 4], F32)
    cum = k.sb("f_cum", [128, NKB, 4], F32)
    nb = k.sb("f_nb", [128, NKB, 4], F32)
    k.dma(ff, kff.re("(kb p) h -> p kb h", p=128))
    for h in range(4):
        k.ts(ff[:, :, h], ff[:, :, h], C.bft[:, h:h + 1], ALU.add)
    k.ts(t1, ff, -1.0, ALU.mult)
    k.tt(t1, t1, ff, ALU.max)
    k.act(t1, t1, AF.Exp, scale=-1.0)
    k.act(t1, t1, AF.Ln, bias=C.one_col[:, 0:1])
    k.ts(t2, ff, 0.0, ALU.min)
    k.tt(lf, t2, t1, ALU.subtract)
    lf2 = lf.re("p kb h -> p (kb h)")
    pw, ptot = C.pss[0], C.pss[1]
    k.mm(pw[:, 0:NKB * 4], C.tri32, lf2)
    k.mm(ptot[:, 0:NKB * 4], C.ones32, lf2)
    k.copy(t1.re("p kb h -> p (kb h)"), ptot[:, 0:NKB * 4])
    for h in range(4):
        k.scan(t2[:, :, h], C.onesrow[:, 0:NKB], t1[:, :, h], 0.0)
    k.tt(t2, t2, t1, ALU.subtract)
    k.tt(cum.re("p kb h -> p (kb h)"), pw[:, 0:NKB * 4], t2.re("p kb h -> p (kb h)"), ALU.add)
    k.ts(nb, cum, -1.0, ALU.mult)
    for h in range(4):
        k.ts(nb[:, 0:4, h], nb[:, 0:4, h], C.padb[:, 0:1], ALU.add)
    cq = k.sb("f_cq", [4, 512], F32)
    cqh = k.sb("f_cqh", [4, 512], BF16)
    cqm = k.sb("f_cqm", [4, 512], BF16)
    cql = k.sb("f_cql", [4, 512], BF16)
    cqf = k.sb("f_cqf", [4, 512], F32)
    for gi in range(NG):
        pq = C.pss[2 + gi % 2]
        for j in range(4):
            kb = (2 * gi + 1) * 4 + j
            k.tr(pq[0:4, j * 128:(j + 1) * 128], cum[:, kb, :], C.ident32)
        k.copy(cq, pq[0:4, :])
        k.copy(cqh, cq)
        k.copy(cqf, cqh)
        k.tt(cq, cq, cqf, ALU.subtract)
        k.copy(cqm, cq)
        k.copy(cqf, cqm)
        k.tt(cq, cq, cqf, ALU.subtract)
        k.copy(cql, cq)
        cols = slice(gi * 512, (gi + 1) * 512)
        for h in range(4):
            k.dma(QT[h][64:65, cols], cqh[h:h + 1, :], key="fq%d" % h)
            k.dma(QT[h][65:66, cols], cqm[h:h + 1, :], key="fq%d" % h)
            k.dma(QT[h][66:67, cols], cql[h:h + 1, :], key="fq%d" % h)
    for gi in range(NG):
        nkb = (2 * gi + 2) * 4
        cols = slice(gi * 512, (gi + 1) * 512)
        yst = C.yst[gi % 2]
        for h in range(4):
            rows = slice((h % 2) * 64, (h % 2) * 64 + 64)
            srows = slice((1 - h % 2) * 64, (1 - h % 2) * 64 + 64)
            pair = h // 2
            attn_head(k, nkb,
                      lambda kb, h=h: KT[h][:, kb * 128:(kb + 1) * 128],
                      QT[h][:, cols],
                      lambda kb, h=h: vhead(Vt, kb, h),
                      lambda kb, h=h: nb[:, kb, h:h + 1],
                      lambda kb, nkb=nkb: ((C.identb, C.mfox[:, kb - (nkb - 4), :]) if kb >= nkb - 4 else None),
                      C.pss, C.pts, yst[rows, pair, :], C.rec, rows, srows)
        k.dma(yT_out[:, cols].re("(c p) t -> p c t", p=128), yst, key=yT_out.tok)


def mla_phase(k, NG, C, kfeat, ktok, qmT, yT_out):
    NS, NK = 2 * NG, 2 * NG * 512
    NKB = NK // 128
    T = NG * 512
    KT = [k.sb("mKT%d" % h, [96, NK], BF16) for h in range(4)]
    QT = [k.sb("mQT%d" % h, [96, T], BF16) for h in range(4)]
    Vt = k.sb("mV", [128, NKB, 4, 128], BF16)
    load_vpad(k, Vt, ktok, TC_MV, NKB)
    for h in range(4):
        k.dma(KT[h], kfeat[FR_MK + h * 96:FR_MK + (h + 1) * 96, :])
        k.dma(QT[h], qmT[h * 96:(h + 1) * 96, :])
    for gi in range(NG):
        nkb = (2 * gi + 2) * 4
        cols = slice(gi * 512, (gi + 1) * 512)
        yst = C.yst[gi % 2]
        for h in range(4):
            rows = slice((h % 2) * 64, (h % 2) * 64 + 64)
            srows = slice((1 - h % 2) * 64, (1 - h % 2) * 64 + 64)
            pair = h // 2
            attn_head(k, nkb,
                      lambda kb, h=h: KT[h][:, kb * 128:(kb + 1) * 128],
                      QT[h][:, cols],
                      lambda kb, h=h: vhead(Vt, kb, h),
                      lambda kb: (C.padb[:, 0:1] if kb < 4 else None),
                      lambda kb, nkb=nkb: ((C.identb, C.mmla[:, kb - (nkb - 4), :]) if kb >= nkb - 4 else None),
                      C.pss, C.pts, yst[rows, pair, :], C.rec, rows, srows)
        k.dma(yT_out[:, cols].re("(c p) t -> p c t", p=128), yst, key=yT_out.tok)


class Cm:
    pass


def common_B(k, cons):
    C = Cm()
    C.identb = k.sb("identb", [128, 128], BF16)
    C.antib = k.sb("antib", [128, 128], BF16)
    C.onesb = k.sb("onesb", [128, 128], BF16)
    C.ident32 = k.sb("ident32", [128, 128], F32)
    C.tri32 = k.sb("tri32", [128, 128], F32)
    C.ones32 = k.sb("ones32", [128, 128], F32)
    C.onesrow = k.sb("onesrow", [128, 512], F32)
    C.one_col = k.sb("one_col", [128, 1], F32)
    C.padb = k.sb("padb", [128, 1], F32)
    C.bft = k.sb("bft", [128, 4], F32)
    k.dma(C.identb, cons["identd"], key="G:cB")
    k.dma(C.antib, cons["antid"], key="G:cB")
    k.dma(C.ident32, cons["ident32d"], key="G:cB")
    k.dma(C.tri32, cons["tri32d"], key="G:cB")
    k.dma(C.padb, cons["padbd"], key="G:cB")
    k.dma(C.bft, cons["bfd"], key="G:cB")
    k.memset(C.onesb, 1.0)
    k.memset(C.ones32, 1.0)
    k.memset(C.onesrow, 1.0)
    k.memset(C.one_col, 1.0)
    C.pss = [k.ps("ps%d" % i, [128, 512], F32) for i in range(6)]
    C.pst = [k.ps("pst%d" % i, [128, 1024], BF16) for i in range(2)]
    return C


def att_common(k, C, cons):
    C.mfox = k.sb("mfox", [128, 4, 512], BF16)
    C.mmla = k.sb("mmla", [128, 4, 512], BF16)
    k.dma(C.mfox, cons["mfoxd"], key="G:att")
    k.dma(C.mmla, cons["mmlad"], key="G:att")
    C.pts = [k.sb("pT%d" % i, [128, 512], BF16) for i in range(4)]
    C.yst = [k.sb("yst%d" % i, [128, 2, 512], BF16) for i in range(2)]
    C.rec = k.sb("rec", [128, 512], F32)


def host_masks():
    kk = np.arange(128)[:, None, None]
    j = np.arange(4)[None, :, None]
    q = np.arange(512)[None, None, :]
    kidx = j * 128 + kk
    mfox = np.where(kidx <= q, 0.0, NEG).astype(np.float32)
    mmla = np.where(kidx // 64 <= q // 64, 0.0, NEG).astype(np.float32)
    return mfox.astype(ml_dtypes.bfloat16), mmla.astype(ml_dtypes.bfloat16)


ANTI = np.ascontiguousarray(np.eye(128, dtype=np.float32)[::-1]).astype(ml_dtypes.bfloat16)
IDENT32 = np.eye(128, dtype=np.float32)
TRI32 = np.triu(np.ones((128, 128), np.float32))
MFOX, MMLA = host_masks()


def slot_tiles(par, NG):
    return [-1] + list(range(2 * NG - 1)) if par == 0 else list(range(2 * NG))


def to_slots(arr_tiles, par, NG, axis):
    a = np.moveaxis(arr_tiles, axis, 0)
    a = a.reshape((2 * NG, 512) + a.shape[1:])
    out = np.zeros_like(a)
    for s, t in enumerate(slot_tiles(par, NG)):
        if t >= 0:
            out[s] = a[t]
    out = out.reshape((2 * NG * 512,) + a.shape[2:])
    return np.ascontiguousarray(np.moveaxis(out, 0, axis))


def build_B_attn_test(NG):
    T, NK = NG * 512, 2 * NG * 512
    k = KB()
    ins = {}
    for name, shape, dt in (("kfeat", [NFR, NK], BF16), ("ktok", [NK, NTC], BF16), ("kff", [NK, 4], F32),
                            ("qfT", [256, T], BF16), ("qmT", [384, T], BF16),
                            ("identd", [128, 128], BF16), ("antid", [128, 128], BF16),
                            ("ident32d", [128, 128], F32), ("tri32d", [128, 128], F32),
                            ("padbd", [128, 1], F32), ("bfd", [128, 4], F32),
                            ("mfoxd", [128, 4, 512], BF16), ("mmlad", [128, 4, 512], BF16)):
        ins[name] = k.dram(name, shape, dt, "ExternalInput")
    yB = k.dram("yB", [256, T], BF16, "ExternalOutput")
    yC = k.dram("yC", [256, T], BF16, "ExternalOutput")
    C = common_B(k, ins)
    att_common(k, C, ins)
    fox_phase(k, NG, C, ins["kfeat"], ins["ktok"], ins["kff"], ins["qfT"], yB)
    mla_phase(k, NG, C, ins["kfeat"], ins["ktok"], ins["qmT"], yC)
    return k.finish()


def host_chunk_mask():
    kkp = np.arange(128)[:, None, None]
    j = np.arange(8)[None, :, None]
    q = np.arange(512)[None, None, :]
    kidx = j * 128 + (127 - kkp)
    dc = (q + 512) // 64 - kidx // 64
    return np.where((dc >= 0) & (dc <= 8), 0.0, NEG).astype(np.float32)


MCHK = host_chunk_mask()
JROW = np.tile(np.arange(512, dtype=np.float32)[None], (128, 1))


def chunk_phase(k, NG, C, kfeat, ktok, dqT, relb, mchkd, Ed, yT_out):
    T = NG * 512
    rb = k.sb("c_rb", [4, 513], F32)
    E = k.sb("c_E", [4, 1536], F32)
    k.dma(rb, relb)
    k.copy(E[:, 255:768], rb)
    k.ts(E[:, 0:255], C.onesrow[0:4, 0:255], rb[:, 0:1], ALU.mult)
    k.ts(E[:, 768:1536], C.onesrow[0:4, 0:256].ap.unsqueeze(1).to_broadcast([4, 3, 256]) if False else
         V(C.onesrow.ap[0:4, 0:256], C.onesrow.tok), rb[:, 512:513], ALU.mult) if False else None
    for j3 in range(3):
        k.ts(E[:, 768 + j3 * 256:768 + (j3 + 1) * 256], C.onesrow[0:4, 0:256], rb[:, 512:513], ALU.mult)
    k.dma(Ed, E)
    Tb = k.sb("c_Tb", [128, 4, 8, 512], BF16)
    mc = k.sb("c_mc", [128, 8, 512], F32)
    k.dma(mc, mchkd)
    tfs = [k.sb("c_tf%d" % i, [128, 512], F32) for i in range(2)]
    n = 0
    for h in range(4):
        for j in range(8):
            tf = tfs[n % 2]
            src = bass.AP(Ed.ap.tensor, h * 1536 + 896 - 128 * j, [[1, 128], [1, 512]])
            k.dma(tf, V(src, Ed.tok), q=("sp", "pool")[n % 2])
            k.tt(Tb[:, h, j, :], tf, mc[:, j, :], ALU.add, eng=("dve", "pool")[n % 2])
            n += 1
    KD = [k.sb("c_KD%d" % i, [128, 2, 1024], BF16) for i in range(2)]
    QD = [k.sb("c_QD%d" % i, [128, 2, 512], BF16) for i in range(2)]
    VD = [k.sb("c_VD%d" % i, [128, 8, 4, 128], BF16) for i in range(2)]
    for gi in range(NG):
        cols = slice(gi * 512, (gi + 1) * 512)
        kc = slice(2 * gi * 512, (2 * gi + 2) * 512)
        kd, qd, vd = KD[gi % 2], QD[gi % 2], VD[gi % 2]
        k.dma(kd, kfeat[FR_DK:FR_DK + 256, kc].re("(c p) t -> p c t", p=128))
        k.dma(qd, dqT[:, cols].re("(c p) t -> p c t", p=128), q="pool")
        load_vpad(k, vd, ktok[kc, :], TC_DV, 8)
        yst = C.yst[gi % 2]
        for h in range(4):
            rows = slice((h % 2) * 64, (h % 2) * 64 + 64)
            srows = slice((1 - h % 2) * 64, (1 - h % 2) * 64 + 64)
            pair = h // 2
            attn_head(k, 8,
                      lambda kb, pair=pair, rows=rows: kd[rows, pair, kb * 128:(kb + 1) * 128],
                      qd[rows, pair, :],
                      lambda kb, h=h, vd=vd: vhead(vd, kb, h),
                      lambda kb, gi=gi: (C.padb[:, 0:1] if (gi == 0 and kb < 4) else None),
                      lambda kb, h=h: (C.antib, Tb[:, h, kb, :]),
                      C.pss, C.pts, yst[rows, pair, :], C.rec, rows, srows)
        k.dma(yT_out[:, cols].re("(c p) t -> p c t", p=128), yst, key=yT_out.tok)


GELU_C = 2.0 * math.sqrt(2.0 / math.pi)


def s5_phase(k, NG, C, kfeat, sp, yT_out):
    NS = 2 * NG
    f32t = lambda n, sh: k.sb("s_" + n, sh, F32)
    X = {}
    for nm in ("arX", "aiX", "ldtX", "brX", "biX"):
        X[nm] = f32t(nm, [128, 128])
        k.dma(X[nm], sp[nm], key="G:s5")
    dt = f32t("dt", [128, 128])
    mag = f32t("mag", [128, 128])
    th = f32t("th", [128, 128])
    th2 = f32t("th2", [128, 128])
    tf = f32t("tf", [128, 128])
    ti = k.sb("s_ti", [128, 128], I32)
    sn = f32t("sn", [128, 128])
    cs = f32t("cs", [128, 128])
    a1 = f32t("a1", [128, 128])
    a2 = f32t("a2", [128, 128])
    a3 = f32t("a3", [128, 128])
    a4 = f32t("a4", [128, 128])
    bbr = f32t("bbr", [128, 128])
    bbi = f32t("bbi", [128, 128])
    k.act(dt, X["ldtX"], AF.Exp)
    k.tt(mag, X["arX"], dt, ALU.mult)
    k.act(mag, mag, AF.Exp)
    k.tt(th, X["aiX"], dt, ALU.mult)
    k.ts(th2, th, math.pi / 2, ALU.add)
    range_reduce(k, th, th, ti, tf)
    range_reduce(k, th2, th2, ti, tf)
    k.act(sn, th, AF.Sin)
    k.act(cs, th2, AF.Sin)
    k.tt(a1, mag, cs, ALU.mult)
    k.tt(a2, mag, sn, ALU.mult)
    k.ts(a1, a1, -1.0, ALU.add)
    k.tt(a3, X["arX"], X["arX"], ALU.mult)
    k.tt(a4, X["aiX"], X["aiX"], ALU.mult)
    k.tt(a3, a3, a4, ALU.add)
    k.recip(a3, a3)
    k.tt(a4, a1, X["arX"], ALU.mult)
    k.tt(tf, a2, X["aiX"], ALU.mult)
    k.tt(a4, a4, tf, ALU.add)
    k.tt(a4, a4, a3, ALU.mult)
    k.tt(tf, a2, X["arX"], ALU.mult)
    k.tt(a1, a1, X["aiX"], ALU.mult)
    k.tt(tf, tf, a1, ALU.subtract)
    k.tt(tf, tf, a3, ALU.mult)
    k.tt(bbr, a4, X["brX"], ALU.mult)
    k.tt(a1, tf, X["biX"], ALU.mult)
    k.tt(bbr, bbr, a1, ALU.subtract)
    k.tt(bbi, a4, X["biX"], ALU.mult)
    k.tt(a1, tf, X["brX"], ALU.mult)
    k.tt(bbi, bbi, a1, ALU.add)
    mB = f32t("mB", [128, 4, 128])
    k.dma(mB, sp["maskB"], key="G:s5")
    cdr = f32t("cdr", [128, 2, 128])
    cdi = f32t("cdi", [128, 2, 128])
    k.dma(cdr, sp["CdupR"], key="G:s5")
    k.dma(cdi, sp["CdupI"], key="G:s5")
    mCt = f32t("mC", [128, 4, 128])
    k.dma(mCt, sp["maskC"], key="G:s5")
    LB = k.sb("s_LB", [128, 8, 2, 128], BF16)
    LC = k.sb("s_LC", [128, 8, 2, 128], BF16)
    for gp in range(8):
        c, kq = gp // 4, gp % 4
        for ri, src in enumerate((bbr, bbi)):
            for g2 in range(2):
                k.tt(LB[:, gp, ri, g2 * 64:(g2 + 1) * 64], src[:, c * 64:(c + 1) * 64],
                     mB[:, kq, g2 * 64:(g2 + 1) * 64], ALU.mult, eng=("dve", "pool")[g2])
        k.tt(LC[:, gp, 0, :], cdr[:, c, :], mCt[:, kq, :], ALU.mult)
        k.stt(LC[:, gp, 1, :], cdi[:, c, :], -1.0, mCt[:, kq, :], ALU.mult, ALU.mult)
    Y = {}
    for nm in ("arY", "aiY", "ldtY"):
        Y[nm] = f32t(nm, [128, 8])
        k.dma(Y[nm], sp[nm], key="G:s5")
    dtY = f32t("dtY", [128, 8])
    rho = f32t("rho", [128, 8])
    thY = f32t("thY", [128, 8])
    k.act(dtY, Y["ldtY"], AF.Exp)
    k.tt(rho, Y["arY"], dtY, ALU.mult)
    k.act(rho, rho, AF.Exp)
    k.tt(thY, Y["aiY"], dtY, ALU.mult)
    t5 = f32t("t5", [128, 8])
    t5b = f32t("t5b", [128, 8])
    t5f = f32t("t5f", [128, 8])
    t5i = k.sb("s_t5i", [128, 8], I32)
    s512 = f32t("s512", [128, 8])
    c512 = f32t("c512", [128, 8])
    k.ts(t5, thY, 512.0, ALU.mult)
    k.ts(t5b, t5, math.pi / 2, ALU.add)
    range_reduce(k, t5, t5, t5i, t5f)
    range_reduce(k, t5b, t5b, t5i, t5f)
    k.act(s512, t5, AF.Sin)
    k.act(c512, t5b, AF.Sin)
    jrow = f32t("jrow", [128, 512])
    k.dma(jrow, sp["jrow"], key="G:s5")
    cosT = f32t("cosT", [128, 8, 512])
    sinT = f32t("sinT", [128, 8, 512])
    rhoT = f32t("rhoT", [128, 8, 512])
    ag = f32t("ag", [128, 512])
    ag2 = f32t("ag2", [128, 512])
    agf = f32t("agf", [128, 512])
    agi = k.sb("s_agi", [128, 512], I32)
    for gp in range(8):
        k.ts(ag, jrow, thY[:, gp:gp + 1], ALU.mult)
        k.ts(ag2, ag, math.pi / 2, ALU.add)
        range_reduce(k, ag, ag, agi, agf)
        range_reduce(k, ag2, ag2, agi, agf)
        k.act(sinT[:, gp, :], ag, AF.Sin)
        k.act(cosT[:, gp, :], ag2, AF.Sin)
        k.ts(rhoT[:, gp, :], C.onesrow, rho[:, gp:gp + 1], ALU.mult, eng="pool")
    dX = f32t("dX", [128, 2])
    k.dma(dX, sp["dX"], key="G:s5")
    wg_s = f32t("wg_s", [128, 2, 256])
    k.dma(wg_s, sp["wglu"].re("(c p) f -> p c f", p=128), key="G:s5")
    wg_b = k.sb("s_wg_b", [128, 2, 256], BF16)
    k.copy(wg_b, wg_s)
    Wre = f32t("Wre", [128, 8, 512])
    Wim = f32t("Wim", [128, 8, 512])
    ini_re = f32t("ini_re", [128, 8])
    ini_im = f32t("ini_im", [128, 8])
    k.memset(ini_re, 0.0)
    k.memset(ini_im, 0.0)
    c1 = f32t("c1", [128, 8])
    c2 = f32t("c2", [128, 8])
    uTs = [k.sb("s_uT%d" % i, [128, 2, 512], BF16) for i in range(2)]
    prs = [f32t("prs%d" % i, [128, 512]) for i in range(2)]
    pis = [f32t("pis%d" % i, [128, 512]) for i in range(2)]
    q1 = [f32t("q1_%d" % i, [128, 512]) for i in range(2)]
    q2 = [f32t("q2_%d" % i, [128, 512]) for i in range(2)]
    q3, q4 = q1, q2
    are = [f32t("are%d" % i, [128, 512]) for i in range(2)]
    aim = [f32t("aim%d" % i, [128, 512]) for i in range(2)]
    xr = [k.sb("s_xr%d" % i, [128, 512], BF16) for i in range(2)]
    xi = [k.sb("s_xi%d" % i, [128, 512], BF16) for i in range(2)]
    yv = f32t("yv", [128, 2, 512])
    y2 = f32t("y2", [128, 512])
    ygT = k.sb("s_ygT", [128, 2, 512], BF16)
    sg = f32t("sg", [128, 512])
    pss = C.pss
    for s in range(NS):
        uT = uTs[s % 2]
        k.dma(uT, kfeat[FR_U:FR_U + 256, s * 512:(s + 1) * 512].re("(c p) t -> p c t", p=128))
        own = (s % 2 == 1)
        for gp in range(8):
            c, kq = gp // 4, gp % 4
            b = gp % 2
            pa, pb = pss[(2 * gp) % 4], pss[(2 * gp + 1) % 4]
            k.mm(pa, LB[:, gp, 0, :], uT[:, c, :])
            k.mm(pb, LB[:, gp, 1, :], uT[:, c, :])
            k.copy(prs[b], pa, eng="act")
            k.copy(pis[b], pb, eng="act")
            k.tt(q1[b], prs[b], cosT[:, gp, :], ALU.mult)
            k.tt(q2[b], pis[b], sinT[:, gp, :], ALU.mult, eng="pool")
            k.tt(are[b], q1[b], q2[b], ALU.add)
            k.tt(q3[b], pis[b], cosT[:, gp, :], ALU.mult)
            k.tt(q4[b], prs[b], sinT[:, gp, :], ALU.mult, eng="pool")
            k.tt(aim[b], q3[b], q4[b], ALU.subtract)
            k.scan(Wre[:, gp, :], rhoT[:, gp, :], are[b], ini_re[:, gp:gp + 1])
            k.scan(Wim[:, gp, :], rhoT[:, gp, :], aim[b], ini_im[:, gp:gp + 1])
            if own:
                k.tt(q1[b], Wre[:, gp, :], cosT[:, gp, :], ALU.mult)
                k.tt(q2[b], Wim[:, gp, :], sinT[:, gp, :], ALU.mult, eng="pool")
                k.tt(xr[b], q1[b], q2[b], ALU.subtract)
                k.tt(q3[b], Wre[:, gp, :], sinT[:, gp, :], ALU.mult, eng="pool")
                k.tt(q4[b], Wim[:, gp, :], cosT[:, gp, :], ALU.mult)
                k.tt(xi[b], q3[b], q4[b], ALU.add)
                py = pss[4 + c]
                k.mm(py, LC[:, gp, 0, :], xr[b], start=(kq == 0), stop=False)
                k.mm(py, LC[:, gp, 1, :], xi[b], start=False, stop=(kq == 3))
        if s + 1 < NS:
            k.tt(c1, c512, Wre[:, :, 511], ALU.mult)
            k.tt(c2, s512, Wim[:, :, 511], ALU.mult)
            k.tt(ini_re, c1, c2, ALU.subtract)
            k.tt(c1, s512, Wre[:, :, 511], ALU.mult)
            k.tt(c2, c512, Wim[:, :, 511], ALU.mult)
            k.tt(ini_im, c1, c2, ALU.add)
        if own:
            gi = s // 2
            cols = slice(gi * 512, (gi + 1) * 512)
            for c in range(2):
                k.stt(yv[:, c, :], uT[:, c, :], dX[:, c:c + 1], pss[4 + c], ALU.mult, ALU.add)
                k.tt(y2, yv[:, c, :], yv[:, c, :], ALU.mult)
                k.ts(y2, y2, 0.044715, ALU.mult, 1.0, ALU.add)
                k.tt(y2, y2, yv[:, c, :], ALU.mult)
                k.act(sg, y2, AF.Sigmoid, scale=GELU_C)
                k.tt(ygT[:, c, :], yv[:, c, :], sg, ALU.mult)
            yst = C.yst[gi % 2]
            for fc in range(2):
                ps = pss[fc]
                k.mm(ps, wg_b[:, 0, fc * 128:(fc + 1) * 128], ygT[:, 0, :], start=True, stop=False)
                k.mm(ps, wg_b[:, 1, fc * 128:(fc + 1) * 128], ygT[:, 1, :], start=False, stop=True)
                k.act(sg, ps, AF.Sigmoid)
                k.tt(yst[:, fc, :], ygT[:, fc, :], sg, ALU.mult)
            k.dma(yT_out[:, cols].re("(c p) t -> p c t", p=128), yst, key=yT_out.tok)


def host_s5_params(a_re, a_im, log_dt, b_re, b_im, c_re, c_im, d, wglu):
    f = np.float32
    def Xl(a):
        a = a.reshape(2, 8, 64)
        return np.ascontiguousarray(np.broadcast_to(a.transpose(1, 0, 2)[:, None], (8, 16, 2, 64)).reshape(128, 128), dtype=f)
    ldt2 = np.broadcast_to(log_dt[:, None], (16, 64))
    def Bl(b):
        b = b.reshape(2, 8, 64, 16)
        return np.ascontiguousarray(b.transpose(1, 3, 0, 2).reshape(128, 128), dtype=f)
    def Yl(a):
        a = a.reshape(8, 2, 64)
        return np.ascontiguousarray(a.transpose(1, 2, 0).reshape(128, 8), dtype=f)
    def Cl(cm):
        cm = cm.reshape(2, 8, 16, 64)
        t = cm.transpose(3, 0, 1, 2).reshape(64, 2, 128)
        return np.ascontiguousarray(np.concatenate([t, t], 0), dtype=f)
    maskB = np.zeros((128, 4, 128), f)
    for kq in range(4):
        for g2 in range(2):
            gl = 2 * kq + g2
            maskB[gl * 16:(gl + 1) * 16, kq, g2 * 64:(g2 + 1) * 64] = 1.0
    maskC = np.ascontiguousarray(maskB.transpose(2, 1, 0))
    return {"arX": Xl(a_re), "aiX": Xl(a_im), "ldtX": Xl(ldt2), "brX": Bl(b_re), "biX": Bl(b_im),
            "arY": Yl(a_re), "aiY": Yl(a_im), "ldtY": Yl(ldt2), "CdupR": Cl(c_re), "CdupI": Cl(c_im),
            "maskB": maskB, "maskC": maskC, "jrow": JROW,
            "dX": np.ascontiguousarray(d.reshape(2, 128).T, dtype=f), "wglu": np.ascontiguousarray(wglu, dtype=f)}


S5_SHAPES = {"arX": [128, 128], "aiX": [128, 128], "ldtX": [128, 128], "brX": [128, 128], "biX": [128, 128],
             "arY": [128, 8], "aiY": [128, 8], "ldtY": [128, 8], "CdupR": [128, 2, 128], "CdupI": [128, 2, 128],
             "maskB": [128, 4, 128], "maskC": [128, 4, 128], "jrow": [128, 512], "dX": [128, 2], "wglu": [256, 256]}


def build_B_mix2_test(NG):
    T, NK = NG * 512, 2 * NG * 512
    k = KB()
    ins = {}
    for name, shape, dt in (("kfeat", [NFR, NK], BF16), ("ktok", [NK, NTC], BF16), ("dqT", [256, T], BF16),
                            ("identd", [128, 128], BF16), ("antid", [128, 128], BF16),
                            ("ident32d", [128, 128], F32), ("tri32d", [128, 128], F32),
                            ("padbd", [128, 1], F32), ("bfd", [128, 4], F32),
                            ("mfoxd", [128, 4, 512], BF16), ("mmlad", [128, 4, 512], BF16),
                            ("relb", [4, 513], F32), ("mchkd", [128, 8, 512], F32)):
        ins[name] = k.dram(name, shape, dt, "ExternalInput")
    sp = {nm: k.dram("s5_" + nm, sh, F32, "ExternalInput") for nm, sh in S5_SHAPES.items()}
    Ed = k.dram("Ed", [4, 1536], F32, "Internal")
    yD = k.dram("yD", [256, T], BF16, "ExternalOutput")
    yA = k.dram("yA", [256, T], BF16, "ExternalOutput")
    C = common_B(k, ins)
    att_common(k, C, ins)
    with k.phase():
        chunk_phase(k, NG, C, ins["kfeat"], ins["ktok"], ins["dqT"], ins["relb"], ins["mchkd"], Ed, yD)
    with k.phase():
        s5_phase(k, NG, C, ins["kfeat"], sp, yA)
    return k.finish()


CV_F = 1024


def convert_weight(k, cv, name, src, shape):
    n = int(np.prod(shape))
    assert n % (128 * CV_F) == 0, (name, shape)
    dst = k.dram("wc_" + name, list(shape), BF16, "Internal")
    letters = "abcd"[:len(shape)]
    flat = "%s -> (%s)" % (" ".join(letters), " ".join(letters))
    sv = src.re(flat).re("(n p f) -> n p f", p=128, f=CV_F)
    dv = dst.re(flat).re("(n p f) -> n p f", p=128, f=CV_F)
    k.P.bg_mode = True
    try:
        _convert_tiles(k, cv, sv, dv, dst, n // (128 * CV_F))
    finally:
        k.P.bg_mode = False
    return dst


def _convert_tiles(k, cv, sv, dv, dst, ntiles):
    for i in range(ntiles):
        st, cb = cv["st"][cv["n"] % 2], cv["cb"][cv["n"] % 2]
        cv["n"] += 1
        k.dma(st, sv[i], q="pool")
        k.copy(cb, st, eng="pool")
        k.dma(dv[i], cb, q="pool", key=dst.tok)
    return dst


class WS:
    def __init__(self, k, nbf=5):
        self.k = k
        self.bf = [k.sb("w_bf%d" % i, [128, 8, 512], BF16) for i in range(nbf)]
        self.n = 0

    def load(self, src, KC, ncols):
        k = self.k
        bf = self.bf[self.n % len(self.bf)]
        self.n += 1
        k.dma(bf[:, 0:KC, 0:ncols], src.re("(kc p) c -> p kc c", p=128))
        return bf[:, 0:KC, 0:ncols]


def local_phase(k, NG, C, kind, final, x_in, p_in, yTs, W, x_out):
    T = NG * 512
    ws = WS(k)
    pss, pst = C.pss, C.pst
    ident = C.identb
    gts = {}
    for nm in ("g_mix", "g_ffn", "g_ple"):
        gts[nm] = k.sb("l_" + nm, [128, 8], F32)
        k.dma(gts[nm], W[nm], key="G:loc")
    bg = k.sb("l_bg", [128, 4, 8], F32)
    k.dma(bg, W["b_gate"], key="G:loc")
    scr = alloc_norm_scratch(k, "l")
    xgs = [k.sb("l_xg%d" % i, [128, 4, 1024], F32) for i in range(2)]
    hT = k.sb("l_hT", [128, 8, 512], BF16)
    YT = k.sb("l_YT", [128, 4, 2, 512], BF16)
    macc = k.sb("l_macc", [128, 4, 512], F32)
    mT = k.sb("l_mT", [128, 8, 512], BF16)
    gs = [k.sb("l_gs%d" % i, [128, 512], F32) for i in range(2)]
    tmp = [k.sb("l_tmp%d" % i, [128, 512], F32) for i in range(2)]
    nfc = 22 if kind == "ffn" else 11
    hid = k.sb("l_hid", [128, nfc, 512], BF16)
    pf = k.sb("l_pf", [128, 4, 256], F32)
    pb = k.sb("l_pb", [128, 4, 256], BF16)
    pT = k.sb("l_pT", [128, 2, 512], BF16)
    if final:
        gfin = k.sb("l_gfin", [128, 1024], F32)
        k.dma(gfin, W["g_final"], key="G:loc")
    if kind == "moe":
        xhT = k.sb("l_xhT", [128, 8, 512], BF16)
        xlT = k.sb("l_xlT", [128, 8, 512], BF16)
        xn32 = k.sb("l_xn32", [128, 1024], F32)
        xnf = k.sb("l_xnf", [128, 1024], F32)
        xlo = k.sb("l_xlo", [128, 4, 1024], BF16)
        wr_s = k.sb("l_wr_s", [128, 8, 8], F32)
        wg32 = k.sb("l_wg32", [128, 8, 8], F32)
        wgf = k.sb("l_wgf", [128, 8, 8], F32)
        wg_hi = k.sb("l_wg_hi", [128, 8, 8], BF16)
        wg_lo = k.sb("l_wg_lo", [128, 8, 8], BF16)
        brt = k.sb("l_brt", [128, 8], F32)
        k.dma(wr_s, W["w_router"].re("(kc p) e -> p kc e", p=128), key="G:loc")
        k.dma(brt, W["b_router"], key="G:loc")
        for kc in range(8):
            k.ts(wg32[:, kc, :], wr_s[:, kc, :], gts["g_ffn"][:, kc:kc + 1], ALU.mult)
        k.copy(wg_hi, wg32)
        k.copy(wgf, wg_hi)
        k.tt(wg32, wg32, wgf, ALU.subtract)
        k.copy(wg_lo, wg32)
        lg = k.sb("l_lg", [128, 4, 8], F32)
        lg2 = k.sb("l_lg2", [128, 4, 8], F32)
        eq1 = k.sb("l_eq1", [128, 4, 8], F32)
        eq2 = k.sb("l_eq2", [128, 4, 8], F32)
        wts = k.sb("l_wts", [128, 4, 8], F32)
        m1 = k.sb("l_m1", [128, 4], F32)
        m2 = k.sb("l_m2", [128, 4], F32)
        g1 = k.sb("l_g1", [128, 4], F32)
        g2 = k.sb("l_g2", [128, 4], F32)

    def xadd(xg, tb, half, ps):
        xs = xg[:, tb, half * 512:(half + 1) * 512]
        k.tt(xs, xs, ps, ALU.add)

    k.dma(xgs[0], x_in[0:512, :].re("(b p) f -> p b f", p=128))
    for gi in range(NG):
        xg = xgs[gi % 2]
        cols = slice(gi * 512, (gi + 1) * 512)
        if gi + 1 < NG:
            k.dma(xgs[(gi + 1) % 2], x_in[(gi + 1) * 512:(gi + 2) * 512, :].re("(b p) f -> p b f", p=128), q="pool")
        for mi in range(4):
            k.dma(YT[:, mi, :, :].sub("S:l_YT%d" % mi), yTs[mi][:, cols].re("(c p) t -> p c t", p=128),
                  q="pool", key="l_YT%d" % mi)
        k.dma(pf, p_in[cols, :].re("(b p) c -> p b c", p=128), q="pool")
        norm_hT(k, xg, gts["g_mix"], hT, scr, pst, ident)
        for fc4 in range(2):
            for br in range(4):
                gtile = ws.load(W["w_gate"][br][:, fc4 * 512:(fc4 + 1) * 512], 8, 512)
                btile = ws.load(W["w_branch"][br][:, fc4 * 512:(fc4 + 1) * 512], 2, 512)
                for fl in range(4):
                    fc = fc4 * 4 + fl
                    p1, p2 = pss[(2 * fl) % 6], pss[(2 * fl + 1) % 6]
                    for kc in range(8):
                        k.mm(p1, gtile[:, kc, fl * 128:(fl + 1) * 128], hT[:, kc, :], start=(kc == 0), stop=(kc == 7))
                    g_ = gs[fl % 2]
                    k.act(g_, p1, AF.Sigmoid, bias=bg[:, br, fc:fc + 1])
                    for c in range(2):
                        k.mm(p2, btile[:, c, fl * 128:(fl + 1) * 128], YT[:, br, c, :].sub("S:l_YT%d" % br),
                             start=(c == 0), stop=(c == 1))
                    if br == 0:
                        k.tt(macc[:, fl, :], g_, p2, ALU.mult)
                    else:
                        t_ = tmp[fl % 2]
                        k.tt(t_, g_, p2, ALU.mult)
                        if br < 3:
                            k.tt(macc[:, fl, :], macc[:, fl, :], t_, ALU.add, eng="pool")
                        else:
                            k.tt(mT[:, fc, :], macc[:, fl, :], t_, ALU.add, eng="pool")
        for half in range(2):
            wt = ws.load(W["w_o"][:, half * 512:(half + 1) * 512], 8, 512)
            for tb in range(4):
                ps = pss[(half * 4 + tb) % 6]
                for kc in range(8):
                    k.mm(ps, mT[:, kc, tb * 128:(tb + 1) * 128], wt[:, kc, :], start=(kc == 0), stop=(kc == 7))
                xadd(xg, tb, half, ps)
        import os
        upto = int(os.environ.get("LOCAL_UPTO", "9"))
        if upto < 1:
            k.dma(x_out[cols, :].re("(b p) f -> p b f", p=128), xg, key=x_out.tok)
            continue
        if kind == "ffn":
            norm_hT(k, xg, gts["g_ffn"], hT, scr, pst, ident, flip=1)
            ffn_expert(k, ws, pss, hT, hid, gs, W["w1"], W["w3"], W["w2"], 2816,
                       lambda tb, half, ps: xadd(xg, tb, half, ps))
        else:
            norm_hT(k, xg, gts["g_ffn"], hT, scr, pst, ident, flip=1, raw_out=xhT)
            import os
            mstop = int(os.environ.get("MOE_STOP", "9"))
            for b in range(4 if mstop >= 1 else 0):
                k.ts(xn32, xg[:, b, :], scr.rstd[:, b:b + 1], ALU.mult)
                k.copy(xnf, scr.xn[:, b, :], eng="pool")
                k.tt(xlo[:, b, :], xn32, xnf, ALU.subtract)
            for c in range(8 if mstop >= 2 else 0):
                pt = pst[c % 2]
                for b in range(4):
                    k.tr(pt[:, b * 128:(b + 1) * 128], xlo[:, b, c * 128:(c + 1) * 128], ident)
                k.copy(xlT[:, c, :], pt[:, 0:512], eng=("act" if c % 2 else "dve"))
            for tb in range(4 if mstop >= 3 else 0):
                ps = pss[tb % 6]
                tsl = slice(tb * 128, (tb + 1) * 128)
                n = 0
                for (a_, w_) in ((xhT, wg_hi), (xlT, wg_hi), (xhT, wg_lo)):
                    for kc in range(8):
                        k.mm(ps[:, 0:8], a_[:, kc, tsl], w_[:, kc, :], start=(n == 0), stop=(n == 23))
                        n += 1
                k.tt(lg[:, tb, :], ps[:, 0:8], brt, ALU.add)
            if mstop < 4:
                k.dma(x_out[cols, :].re("(b p) f -> p b f", p=128), xg, key=x_out.tok)
                continue
            k.reduce(m1, lg, ALU.max)
            for tb in range(4):
                k.ts(eq1[:, tb, :], lg[:, tb, :], m1[:, tb:tb + 1], ALU.is_equal)
            k.stt(lg2, eq1, NEG, lg, ALU.mult, ALU.add)
            k.reduce(m2, lg2, ALU.max)
            for tb in range(4):
                k.ts(eq2[:, tb, :], lg2[:, tb, :], m2[:, tb:tb + 1], ALU.is_equal)
            k.tt(g1, m1, m2, ALU.subtract)
            k.act(g1, g1, AF.Sigmoid)
            k.ts(g2, g1, -1.0, ALU.mult, 1.0, ALU.add)
            for tb in range(4):
                k.ts(eq1[:, tb, :], eq1[:, tb, :], g1[:, tb:tb + 1], ALU.mult)
                k.stt(wts[:, tb, :], eq2[:, tb, :], g2[:, tb:tb + 1], eq1[:, tb, :], ALU.mult, ALU.add)
            import os
            for e in range(int(os.environ.get("MOE_NE", "8"))):
                def addw(tb, half, ps, e=e):
                    xs = xg[:, tb, half * 512:(half + 1) * 512]
                    k.stt(xs, ps, wts[:, tb, e:e + 1], xs, ALU.mult, ALU.add)
                ffn_expert(k, ws, pss, hT, hid, gs, W["w1"][e], W["w3"][e], W["w2"][e], 1408, addw)
        if upto < 2:
            k.dma(x_out[cols, :].re("(b p) f -> p b f", p=128), xg, key=x_out.tok)
            continue
        norm_hT(k, xg, gts["g_ple"], hT, scr, pst, ident)
        k.copy(pb, pf)
        for c in range(2):
            pt = pst[c % 2]
            for b in range(4):
                k.tr(pt[:, b * 128:(b + 1) * 128], pb[:, b, c * 128:(c + 1) * 128], ident)
            k.copy(pT[:, c, :], pt[:, 0:512], eng=("act" if c % 2 else "dve"))
        for half in range(2):
            wpg = ws.load(W["ple_w_gate"][:, half * 512:(half + 1) * 512], 8, 512)
            wup = ws.load(W["ple_w_up"][:, half * 512:(half + 1) * 512], 2, 512)
            for tb in range(4):
                p1, p2 = pss[(2 * tb) % 6], pss[(2 * tb + 1) % 6]
                tsl = slice(tb * 128, (tb + 1) * 128)
                for kc in range(8):
                    k.mm(p1, hT[:, kc, tsl], wpg[:, kc, :], start=(kc == 0), stop=(kc == 7))
                g_ = gs[tb % 2]
                k.act(g_, p1, AF.Sigmoid)
                for c in range(2):
                    k.mm(p2, pT[:, c, tsl], wup[:, c, :], start=(c == 0), stop=(c == 1))
                t_ = tmp[tb % 2]
                k.tt(t_, g_, p2, ALU.mult)
                xadd(xg, tb, half, t_)
        if final:
            for b in range(4):
                k.act(scr.junk.ap, xg[:, b, :], AF.Square, accum_out=scr.ssq[:, b:b + 1])
            k.ts(scr.rstd, scr.ssq, 1.0 / 1024, ALU.mult, RMS_EPS, ALU.add)
            k.act(scr.rstd, scr.rstd, AF.Sqrt)
            k.recip(scr.rstd, scr.rstd)
            for b in range(4):
                k.stt(xg[:, b, :], xg[:, b, :], scr.rstd[:, b:b + 1], gfin, ALU.mult, ALU.mult,
                      )
        k.dma(x_out[cols, :].re("(b p) f -> p b f", p=128), xg, key=x_out.tok)


def ffn_expert(k, ws, pss, hT, hid, gs, w1, w3, w2, dff, addfn):
    nfc = dff // 128
    c0 = 0
    fc = 0
    while c0 < dff:
        nc_ = min(512, dff - c0)
        t1 = ws.load(w1[:, c0:c0 + nc_], 8, nc_)
        t3 = ws.load(w3[:, c0:c0 + nc_], 8, nc_)
        for fl in range(nc_ // 128):
            p1, p3 = pss[(2 * fl) % 6], pss[(2 * fl + 1) % 6]
            for kc in range(8):
                k.mm(p1, t1[:, kc, fl * 128:(fl + 1) * 128], hT[:, kc, :], start=(kc == 0), stop=(kc == 7))
            for kc in range(8):
                k.mm(p3, t3[:, kc, fl * 128:(fl + 1) * 128], hT[:, kc, :], start=(kc == 0), stop=(kc == 7))
            g_ = gs[fl % 2]
            import os
            if os.environ.get("NOSILU"):
                k.act(g_, p1, AF.Sigmoid)
                k.tt(g_, g_, p1, ALU.mult)
            else:
                k.act(g_, p1, AF.Silu)
            k.tt(hid[:, fc, :], g_, p3, ALU.mult)
            fc += 1
        c0 += nc_
    for half in range(2):
        k0 = 0
        while k0 < nfc:
            kn = min(8, nfc - k0)
            wt = ws.load(w2[k0 * 128:(k0 + kn) * 128, half * 512:(half + 1) * 512], kn, 512)
            for tb in range(4):
                ps = pss[tb]
                for kc in range(kn):
                    k.mm(ps, hid[:, k0 + kc, tb * 128:(tb + 1) * 128], wt[:, kc, :],
                         start=(k0 + kc == 0), stop=(k0 + kc == nfc - 1))
            k0 += kn
        for tb in range(4):
            addfn(tb, half, pss[tb])


B_CONST_SPECS = (("identd", [128, 128], BF16), ("antid", [128, 128], BF16), ("ident32d", [128, 128], F32),
                 ("tri32d", [128, 128], F32), ("padbd", [128, 1], F32), ("bfd", [128, 4], F32),
                 ("mfoxd", [128, 4, 512], BF16), ("mmlad", [128, 4, 512], BF16),
                 ("relb", [4, 513], F32), ("mchkd", [128, 8, 512], F32))


def build_B(NG, kind, final, stages=("fox", "mla", "chunk", "s5", "local")):
    T, NK = NG * 512, 2 * NG * 512
    k = KB()
    ins = {}
    for name, shape, dt in (("kfeat", [NFR, NK], BF16), ("ktok", [NK, NTC], BF16), ("kff", [NK, 4], F32),
                            ("qfT", [256, T], BF16), ("dqT", [256, T], BF16), ("qmT", [384, T], BF16),
                            ("x", [T, D], F32), ("p", [T, 256], F32)) + B_CONST_SPECS:
        ins[name] = k.dram(name, shape, dt, "ExternalInput")
    sp = {nm: k.dram("s5_" + nm, sh, F32, "ExternalInput") for nm, sh in S5_SHAPES.items()}
    W = {}
    for nm in ("g_mix", "g_ffn", "g_ple"):
        W[nm] = k.dram(nm, [128, 8], F32, "ExternalInput")
    W["b_gate"] = k.dram("b_gate", [128, 4, 8], F32, "ExternalInput")
    wgate = k.dram("w_gate", [4, D, D], F32, "ExternalInput")
    wbr = k.dram("w_branch", [4, 256, D], F32, "ExternalInput")
    W["w_gate"] = [wgate[i] for i in range(4)]
    W["w_branch"] = [wbr[i] for i in range(4)]
    W["w_o"] = k.dram("w_o", [D, D], F32, "ExternalInput")
    if kind == "ffn":
        W["w1"] = k.dram("w1", [D, 2816], F32, "ExternalInput")
        W["w3"] = k.dram("w3", [D, 2816], F32, "ExternalInput")
        W["w2"] = k.dram("w2", [2816, D], F32, "ExternalInput")
    else:
        w1 = k.dram("w1", [8, D, 1408], F32, "ExternalInput")
        w3 = k.dram("w3", [8, D, 1408], F32, "ExternalInput")
        w2 = k.dram("w2", [8, 1408, D], F32, "ExternalInput")
        W["w1"] = [w1[e] for e in range(8)]
        W["w3"] = [w3[e] for e in range(8)]
        W["w2"] = [w2[e] for e in range(8)]
        W["w_router"] = k.dram("w_router", [D, 8], F32, "ExternalInput")
        W["b_router"] = k.dram("b_router", [128, 8], F32, "ExternalInput")
    W["ple_w_gate"] = k.dram("ple_w_gate", [D, D], F32, "ExternalInput")
    W["ple_w_up"] = k.dram("ple_w_up", [256, D], F32, "ExternalInput")
    if final:
        W["g_final"] = k.dram("g_final", [128, D], F32, "ExternalInput")
    Ed = k.dram("Ed", [4, 1536], F32, "Internal")
    dbg = len(stages) < 5
    yT = [k.dram("yT%d" % i, [256, T], BF16, "ExternalOutput" if dbg else "Internal") for i in range(4)]
    xo = k.dram("xo", [T, D], F32, "ExternalOutput")
    C = common_B(k, ins)
    if "local" in stages:
        cv = {"st": [k.sb("cv_st%d" % i, [128, CV_F], F32) for i in range(2)],
              "cb": [k.sb("cv_cb%d" % i, [128, CV_F], BF16) for i in range(2)], "n": 0}
        g_ = convert_weight(k, cv, "w_gate", wgate, [4, D, D])
        b_ = convert_weight(k, cv, "w_branch", wbr, [4, 256, D])
        W["w_gate"] = [g_[i] for i in range(4)]
        W["w_branch"] = [b_[i] for i in range(4)]
        W["w_o"] = convert_weight(k, cv, "w_o", W["w_o"], [D, D])
        if kind == "ffn":
            W["w1"] = convert_weight(k, cv, "w1", W["w1"], [D, 2816])
            W["w3"] = convert_weight(k, cv, "w3", W["w3"], [D, 2816])
            W["w2"] = convert_weight(k, cv, "w2", W["w2"], [2816, D])
        else:
            c1 = convert_weight(k, cv, "w1", w1, [8, D, 1408])
            c3 = convert_weight(k, cv, "w3", w3, [8, D, 1408])
            c2 = convert_weight(k, cv, "w2", w2, [8, 1408, D])
            W["w1"] = [c1[e] for e in range(8)]
            W["w3"] = [c3[e] for e in range(8)]
            W["w2"] = [c2[e] for e in range(8)]
        W["ple_w_gate"] = convert_weight(k, cv, "ple_w_gate", W["ple_w_gate"], [D, D])
        W["ple_w_up"] = convert_weight(k, cv, "ple_w_up", W["ple_w_up"], [256, D])
    with k.phase():
        att_common(k, C, ins)
        if "fox" in stages:
            with k.phase():
                fox_phase(k, NG, C, ins["kfeat"], ins["ktok"], ins["kff"], ins["qfT"], yT[1])
        if "mla" in stages:
            with k.phase():
                mla_phase(k, NG, C, ins["kfeat"], ins["ktok"], ins["qmT"], yT[2])
        if "chunk" in stages:
            with k.phase():
                chunk_phase(k, NG, C, ins["kfeat"], ins["ktok"], ins["dqT"], ins["relb"], ins["mchkd"], Ed, yT[3])
        if "s5" in stages:
            with k.phase():
                s5_phase(k, NG, C, ins["kfeat"], sp, yT[0])
    if "local" in stages:
        with k.phase():
            local_phase(k, NG, C, kind, final, ins["x"], ins["p"], yT, W, xo)
    return k.finish()


_PROGS = {}


def _prog(key, fn):
    if key not in _PROGS:
        _PROGS[key] = fn()
    return _PROGS[key]


def _own_idx(par, NG):
    return np.concatenate([np.arange((par + 2 * i) * 512, (par + 2 * i + 1) * 512) for i in range(NG)])


def _c(a, dt=np.float32):
    return np.ascontiguousarray(a, dtype=dt)


def run_model(inp, NG, n_cores=8, stages=None):
    f = np.float32
    nb = n_cores // 2
    S = 1024 * NG
    x = _c(inp["x"])
    p = _c(inp["p"])
    pos = np.asarray(inp["positions"]).astype(np.int32)
    own = [_own_idx(par, NG) for par in range(2)]
    xs = [x[c // 2][own[c % 2]] for c in range(n_cores)]
    cores = list(range(n_cores))
    for l in range(2):
        kind = "ffn" if l % 2 == 0 else "moe"
        final = (l == 1)
        ncA = _prog(("A", NG), lambda: build_A(NG))
        insA = [host_A_inputs(xs[c], np.asarray(inp["g_mix"][l]), np.asarray(inp["w_in"][l]),
                              np.asarray(inp["mla_w_uq"][l]), np.asarray(inp["mla_w_ukv"][l]),
                              np.asarray(inp["mla_g_q"][l]), np.asarray(inp["mla_g_kv"][l]),
                              pos[c // 2][own[c % 2]]) for c in cores]
        ra = run_bass_kernel_spmd(ncA, insA, core_ids=cores).results
        ncB = _prog(("B", NG, kind, final, stages), lambda: (build_B(NG, kind, final, stages) if stages else build_B(NG, kind, final)))
        spd = host_s5_params(*[np.asarray(inp[n][l]) for n in ("ssm_a_re", "ssm_a_im", "ssm_log_dt", "ssm_b_re",
                                                                 "ssm_b_im", "ssm_c_re", "ssm_c_im", "ssm_d",
                                                                 "ssm_w_glu")])
        wd = {"g_mix": _c(np.asarray(inp["g_mix"][l]).reshape(8, 128).T),
              "g_ffn": _c(np.asarray(inp["g_ffn"][l]).reshape(8, 128).T),
              "g_ple": _c(np.asarray(inp["g_ple"][l]).reshape(8, 128).T),
              "b_gate": _c(np.asarray(inp["b_gate"][l]).reshape(4, 8, 128).transpose(2, 0, 1)),
              "w_gate": _c(inp["w_gate"][l]), "w_branch": _c(inp["w_branch"][l]), "w_o": _c(inp["w_o"][l]),
              "ple_w_gate": _c(inp["ple_w_gate"][l]), "ple_w_up": _c(inp["ple_w_up"][l]),
              "identd": IDENT, "antid": ANTI, "ident32d": IDENT32, "tri32d": TRI32,
              "bfd": _c(np.tile(np.asarray(inp["fox_b_f"][l])[None], (128, 1))),
              "mfoxd": MFOX, "mmlad": MMLA, "relb": _c(inp["chk_rel_bias"][l]), "mchkd": MCHK}
        if kind == "ffn":
            wd.update({"w1": _c(inp["ffn_w1"][l // 2]), "w3": _c(inp["ffn_w3"][l // 2]), "w2": _c(inp["ffn_w2"][l // 2])})
        else:
            j = l // 2
            wd.update({"w1": _c(inp["moe_w1"][j]), "w3": _c(inp["moe_w3"][j]), "w2": _c(inp["moe_w2"][j]),
                       "w_router": _c(inp["moe_w_router"][j]),
                       "b_router": _c(np.tile(np.asarray(inp["moe_b_router"][j])[None], (128, 1)))})
        if final:
            wd["g_final"] = _c(np.tile(np.asarray(inp["g_final"])[None], (128, 1)))
        for nm, v in spd.items():
            wd["s5_" + nm] = v
        insB = []
        for c in cores:
            b, par = c // 2, c % 2
            ra0, ra1 = ra[2 * b], ra[2 * b + 1]

            def glob(nm, axis):
                a0, a1 = np.asarray(ra0[nm]), np.asarray(ra1[nm])
                a0 = np.moveaxis(a0, axis, 0).reshape((NG, 512) + tuple(np.delete(a0.shape, axis)))
                a1 = np.moveaxis(a1, axis, 0).reshape((NG, 512) + tuple(np.delete(a1.shape, axis)))
                g_ = np.stack([a0, a1], 1).reshape((2 * NG * 512,) + a0.shape[2:])
                return np.moveaxis(g_, 0, axis)
            d = dict(wd)
            d["kfeat"] = to_slots(glob("featT", 1), par, NG, 1)
            d["ktok"] = to_slots(glob("tokM", 0), par, NG, 0)
            d["kff"] = to_slots(glob("ffM", 0), par, NG, 0)
            me = ra[c]
            d["qfT"], d["dqT"], d["qmT"] = np.asarray(me["qfT"]), np.asarray(me["dqT"]), np.asarray(me["qmT"])
            d["x"] = _c(xs[c])
            d["p"] = _c(p[l][b][own[par]])
            d["padbd"] = np.full((128, 1), NEG if par == 0 else 0.0, f)
            insB.append(d)
        rb = run_bass_kernel_spmd(ncB, insB, core_ids=cores).results
        print('layer', l, 'B done', flush=True)
        xs = [np.asarray(rb[c]["xo"]) for c in cores]
    out = np.zeros((nb, S, D), f)
    for c in cores:
        out[c // 2][own[c % 2]] = xs[c]
    return out


def kernel(**inputs):
    return run_model(inputs, 8)
```

```python
import contextlib
import math
import numpy as np
import ml_dtypes
import concourse.bass as bass
import concourse.mybir as mybir
from concourse.bass_utils import run_bass_kernel_spmd

F32 = mybir.dt.float32
BF16 = mybir.dt.bfloat16
I32 = mybir.dt.int32
AF = mybir.ActivationFunctionType
ALU = mybir.AluOpType
AX = mybir.AxisListType

ENGS = ("pe", "act", "dve", "pool", "sp")
CH = 30000
NEG = -30000.0


class Op:
    __slots__ = ("eng", "fn", "waits", "dma", "dsem", "dval", "signal", "sigval", "sigsem", "bg")

    def __init__(self, eng, fn, dma):
        self.eng = eng
        self.fn = fn
        self.dma = dma
        self.waits = []
        self.signal = False
        self.sigval = 0
        self.sigsem = 0
        self.dsem = None
        self.dval = 0
        self.bg = False


class Prog:
    def __init__(self, nc):
        self.nc = nc
        self.ops = {e: [] for e in ENGS}
        self.lastw = {}
        self.readers = {}
        self.dma_sems = {}
        self.last_dma = {}
        self.bg_mode = False
        self.selfwait = {"pe": False, "act": True, "dve": True, "pool": True, "sp": False}

    def op(self, eng, fn, reads=(), writes=(), dma_key=None):
        o = Op(eng, fn, dma_key is not None)
        o.bg = self.bg_mode
        deps = []
        for t in reads:
            w = self.lastw.get(t)
            if w is not None:
                deps.append((w, 0))
        for t in writes:
            w = self.lastw.get(t)
            if w is not None:
                deps.append((w, 1))
            for r in self.readers.get(t, ()):
                deps.append((r, 2))
        seen = set()
        for d, kind in deps:
            if d is o or id(d) in seen:
                continue
            if (not d.dma) and d.eng == eng:
                if not self.selfwait[eng]:
                    continue
            seen.add(id(d))
            o.waits.append(d)
        for t in reads:
            self.readers.setdefault(t, []).append(o)
        for t in writes:
            self.lastw[t] = o
            self.readers[t] = []
        if o.dma:
            ent = self.dma_sems.setdefault(dma_key, [0])
            ent[0] += 16
            o.dsem = dma_key
            o.dval = ent[0]
        self.ops[eng].append(o)
        if o.dma:
            self.last_dma[dma_key] = o
        return o

    def barrier(self):
        lasts = []
        for e in ENGS:
            for o in reversed(self.ops[e]):
                if not o.dma and not o.bg:
                    lasts.append(o)
                    break
        dl = [o for o in self.last_dma.values() if not o.bg]
        for e in ENGS:
            o = Op(e, lambda eng: eng.nop(), False)
            o.waits = [d for d in lasts if d.eng != e] + dl
            self.ops[e].append(o)
        self.lastw = {t: o for t, o in self.lastw.items() if o.bg}
        self.readers = {t: [r for r in rs if r.bg] for t, rs in self.readers.items()}

    def emit(self):
        nc = self.nc
        for e in ENGS:
            for o in self.ops[e]:
                for d in o.waits:
                    if not d.dma:
                        d.signal = True
        nsig = {}
        for e in ENGS:
            c = 0
            for o in self.ops[e]:
                if o.signal and not o.dma:
                    o.sigsem = c // CH
                    o.sigval = c % CH + 1
                    c += 1
            nsig[e] = c
        with contextlib.ExitStack() as st:
            esem = {e: [st.enter_context(nc.semaphore("s_%s%d" % (e, j)))
                        for j in range(nsig[e] // CH + 1)] for e in ENGS}
            dsem = {}
            for i, k in enumerate(self.dma_sems):
                dsem[k] = st.enter_context(nc.semaphore("d%d" % i))
            block = st.enter_context(nc.Block())
            hooks = {"pe": block.tensor, "act": block.scalar, "dve": block.vector,
                     "pool": block.gpsimd, "sp": block.sync}

            def make(e):
                def body(eng):
                    waited = {}
                    for o in self.ops[e]:
                        need = {}
                        for d in o.waits:
                            if d.dma:
                                s, v = dsem[d.dsem], d.dval
                            else:
                                s, v = esem[d.eng][d.sigsem], d.sigval
                            key = id(s)
                            if waited.get(key, 0) >= v:
                                continue
                            if key not in need or need[key][1] < v:
                                need[key] = (s, v)
                        for key, (s, v) in need.items():
                            eng.wait_ge(s, v)
                            waited[key] = v
                        inst = o.fn(eng)
                        if o.dma:
                            inst.then_inc(dsem[o.dsem], 16)
                        elif o.signal:
                            inst.then_inc(esem[e][o.sigsem], 1)
                    if e == "sp":
                        for k, ent in self.dma_sems.items():
                            eng.wait_ge(dsem[k], ent[0])
                return body

            for e in ENGS:
                if self.ops[e] or e == "sp":
                    hooks[e](make(e))


class V:
    __slots__ = ("ap", "tok")

    def __init__(self, ap, tok):
        self.ap = ap
        self.tok = tok

    def __getitem__(self, idx):
        return V(self.ap[idx], self.tok)

    def re(self, s, **kw):
        return V(self.ap.rearrange(s, **kw), self.tok)

    def sub(self, tok):
        return V(self.ap, tok)


def _ap(x):
    return x.ap if isinstance(x, V) else x


def _toks(*xs):
    return [x.tok for x in xs if isinstance(x, V)]


class KB:
    def __init__(self):
        self.nc = bass.Bass("TRN2", target_bir_lowering=False)
        self.P = Prog(self.nc)
        self.st = contextlib.ExitStack()
        self.nps = 0
        self.dq = 0

    @contextlib.contextmanager
    def phase(self):
        old = self.st
        self.st = contextlib.ExitStack()
        try:
            yield
        finally:
            self.P.barrier()
            self.st.close()
            self.st = old

    def dram(self, name, shape, dt, kind):
        return V(self.nc.dram_tensor(name, list(shape), dt, kind=kind).ap(), "D:" + name)

    def sb(self, name, shape, dt):
        t = self.st.enter_context(self.nc.sbuf_tensor(name, list(shape), dt))
        return V(t[:], "S:" + name)

    def ps(self, name, shape, dt):
        t = self.st.enter_context(self.nc.psum_tensor(name, list(shape), dt))
        return V(t[:], "P:" + name)

    def dma(self, out, in_, q="sp", key=None, **kw):
        o, i = _ap(out), _ap(in_)
        self.P.op(q, lambda e: e.dma_start(out=o, in_=i, **kw), reads=_toks(in_), writes=_toks(out),
                  dma_key=key or out.tok)

    def mm(self, out, lhsT, rhs, start=True, stop=True, extra_reads=()):
        o, l, r = _ap(out), _ap(lhsT), _ap(rhs)
        self.P.op("pe", lambda e: e.matmul(o, l, r, start=start, stop=stop),
                  reads=_toks(lhsT, rhs) + list(extra_reads), writes=_toks(out))

    def tr(self, out, in_, ident):
        o, i, d = _ap(out), _ap(in_), _ap(ident)
        self.P.op("pe", lambda e: e.transpose(o, i, d), reads=_toks(in_, ident), writes=_toks(out))

    def act(self, out, in_, func, bias=None, scale=1.0, accum_out=None, eng="act"):
        o, i = _ap(out), _ap(in_)
        b = _ap(bias)
        s = _ap(scale)
        a = _ap(accum_out)
        kw = {}
        if b is not None:
            kw["bias"] = b
        if a is not None:
            kw["accum_out"] = a
        self.P.op("act", lambda e: e.activation(out=o, in_=i, func=func, scale=s, **kw),
                  reads=_toks(in_, bias, scale), writes=_toks(out, accum_out))

    def tt(self, out, in0, in1, op, eng="dve"):
        o, a, b = _ap(out), _ap(in0), _ap(in1)
        self.P.op(eng, lambda e: e.tensor_tensor(out=o, in0=a, in1=b, op=op),
                  reads=_toks(in0, in1), writes=_toks(out))

    def ts(self, out, in0, s1, op0, s2=None, op1=None, eng="dve", accum_out=None):
        o, a = _ap(out), _ap(in0)
        x1, x2 = _ap(s1), _ap(s2)
        acc = _ap(accum_out)
        kw = {}
        if op1 is not None:
            kw["op1"] = op1
        if acc is not None:
            kw["accum_out"] = acc
        self.P.op(eng, lambda e: e.tensor_scalar(out=o, in0=a, scalar1=x1, scalar2=x2, op0=op0, **kw),
                  reads=_toks(in0, s1, s2), writes=_toks(out, accum_out))

    def stt(self, out, in0, scalar, in1, op0, op1, eng="dve"):
        o, a, s, b = _ap(out), _ap(in0), _ap(scalar), _ap(in1)
        self.P.op(eng, lambda e: e.scalar_tensor_tensor(out=o, in0=a, scalar=s, in1=b, op0=op0, op1=op1),
                  reads=_toks(in0, scalar, in1), writes=_toks(out))

    def copy(self, out, in_, eng="dve"):
        o, i = _ap(out), _ap(in_)
        if eng == "act":
            self.P.op("act", lambda e: e.copy(out=o, in_=i), reads=_toks(in_), writes=_toks(out))
        else:
            self.P.op(eng, lambda e: e.tensor_copy(out=o, in_=i), reads=_toks(in_), writes=_toks(out))

    def recip(self, out, in_):
        o, i = _ap(out), _ap(in_)
        self.P.op("dve", lambda e: e.reciprocal(out=o, in_=i), reads=_toks(in_), writes=_toks(out))

    def memset(self, out, val, eng="pool"):
        o = _ap(out)
        self.P.op(eng, lambda e: e.memset(o, val), writes=_toks(out))

    def scan(self, out, d0, d1, init, op0=ALU.mult, op1=ALU.add):
        o, a, b, i = _ap(out), _ap(d0), _ap(d1), _ap(init)
        self.P.op("dve", lambda e: e.tensor_tensor_scan(out=o, data0=a, data1=b, initial=i, op0=op0, op1=op1),
                  reads=_toks(d0, d1, init), writes=_toks(out))

    def reduce(self, out, in_, op, axis=AX.X):
        o, i = _ap(out), _ap(in_)
        self.P.op("dve", lambda e: e.tensor_reduce(out=o, in_=i, axis=axis, op=op),
                  reads=_toks(in_), writes=_toks(out))

    def finish(self):
        self.P.emit()
        self.st.close()
        return self.nc


D = 1024
RMS_EPS = 1e-6
C_U, C_FQ, C_FK, C_FV, C_FF, C_CQ, C_CKV, C_KPE, C_DQ, C_DK, C_DV = (
    0, 256, 512, 768, 1024, 1028, 1220, 1348, 1380, 1636, 1892)
D_IN = 2148
WA_COLS = np.concatenate([np.arange(C_U, C_U + 256), np.arange(C_FK, C_FK + 256), np.arange(C_DK, C_DK + 256),
                          np.arange(C_CKV, C_CKV + 128), np.arange(C_KPE, C_KPE + 32),
                          np.arange(C_FV, C_FV + 256), np.arange(C_DV, C_DV + 256), np.arange(C_FF, C_FF + 4)])
NWA = 1444
R_U, R_FK, R_DK, R_CKV, R_KPE, R_KSW, NFEAT = 0, 256, 512, 768, 896, 928, 960


class Scr:
    pass


def alloc_norm_scratch(k, tag=""):
    s = Scr()
    s.junk = k.sb("njunk" + tag, [128, 1024], BF16)
    s.ssq = k.sb("nssq" + tag, [128, 4], F32)
    s.rstd = k.sb("nrstd" + tag, [128, 4], F32)
    s.xn = k.sb("nxn" + tag, [128, 4, 1024], BF16)
    return s


def norm_hT(k, xg, gt, hT, s, pst, ident, flip=0, raw_out=None):
    for b in range(4):
        k.act(s.junk.ap, xg[:, b, :], AF.Square, accum_out=s.ssq[:, b:b + 1])
    k.ts(s.rstd, s.ssq, 1.0 / 1024, ALU.mult, RMS_EPS, ALU.add)
    k.act(s.rstd, s.rstd, AF.Sqrt)
    k.recip(s.rstd, s.rstd)
    for b in range(4):
        k.ts(s.xn[:, b, :], xg[:, b, :], s.rstd[:, b:b + 1], ALU.mult, eng=("dve" if b % 2 == 0 else "pool"))
    for c in range(8):
        pt = pst[c % len(pst)]
        for b in range(4):
            k.tr(pt[:, b * 128:(b + 1) * 128], s.xn[:, b, c * 128:(c + 1) * 128], ident)
        if raw_out is not None:
            k.copy(raw_out[:, c, :], pt[:, 0:512], eng=("act" if (c + flip) % 2 == 0 else "dve"))
            k.ts(hT[:, c, :], raw_out[:, c, :], gt[:, c:c + 1], ALU.mult, eng="pool")
        elif (c + flip) % 2 == 0:
            k.act(hT[:, c, :], pt[:, 0:512], AF.Copy, scale=gt[:, c:c + 1])
        else:
            k.ts(hT[:, c, :], pt[:, 0:512], gt[:, c:c + 1], ALU.mult)


S96 = 96 ** -0.5
TWO_PI = 2.0 * math.pi
RC1 = 6.28125
RC2 = float(np.float32(TWO_PI - 6.28125))
RC3 = float(TWO_PI - 6.28125 - np.float64(np.float32(TWO_PI - 6.28125)))
PI_LO = 3.1415925


def range_reduce(k, out, in_, tmp_i, tmp_f):
    k.ts(tmp_f, in_, 1.0 / TWO_PI, ALU.mult, 0.5, ALU.add)
    k.copy(tmp_i, tmp_f)
    k.copy(tmp_f, tmp_i)
    k.stt(out, tmp_f, -RC1, in_, ALU.mult, ALU.add)
    k.stt(out, tmp_f, -RC2, out, ALU.mult, ALU.add)
    k.stt(out, tmp_f, -RC3, out, ALU.mult, ALU.add)
    k.ts(tmp_f, out, -math.pi, ALU.is_lt)
    k.stt(out, tmp_f, TWO_PI, out, ALU.mult, ALU.add)
    k.ts(tmp_f, out, math.pi, ALU.is_gt)
    k.stt(out, tmp_f, -TWO_PI, out, ALU.mult, ALU.add)
    k.ts(out, out, -PI_LO, ALU.max, PI_LO, ALU.min)


FR_U, FR_FK, FR_DK, FR_MK, NFR = 0, 256, 512, 768, 1152
TC_FV, TC_DV, TC_MV, NTC = 0, 256, 512, 768
NWB = 2180


def build_A(NG):
    T = NG * 512
    k = KB()
    x = k.dram("x", [T, D], F32, "ExternalInput")
    g = k.dram("g", [128, 8], F32, "ExternalInput")
    win = k.dram("win", [D, D_IN], F32, "ExternalInput")
    wuq = k.dram("wuq", [192, 384], F32, "ExternalInput")
    wukv = k.dram("wukv", [128, 512], F32, "ExternalInput")
    gq = k.dram("gq", [128, 2], F32, "ExternalInput")
    gkv = k.dram("gkv", [128, 1], F32, "ExternalInput")
    pos = k.dram("pos", [1, T], I32, "ExternalInput")
    cst = k.dram("cst", [128, 2], F32, "ExternalInput")
    idn = k.dram("identd", [128, 128], BF16, "ExternalInput")
    featT = k.dram("featT", [NFR, T], BF16, "ExternalOutput")
    tokM = k.dram("tokM", [T, NTC], BF16, "ExternalOutput")
    ffM = k.dram("ffM", [T, 4], F32, "ExternalOutput")
    qfT = k.dram("qfT", [256, T], BF16, "ExternalOutput")
    dqT = k.dram("dqT", [256, T], BF16, "ExternalOutput")
    qmT = k.dram("qmT", [384, T], BF16, "ExternalOutput")
    build_A_body(k, NG, x, g, win, wuq, wukv, gq, gkv, pos, cst, idn, featT, tokM, ffM, qfT, dqT, qmT)
    return k.finish()


def build_A_body(k, NG, x, g, win, wuq, wukv, gq, gkv, pos, cst, idn, featT, tokM, ffM, qfT, dqT, qmT):
    ident = k.sb("ident", [128, 128], BF16)
    ones = k.sb("ones", [128, 128], BF16)
    gt = k.sb("gt", [128, 8], F32)
    gqt = k.sb("gqt", [128, 2], F32)
    gkvt = k.sb("gkvt", [128, 1], F32)
    cstt = k.sb("cstt", [128, 2], F32)
    wb = k.sb("wb", [128, 8, NWB], BF16)
    k.dma(ident, idn)
    k.dma(gt, g)
    k.dma(gqt, gq)
    k.dma(gkvt, gkv)
    k.dma(cstt, cst)
    k.memset(ones, 1.0)
    xgs = [k.sb("xg%d" % i, [128, 4, 1024], F32) for i in range(2)]
    k.dma(xgs[0], x[0:512, :].re("(b p) f -> p b f", p=128))
    wst = [k.sb("wst%d" % i, [128, D_IN], F32) for i in range(2)]
    wbt = ["S:wb%d" % kc for kc in range(8)]
    for kc in range(8):
        src = wst[kc % 2]
        k.dma(src, win[kc * 128:(kc + 1) * 128, :], q="pool" if kc % 2 else "sp")
        dst = wb[:, kc, :].sub(wbt[kc])
        e = ("pool", "dve")[kc % 2]
        k.copy(dst[:, 0:D_IN], src, eng=e)
        k.ts(dst[:, D_IN:D_IN + 16], src[:, C_KPE + 16:C_KPE + 32], -1.0, ALU.mult, eng=e)
        k.copy(dst[:, D_IN + 16:D_IN + 32], src[:, C_KPE:C_KPE + 16], eng=e)
    wuq_s = k.sb("wuq_s", [128, 2, 384], F32)
    wukv_s = k.sb("wukv_s", [128, 512], F32)
    k.dma(wuq_s[:, 0, :], wuq[0:128, :])
    k.dma(wuq_s[0:64, 1, :], wuq[128:192, :])
    k.dma(wukv_s, wukv)
    wuq_b = k.sb("wuq_b", [128, 2, 384], BF16)
    wuqsw_b = k.sb("wuqsw_b", [128, 2, 384], BF16)
    wkn_b = k.sb("wkn_b", [128, 4, 64], BF16)
    wv_b = k.sb("wv_b", [128, 4, 64], BF16)
    k.copy(wuq_b[:, 0, :], wuq_s[:, 0, :])
    k.copy(wuq_b[0:64, 1, :], wuq_s[0:64, 1, :])
    k.copy(wuqsw_b[:, 0, :], wuq_s[:, 0, :])
    k.copy(wuqsw_b[0:64, 1, :], wuq_s[0:64, 1, :])
    for c, np_ in ((0, 128), (1, 64)):
        for h in range(4):
            b0 = h * 96 + 64
            k.ts(wuqsw_b[0:np_, c, b0:b0 + 16], wuq_s[0:np_, c, b0 + 16:b0 + 32], -1.0, ALU.mult)
            k.copy(wuqsw_b[0:np_, c, b0 + 16:b0 + 32], wuq_s[0:np_, c, b0:b0 + 16])
    wk4 = wukv_s.re("k (h two d) -> k h two d", h=4, two=2)
    k.copy(wkn_b, wk4[:, :, 0, :])
    k.copy(wv_b, wk4[:, :, 1, :])

    hTs = [k.sb("hT%d" % i, [128, 8, 512], BF16) for i in range(2)]
    scr = alloc_norm_scratch(k)
    pst = [k.ps("pst%d" % i, [128, 1024], BF16) for i in range(2)]
    pss = [k.ps("ps%d" % i, [128, 512], F32) for i in range(6)]
    fst = [k.sb("fst%d" % i, [128, 6, 512], BF16) for i in range(2)]
    qst = [k.sb("qst%d" % i, [128, 4, 512], BF16) for i in range(2)]
    tst = [k.sb("tst%d" % i, [128, 4, NTC], BF16) for i in range(2)]
    ffs = [k.sb("ffs%d" % i, [128, 4, 4], F32) for i in range(2)]
    mkn = [k.sb("mkn%d" % i, [64, 4, 512], BF16) for i in range(2)]
    rk = [k.sb("rk%d" % i, [96, 512], BF16) for i in range(2)]
    qms = [k.sb("qms%d" % i, [96, 4, 512], BF16) for i in range(2)]
    posi = k.sb("posi", [96, 512], I32)
    posf = k.sb("posf", [96, 512], F32)
    ang = k.sb("ang", [96, 512], F32)
    ang2 = k.sb("ang2", [96, 512], F32)
    rtf = k.sb("rtf", [96, 512], F32)
    rti = k.sb("rti", [96, 512], I32)
    Ct = k.sb("Ct", [96, 512], F32)
    St = k.sb("St", [96, 512], F32)
    kpe_s = k.sb("kpe_s", [96, 512], F32)
    ksw_s = k.sb("ksw_s", [96, 512], F32)
    rt1 = k.sb("rt1", [96, 512], F32)
    rt2 = k.sb("rt2", [96, 512], F32)
    ckv_s = k.sb("ckv_s", [128, 512], F32)
    cq_s = k.sb("cq_s", [128, 2, 512], F32)
    sq = k.sb("sq", [128, 2, 512], BF16)
    rstd = k.sb("rstd", [128, 512], F32)
    ckvn = k.sb("ckvn", [128, 512], BF16)
    cqn = k.sb("cqn", [128, 2, 512], BF16)
    R = slice(64, 96)
    pc = [0]

    def nps():
        p_ = pss[pc[0] % 6]
        pc[0] += 1
        return p_

    def proj(c0, m, hT):
        ps = nps()
        for kc in range(8):
            k.mm(ps[0:m, :], wb[:, kc, c0:c0 + m].sub(wbt[kc]), hT[:, kc, :], start=(kc == 0), stop=(kc == 7))
        return ps

    for gi in range(NG):
        xg = xgs[gi % 2]
        if gi + 1 < NG:
            k.dma(xgs[(gi + 1) % 2], x[(gi + 1) * 512:(gi + 2) * 512, :].re("(b p) f -> p b f", p=128))
        hT = hTs[gi % 2]
        norm_hT(k, xg, gt, hT, scr, pst, ident)
        cols = slice(gi * 512, (gi + 1) * 512)
        fs, qs, tsb, ff = fst[gi % 2], qst[gi % 2], tst[gi % 2], ffs[gi % 2]
        k.dma(posi[R, :], pos[0:1, cols].re("o t -> (o) t").ap.partition_broadcast(32)
              if False else V(pos.ap[0:1, cols].partition_broadcast(32), pos.tok))
        k.copy(posf[R, :], posi[R, :])
        k.ts(ang[R, :], posf[R, :], cstt[R, 0:1], ALU.mult)
        k.ts(ang2[R, :], ang[R, :], math.pi / 2, ALU.add)
        range_reduce(k, ang[R, :], ang[R, :], rti[R, :], rtf[R, :])
        range_reduce(k, ang2[R, :], ang2[R, :], rti[R, :], rtf[R, :])
        k.act(St[R, :], ang[R, :], AF.Sin)
        k.act(Ct[R, :], ang2[R, :], AF.Sin)
        for ci, c0 in enumerate((C_U, C_U + 128, C_FK, C_FK + 128, C_DK, C_DK + 128)):
            ps = proj(c0, 128, hT)
            k.copy(fs[:, ci, :], ps, eng=("act" if ci % 2 else "dve"))
        k.dma(featT[0:768, cols].re("(c p) t -> p c t", p=128), fs, key="featT")
        for ci, c0 in enumerate((C_FQ, C_FQ + 128, C_DQ, C_DQ + 128)):
            ps = proj(c0, 128, hT)
            if ci % 2:
                k.act(qs[:, ci, :], ps, AF.Copy, scale=0.125)
            else:
                k.ts(qs[:, ci, :], ps, 0.125, ALU.mult)
        k.dma(qfT[:, cols].re("(c p) t -> p c t", p=128), qs[:, 0:2, :], key="qfT")
        k.dma(dqT[:, cols].re("(c p) t -> p c t", p=128), qs[:, 2:4, :], key="dqT")
        for tb in range(4):
            for (c0, n, o0) in ((C_FV, 256, TC_FV), (C_DV, 256, TC_DV)):
                ps = nps()
                for kc in range(8):
                    k.mm(ps[:, 0:n], hT[:, kc, tb * 128:(tb + 1) * 128], wb[:, kc, c0:c0 + n].sub(wbt[kc]),
                         start=(kc == 0), stop=(kc == 7))
                k.copy(tsb[:, tb, o0:o0 + n], ps[:, 0:n], eng=("act" if tb % 2 else "dve"))
            ps = nps()
            for kc in range(8):
                k.mm(ps[:, 0:4], hT[:, kc, tb * 128:(tb + 1) * 128], wb[:, kc, C_FF:C_FF + 4].sub(wbt[kc]),
                     start=(kc == 0), stop=(kc == 7))
            k.copy(ff[:, tb, :], ps[:, 0:4])
        k.dma(ffM[cols, :].re("(b p) c -> p b c", p=128), ff, key="ffM")
        ps = proj(C_CKV, 128, hT)
        k.copy(ckv_s, ps, eng="act")
        k.act(sq[:, 0, :], ckv_s, AF.Square)
        ps = nps()
        k.mm(ps, ones, sq[:, 0, :])
        k.ts(rstd, ps, 1.0 / 128, ALU.mult, RMS_EPS, ALU.add)
        k.act(rstd, rstd, AF.Sqrt)
        k.recip(rstd, rstd)
        k.stt(ckvn, ckv_s, gkvt[:, 0:1], rstd, ALU.mult, ALU.mult)
        mk = mkn[gi % 2]
        for h in range(4):
            ps = nps()
            k.mm(ps[0:64, :], wkn_b[:, h, :], ckvn)
            k.copy(mk[:, h, :], ps[0:64, :], eng=("act" if h % 2 else "dve"))
        for tb in range(4):
            ps = nps()
            k.mm(ps[:, 0:256], ckvn[:, tb * 128:(tb + 1) * 128], wv_b.re("k h d -> k (h d)"))
            k.copy(tsb[:, tb, TC_MV:TC_MV + 256], ps[:, 0:256], eng=("act" if tb % 2 else "dve"))
        k.dma(tokM[cols, :].re("(b p) c -> p b c", p=128), tsb, key="tokM")
        psA = proj(C_KPE - 64, 96, hT)
        psB = proj(D_IN - 64, 96, hT)
        k.tt(rt1[R, :], psA[R, :], Ct[R, :], ALU.mult)
        k.tt(rt2[R, :], psB[R, :], St[R, :], ALU.mult)
        rkk = rk[gi % 2]
        k.tt(rkk[R, :], rt1[R, :], rt2[R, :], ALU.add)
        for h in range(4):
            r0 = FR_MK + h * 96
            k.dma(featT[r0:r0 + 64, cols], mk[:, h, :], key="featT")
            k.dma(featT[r0 + 64:r0 + 96, cols], rkk[R, :], key="featT")
        ps0 = proj(C_CQ, 128, hT)
        ps1 = proj(C_CQ + 128, 64, hT)
        k.copy(cq_s[:, 0, :], ps0, eng="act")
        k.copy(cq_s[0:64, 1, :], ps1[0:64, :], eng="act")
        k.act(sq[:, 0, :], cq_s[:, 0, :], AF.Square)
        k.act(sq[0:64, 1, :], cq_s[0:64, 1, :], AF.Square)
        ps = nps()
        k.mm(ps, ones, sq[:, 0, :], start=True, stop=False)
        k.mm(ps, ones[0:64, :], sq[0:64, 1, :], start=False, stop=True)
        k.ts(rstd, ps, 1.0 / 192, ALU.mult, RMS_EPS, ALU.add)
        k.act(rstd, rstd, AF.Sqrt)
        k.recip(rstd, rstd)
        k.stt(cqn[:, 0, :], cq_s[:, 0, :], gqt[:, 0:1], rstd, ALU.mult, ALU.mult)
        k.stt(cqn[0:64, 1, :], cq_s[0:64, 1, :], gqt[0:64, 1:2], rstd[0:64, :], ALU.mult, ALU.mult)
        qm = qms[gi % 2]
        for h in range(4):
            pa = nps()
            k.mm(pa[0:96, :], wuq_b[:, 0, h * 96:(h + 1) * 96], cqn[:, 0, :], start=True, stop=False)
            k.mm(pa[0:96, :], wuq_b[0:64, 1, h * 96:(h + 1) * 96], cqn[0:64, 1, :], start=False, stop=True)
            pb = nps()
            k.mm(pb[0:96, :], wuqsw_b[:, 0, h * 96:(h + 1) * 96], cqn[:, 0, :], start=True, stop=False)
            k.mm(pb[0:96, :], wuqsw_b[0:64, 1, h * 96:(h + 1) * 96], cqn[0:64, 1, :], start=False, stop=True)
            k.ts(qm[0:64, h, :], pa[0:64, :], S96, ALU.mult)
            k.stt(rt1[R, :], pa[R, :], S96, Ct[R, :], ALU.mult, ALU.mult)
            k.stt(rt2[R, :], pb[R, :], S96, St[R, :], ALU.mult, ALU.mult)
            k.tt(qm[R, h, :], rt1[R, :], rt2[R, :], ALU.add)
        k.dma(qmT[:, cols].re("(h r) t -> r h t", r=96), qm, key="qmT")


INV_FREQ = (10000.0 ** (-np.arange(0, 32, 2, dtype=np.float32) / 32)).astype(np.float32)
IDENT = np.eye(128, dtype=np.float32).astype(ml_dtypes.bfloat16)


def host_A_inputs(x_own, g, w_in, wuq, wukv, gq, gkv, pos_own):
    cst = np.zeros((128, 2), np.float32)
    cst[64:80, 0] = INV_FREQ
    cst[80:96, 0] = INV_FREQ
    gqp = np.zeros((256,), np.float32)
    gqp[:192] = gq
    return {"x": np.ascontiguousarray(x_own, dtype=np.float32),
            "g": np.ascontiguousarray(g.reshape(8, 128).T),
            "win": np.ascontiguousarray(w_in), "wuq": np.ascontiguousarray(wuq),
            "wukv": np.ascontiguousarray(wukv),
            "gq": np.ascontiguousarray(gqp.reshape(2, 128).T), "gkv": np.ascontiguousarray(gkv.reshape(128, 1)),
            "pos": np.ascontiguousarray(pos_own.reshape(1, -1).astype(np.int32)),
            "cst": cst, "identd": IDENT}


def attn_head(k, nkb, kt_fn, qt, v_fn, bias_fn, mask_fn, pss, pts, ones_b, yrow_out, rec_t, rows):
    O, S = pss[3], pss[4]
    prev = None
    for idx in range(nkb):
        sc = pss[idx % 3]
        m = mask_fn(idx)
        k.mm(sc, kt_fn(idx), qt, start=True, stop=(m is None))
        if m is not None:
            k.mm(sc, m[0], m[1], start=False, stop=True)
        pt = pts[idx % len(pts)]
        b = bias_fn(idx)
        if b is None:
            k.act(pt, sc, AF.Exp)
        else:
            k.act(pt, sc, AF.Exp, bias=b)
        if prev is not None:
            pi, pp = prev
            k.mm(O, v_fn(pi), pp, start=(pi == 0), stop=False)
            k.mm(S, ones_b, pp, start=(pi == 0), stop=False)
        prev = (idx, pt)
    pi, pp = prev
    k.mm(O, v_fn(pi), pp, start=(pi == 0), stop=True)
    k.mm(S, ones_b, pp, start=(pi == 0), stop=True)
    k.recip(rec_t[rows, :], S[rows, :])
    k.tt(yrow_out, O[rows, :], rec_t[rows, :], ALU.mult)


def fox_phase(k, NG, C, kfeat, ktok, kff, qfT, yT_out):
    NS, NK = 2 * NG, 2 * NG * 512
    NKB = NK // 128
    T = NG * 512
    KT = [k.sb("fKT%d" % h, [67, NK], BF16) for h in range(4)]
    QT = [k.sb("fQT%d" % h, [67, T], BF16) for h in range(4)]
    Vt = k.sb("fV", [128, NKB, 256], BF16)
    for h in range(4):
        k.dma(KT[h][0:64, :], kfeat[FR_FK + h * 64:FR_FK + (h + 1) * 64, :])
        k.memset(KT[h][64:67, :], 1.0, eng="dve")
        k.dma(QT[h][0:64, :], qfT[h * 64:(h + 1) * 64, :])
    k.dma(Vt, ktok[:, TC_FV:TC_FV + 256].re("(kb p) c -> p kb c", p=128))
    ff = k.sb("f_ff", [128, NKB, 4], F32)
    t1 = k.sb("f_t1", [128, NKB, 4], F32)
    t2 = k.sb("f_t2", [128, NKB, 4], F32)
    lf = k.sb("f_lf", [128, NKB, 4], F32)
    cum = k.sb("f_cum", [128, NKB, 4], F32)
    nb = k.sb("f_nb", [128, NKB, 4], F32)
    k.dma(ff, kff.re("(kb p) h -> p kb h", p=128))
    for h in range(4):
        k.ts(ff[:, :, h], ff[:, :, h], C.bft[:, h:h + 1], ALU.add)
    k.ts(t1, ff, -1.0, ALU.mult)
    k.tt(t1, t1, ff, ALU.max)
    k.act(t1, t1, AF.Exp, scale=-1.0)
    k.act(t1, t1, AF.Ln, bias=C.one_col[:, 0:1])
    k.ts(t2, ff, 0.0, ALU.min)
    k.tt(lf, t2, t1, ALU.subtract)
    lf2 = lf.re("p kb h -> p (kb h)")
    pw, ptot = C.pss[0], C.pss[1]
    k.mm(pw[:, 0:NKB * 4], C.tri32, lf2)
    k.mm(ptot[:, 0:NKB * 4], C.ones32, lf2)
    k.copy(t1.re("p kb h -> p (kb h)"), ptot[:, 0:NKB * 4])
    for h in range(4):
        k.scan(t2[:, :, h], C.onesrow[:, 0:NKB], t1[:, :, h], 0.0)
    k.tt(t2, t2, t1, ALU.subtract)
    k.tt(cum.re("p kb h -> p (kb h)"), pw[:, 0:NKB * 4], t2.re("p kb h -> p (kb h)"), ALU.add)
    k.ts(nb, cum, -1.0, ALU.mult)
    for h in range(4):
        k.ts(nb[:, 0:4, h], nb[:, 0:4, h], C.padb[:, 0:1], ALU.add)
    cq = k.sb("f_cq", [4, 512], F32)
    cqh = k.sb("f_cqh", [4, 512], BF16)
    cqm = k.sb("f_cqm", [4, 512], BF16)
    cql = k.sb("f_cql", [4, 512], BF16)
    cqf = k.sb("f_cqf", [4, 512], F32)
    for gi in range(NG):
        pq = C.pss[2 + gi % 2]
        for j in range(4):
            kb = (2 * gi + 1) * 4 + j
            k.tr(pq[0:4, j * 128:(j + 1) * 128], cum[:, kb, :], C.ident32)
        k.copy(cq, pq[0:4, :])
        k.copy(cqh, cq)
        k.copy(cqf, cqh)
        k.tt(cq, cq, cqf, ALU.subtract)
        k.copy(cqm, cq)
        k.copy(cqf, cqm)
        k.tt(cq, cq, cqf, ALU.subtract)
        k.copy(cql, cq)
        cols = slice(gi * 512, (gi + 1) * 512)
        for h in range(4):
            k.dma(QT[h][64:65, cols], cqh[h:h + 1, :], key="fq%d" % h)
            k.dma(QT[h][65:66, cols], cqm[h:h + 1, :], key="fq%d" % h)
            k.dma(QT[h][66:67, cols], cql[h:h + 1, :], key="fq%d" % h)
    for gi in range(NG):
        nkb = (2 * gi + 2) * 4
        cols = slice(gi * 512, (gi + 1) * 512)
        yst = C.yst[gi % 2]
        for h in range(4):
            rows = slice((h % 2) * 64, (h % 2) * 64 + 64)
            pair = h // 2
            attn_head(k, nkb,
                      lambda kb, h=h: KT[h][:, kb * 128:(kb + 1) * 128],
                      QT[h][:, cols],
                      lambda kb, pair=pair: Vt[:, kb, pair * 128:(pair + 1) * 128],
                      lambda kb, h=h: nb[:, kb, h:h + 1],
                      lambda kb, nkb=nkb: ((C.identb, C.mfox[:, kb - (nkb - 4), :]) if kb >= nkb - 4 else None),
                      C.pss, C.pts, C.onesb, yst[rows, pair, :], C.rec, rows)
        k.dma(yT_out[:, cols].re("(c p) t -> p c t", p=128), yst, key=yT_out.tok)


def mla_phase(k, NG, C, kfeat, ktok, qmT, yT_out):
    NS, NK = 2 * NG, 2 * NG * 512
    NKB = NK // 128
    T = NG * 512
    KT = [k.sb("mKT%d" % h, [96, NK], BF16) for h in range(4)]
    QT = [k.sb("mQT%d" % h, [96, T], BF16) for h in range(4)]
    Vt = k.sb("mV", [128, NKB, 256], BF16)
    for h in range(4):
        k.dma(KT[h], kfeat[FR_MK + h * 96:FR_MK + (h + 1) * 96, :])
        k.dma(QT[h], qmT[h * 96:(h + 1) * 96, :])
    k.dma(Vt, ktok[:, TC_MV:TC_MV + 256].re("(kb p) c -> p kb c", p=128))
    for gi in range(NG):
        nkb = (2 * gi + 2) * 4
        cols = slice(gi * 512, (gi + 1) * 512)
        yst = C.yst[gi % 2]
        for h in range(4):
            rows = slice((h % 2) * 64, (h % 2) * 64 + 64)
            pair = h // 2
            attn_head(k, nkb,
                      lambda kb, h=h: KT[h][:, kb * 128:(kb + 1) * 128],
                      QT[h][:, cols],
                      lambda kb, pair=pair: Vt[:, kb, pair * 128:(pair + 1) * 128],
                      lambda kb: (C.padb[:, 0:1] if kb < 4 else None),
                      lambda kb, nkb=nkb: ((C.identb, C.mmla[:, kb - (nkb - 4), :]) if kb >= nkb - 4 else None),
                      C.pss, C.pts, C.onesb, yst[rows, pair, :], C.rec, rows)
        k.dma(yT_out[:, cols].re("(c p) t -> p c t", p=128), yst, key=yT_out.tok)


class Cm:
    pass


def common_B(k, cons):
    C = Cm()
    C.identb = k.sb("identb", [128, 128], BF16)
    C.antib = k.sb("antib", [128, 128], BF16)
    C.onesb = k.sb("onesb", [128, 128], BF16)
    C.ident32 = k.sb("ident32", [128, 128], F32)
    C.tri32 = k.sb("tri32", [128, 128], F32)
    C.ones32 = k.sb("ones32", [128, 128], F32)
    C.onesrow = k.sb("onesrow", [128, 512], F32)
    C.one_col = k.sb("one_col", [128, 1], F32)
    C.padb = k.sb("padb", [128, 1], F32)
    C.bft = k.sb("bft", [128, 4], F32)
    k.dma(C.identb, cons["identd"])
    k.dma(C.antib, cons["antid"])
    k.dma(C.ident32, cons["ident32d"])
    k.dma(C.tri32, cons["tri32d"])
    k.dma(C.padb, cons["padbd"])
    k.dma(C.bft, cons["bfd"])
    k.memset(C.onesb, 1.0)
    k.memset(C.ones32, 1.0)
    k.memset(C.onesrow, 1.0)
    k.memset(C.one_col, 1.0)
    C.pss = [k.ps("ps%d" % i, [128, 512], F32) for i in range(6)]
    C.pst = [k.ps("pst%d" % i, [128, 1024], BF16) for i in range(2)]
    return C


def att_common(k, C, cons):
    C.mfox = k.sb("mfox", [128, 4, 512], BF16)
    C.mmla = k.sb("mmla", [128, 4, 512], BF16)
    k.dma(C.mfox, cons["mfoxd"])
    k.dma(C.mmla, cons["mmlad"])
    C.pts = [k.sb("pT%d" % i, [128, 512], BF16) for i in range(4)]
    C.yst = [k.sb("yst%d" % i, [128, 2, 512], BF16) for i in range(2)]
    C.rec = k.sb("rec", [128, 512], F32)


def host_masks():
    kk = np.arange(128)[:, None, None]
    j = np.arange(4)[None, :, None]
    q = np.arange(512)[None, None, :]
    kidx = j * 128 + kk
    mfox = np.where(kidx <= q, 0.0, NEG).astype(np.float32)
    mmla = np.where(kidx // 64 <= q // 64, 0.0, NEG).astype(np.float32)
    return mfox.astype(ml_dtypes.bfloat16), mmla.astype(ml_dtypes.bfloat16)


ANTI = np.ascontiguousarray(np.eye(128, dtype=np.float32)[::-1]).astype(ml_dtypes.bfloat16)
IDENT32 = np.eye(128, dtype=np.float32)
TRI32 = np.triu(np.ones((128, 128), np.float32))
MFOX, MMLA = host_masks()


def slot_tiles(par, NG):
    return [-1] + list(range(2 * NG - 1)) if par == 0 else list(range(2 * NG))


def to_slots(arr_tiles, par, NG, axis):
    a = np.moveaxis(arr_tiles, axis, 0)
    a = a.reshape((2 * NG, 512) + a.shape[1:])
    out = np.zeros_like(a)
    for s, t in enumerate(slot_tiles(par, NG)):
        if t >= 0:
            out[s] = a[t]
    out = out.reshape((2 * NG * 512,) + a.shape[2:])
    return np.ascontiguousarray(np.moveaxis(out, 0, axis))


def build_B_attn_test(NG):
    T, NK = NG * 512, 2 * NG * 512
    k = KB()
    ins = {}
    for name, shape, dt in (("kfeat", [NFR, NK], BF16), ("ktok", [NK, NTC], BF16), ("kff", [NK, 4], F32),
                            ("qfT", [256, T], BF16), ("qmT", [384, T], BF16),
                            ("identd", [128, 128], BF16), ("antid", [128, 128], BF16),
                            ("ident32d", [128, 128], F32), ("tri32d", [128, 128], F32),
                            ("padbd", [128, 1], F32), ("bfd", [128, 4], F32),
                            ("mfoxd", [128, 4, 512], BF16), ("mmlad", [128, 4, 512], BF16)):
        ins[name] = k.dram(name, shape, dt, "ExternalInput")
    yB = k.dram("yB", [256, T], BF16, "ExternalOutput")
    yC = k.dram("yC", [256, T], BF16, "ExternalOutput")
    C = common_B(k, ins)
    att_common(k, C, ins)
    fox_phase(k, NG, C, ins["kfeat"], ins["ktok"], ins["kff"], ins["qfT"], yB)
    mla_phase(k, NG, C, ins["kfeat"], ins["ktok"], ins["qmT"], yC)
    return k.finish()


def host_chunk_mask():
    kkp = np.arange(128)[:, None, None]
    j = np.arange(8)[None, :, None]
    q = np.arange(512)[None, None, :]
    kidx = j * 128 + (127 - kkp)
    dc = (q + 512) // 64 - kidx // 64
    return np.where((dc >= 0) & (dc <= 8), 0.0, NEG).astype(np.float32)


MCHK = host_chunk_mask()
JROW = np.tile(np.arange(512, dtype=np.float32)[None], (128, 1))


def chunk_phase(k, NG, C, kfeat, ktok, dqT, relb, mchkd, Ed, yT_out):
    T = NG * 512
    rb = k.sb("c_rb", [4, 513], F32)
    E = k.sb("c_E", [4, 1536], F32)
    k.dma(rb, relb)
    k.copy(E[:, 255:768], rb)
    k.ts(E[:, 0:255], C.onesrow[0:4, 0:255], rb[:, 0:1], ALU.mult)
    k.ts(E[:, 768:1536], C.onesrow[0:4, 0:256].ap.unsqueeze(1).to_broadcast([4, 3, 256]) if False else
         V(C.onesrow.ap[0:4, 0:256], C.onesrow.tok), rb[:, 512:513], ALU.mult) if False else None
    for j3 in range(3):
        k.ts(E[:, 768 + j3 * 256:768 + (j3 + 1) * 256], C.onesrow[0:4, 0:256], rb[:, 512:513], ALU.mult)
    k.dma(Ed, E)
    Tb = k.sb("c_Tb", [128, 4, 8, 512], BF16)
    mc = k.sb("c_mc", [128, 8, 512], F32)
    k.dma(mc, mchkd)
    tfs = [k.sb("c_tf%d" % i, [128, 512], F32) for i in range(2)]
    n = 0
    for h in range(4):
        for j in range(8):
            tf = tfs[n % 2]
            src = bass.AP(Ed.ap.tensor, h * 1536 + 896 - 128 * j, [[1, 128], [1, 512]])
            k.dma(tf, V(src, Ed.tok), q=("sp", "pool")[n % 2])
            k.tt(Tb[:, h, j, :], tf, mc[:, j, :], ALU.add, eng=("dve", "pool")[n % 2])
            n += 1
    KD = [k.sb("c_KD%d" % i, [128, 2, 1024], BF16) for i in range(2)]
    QD = [k.sb("c_QD%d" % i, [128, 2, 512], BF16) for i in range(2)]
    VD = [k.sb("c_VD%d" % i, [128, 8, 256], BF16) for i in range(2)]
    for gi in range(NG):
        cols = slice(gi * 512, (gi + 1) * 512)
        kc = slice(2 * gi * 512, (2 * gi + 2) * 512)
        kd, qd, vd = KD[gi % 2], QD[gi % 2], VD[gi % 2]
        k.dma(kd, kfeat[FR_DK:FR_DK + 256, kc].re("(c p) t -> p c t", p=128))
        k.dma(qd, dqT[:, cols].re("(c p) t -> p c t", p=128), q="pool")
        k.dma(vd, ktok[kc, TC_DV:TC_DV + 256].re("(kb p) c -> p kb c", p=128))
        yst = C.yst[gi % 2]
        for h in range(4):
            rows = slice((h % 2) * 64, (h % 2) * 64 + 64)
            pair = h // 2
            attn_head(k, 8,
                      lambda kb, pair=pair, rows=rows: kd[rows, pair, kb * 128:(kb + 1) * 128],
                      qd[rows, pair, :],
                      lambda kb, pair=pair: vd[:, kb, pair * 128:(pair + 1) * 128],
                      lambda kb, gi=gi: (C.padb[:, 0:1] if (gi == 0 and kb < 4) else None),
                      lambda kb, h=h: (C.antib, Tb[:, h, kb, :]),
                      C.pss, C.pts, C.onesb, yst[rows, pair, :], C.rec, rows)
        k.dma(yT_out[:, cols].re("(c p) t -> p c t", p=128), yst, key=yT_out.tok)


GELU_C = 2.0 * math.sqrt(2.0 / math.pi)


def s5_phase(k, NG, C, kfeat, sp, yT_out):
    NS = 2 * NG
    f32t = lambda n, sh: k.sb("s_" + n, sh, F32)
    X = {}
    for nm in ("arX", "aiX", "ldtX", "brX", "biX"):
        X[nm] = f32t(nm, [128, 128])
        k.dma(X[nm], sp[nm])
    dt = f32t("dt", [128, 128])
    mag = f32t("mag", [128, 128])
    th = f32t("th", [128, 128])
    th2 = f32t("th2", [128, 128])
    tf = f32t("tf", [128, 128])
    ti = k.sb("s_ti", [128, 128], I32)
    sn = f32t("sn", [128, 128])
    cs = f32t("cs", [128, 128])
    a1 = f32t("a1", [128, 128])
    a2 = f32t("a2", [128, 128])
    a3 = f32t("a3", [128, 128])
    a4 = f32t("a4", [128, 128])
    bbr = f32t("bbr", [128, 128])
    bbi = f32t("bbi", [128, 128])
    k.act(dt, X["ldtX"], AF.Exp)
    k.tt(mag, X["arX"], dt, ALU.mult)
    k.act(mag, mag, AF.Exp)
    k.tt(th, X["aiX"], dt, ALU.mult)
    k.ts(th2, th, math.pi / 2, ALU.add)
    range_reduce(k, th, th, ti, tf)
    range_reduce(k, th2, th2, ti, tf)
    k.act(sn, th, AF.Sin)
    k.act(cs, th2, AF.Sin)
    k.tt(a1, mag, cs, ALU.mult)
    k.tt(a2, mag, sn, ALU.mult)
    k.ts(a1, a1, -1.0, ALU.add)
    k.tt(a3, X["arX"], X["arX"], ALU.mult)
    k.tt(a4, X["aiX"], X["aiX"], ALU.mult)
    k.tt(a3, a3, a4, ALU.add)
    k.recip(a3, a3)
    k.tt(a4, a1, X["arX"], ALU.mult)
    k.tt(tf, a2, X["aiX"], ALU.mult)
    k.tt(a4, a4, tf, ALU.add)
    k.tt(a4, a4, a3, ALU.mult)
    k.tt(tf, a2, X["arX"], ALU.mult)
    k.tt(a1, a1, X["aiX"], ALU.mult)
    k.tt(tf, tf, a1, ALU.subtract)
    k.tt(tf, tf, a3, ALU.mult)
    k.tt(bbr, a4, X["brX"], ALU.mult)
    k.tt(a1, tf, X["biX"], ALU.mult)
    k.tt(bbr, bbr, a1, ALU.subtract)
    k.tt(bbi, a4, X["biX"], ALU.mult)
    k.tt(a1, tf, X["brX"], ALU.mult)
    k.tt(bbi, bbi, a1, ALU.add)
    mB = f32t("mB", [128, 4, 128])
    k.dma(mB, sp["maskB"])
    cdr = f32t("cdr", [128, 2, 128])
    cdi = f32t("cdi", [128, 2, 128])
    k.dma(cdr, sp["CdupR"])
    k.dma(cdi, sp["CdupI"])
    mCt = f32t("mC", [128, 4, 128])
    k.dma(mCt, sp["maskC"])
    LB = k.sb("s_LB", [128, 8, 2, 128], BF16)
    LC = k.sb("s_LC", [128, 8, 2, 128], BF16)
    for gp in range(8):
        c, kq = gp // 4, gp % 4
        for ri, src in enumerate((bbr, bbi)):
            for g2 in range(2):
                k.tt(LB[:, gp, ri, g2 * 64:(g2 + 1) * 64], src[:, c * 64:(c + 1) * 64],
                     mB[:, kq, g2 * 64:(g2 + 1) * 64], ALU.mult, eng=("dve", "pool")[g2])
        k.tt(LC[:, gp, 0, :], cdr[:, c, :], mCt[:, kq, :], ALU.mult)
        k.stt(LC[:, gp, 1, :], cdi[:, c, :], -1.0, mCt[:, kq, :], ALU.mult, ALU.mult)
    Y = {}
    for nm in ("arY", "aiY", "ldtY"):
        Y[nm] = f32t(nm, [128, 8])
        k.dma(Y[nm], sp[nm])
    dtY = f32t("dtY", [128, 8])
    rho = f32t("rho", [128, 8])
    thY = f32t("thY", [128, 8])
    k.act(dtY, Y["ldtY"], AF.Exp)
    k.tt(rho, Y["arY"], dtY, ALU.mult)
    k.act(rho, rho, AF.Exp)
    k.tt(thY, Y["aiY"], dtY, ALU.mult)
    t5 = f32t("t5", [128, 8])
    t5b = f32t("t5b", [128, 8])
    t5f = f32t("t5f", [128, 8])
    t5i = k.sb("s_t5i", [128, 8], I32)
    s512 = f32t("s512", [128, 8])
    c512 = f32t("c512", [128, 8])
    k.ts(t5, thY, 512.0, ALU.mult)
    k.ts(t5b, t5, math.pi / 2, ALU.add)
    range_reduce(k, t5, t5, t5i, t5f)
    range_reduce(k, t5b, t5b, t5i, t5f)
    k.act(s512, t5, AF.Sin)
    k.act(c512, t5b, AF.Sin)
    jrow = f32t("jrow", [128, 512])
    k.dma(jrow, sp["jrow"])
    cosT = f32t("cosT", [128, 8, 512])
    sinT = f32t("sinT", [128, 8, 512])
    rhoT = f32t("rhoT", [128, 8, 512])
    ag = f32t("ag", [128, 512])
    ag2 = f32t("ag2", [128, 512])
    agf = f32t("agf", [128, 512])
    agi = k.sb("s_agi", [128, 512], I32)
    for gp in range(8):
        k.ts(ag, jrow, thY[:, gp:gp + 1], ALU.mult)
        k.ts(ag2, ag, math.pi / 2, ALU.add)
        range_reduce(k, ag, ag, agi, agf)
        range_reduce(k, ag2, ag2, agi, agf)
        k.act(sinT[:, gp, :], ag, AF.Sin)
        k.act(cosT[:, gp, :], ag2, AF.Sin)
        k.ts(rhoT[:, gp, :], C.onesrow, rho[:, gp:gp + 1], ALU.mult, eng="pool")
    dX = f32t("dX", [128, 2])
    k.dma(dX, sp["dX"])
    wg_s = f32t("wg_s", [128, 2, 256])
    k.dma(wg_s, sp["wglu"].re("(c p) f -> p c f", p=128))
    wg_b = k.sb("s_wg_b", [128, 2, 256], BF16)
    k.copy(wg_b, wg_s)
    Wre = f32t("Wre", [128, 8, 512])
    Wim = f32t("Wim", [128, 8, 512])
    ini_re = f32t("ini_re", [128, 8])
    ini_im = f32t("ini_im", [128, 8])
    k.memset(ini_re, 0.0)
    k.memset(ini_im, 0.0)
    c1 = f32t("c1", [128, 8])
    c2 = f32t("c2", [128, 8])
    uTs = [k.sb("s_uT%d" % i, [128, 2, 512], BF16) for i in range(2)]
    prs = [f32t("prs%d" % i, [128, 512]) for i in range(2)]
    pis = [f32t("pis%d" % i, [128, 512]) for i in range(2)]
    q1 = [f32t("q1_%d" % i, [128, 512]) for i in range(2)]
    q2 = [f32t("q2_%d" % i, [128, 512]) for i in range(2)]
    q3, q4 = q1, q2
    are = [f32t("are%d" % i, [128, 512]) for i in range(2)]
    aim = [f32t("aim%d" % i, [128, 512]) for i in range(2)]
    xr = [k.sb("s_xr%d" % i, [128, 512], BF16) for i in range(2)]
    xi = [k.sb("s_xi%d" % i, [128, 512], BF16) for i in range(2)]
    yv = f32t("yv", [128, 2, 512])
    y2 = f32t("y2", [128, 512])
    ygT = k.sb("s_ygT", [128, 2, 512], BF16)
    sg = f32t("sg", [128, 512])
    pss = C.pss
    for s in range(NS):
        uT = uTs[s % 2]
        k.dma(uT, kfeat[FR_U:FR_U + 256, s * 512:(s + 1) * 512].re("(c p) t -> p c t", p=128))
        own = (s % 2 == 1)
        for gp in range(8):
            c, kq = gp // 4, gp % 4
            b = gp % 2
            pa, pb = pss[(2 * gp) % 4], pss[(2 * gp + 1) % 4]
            k.mm(pa, LB[:, gp, 0, :], uT[:, c, :])
            k.mm(pb, LB[:, gp, 1, :], uT[:, c, :])
            k.copy(prs[b], pa, eng="act")
            k.copy(pis[b], pb, eng="act")
            k.tt(q1[b], prs[b], cosT[:, gp, :], ALU.mult)
            k.tt(q2[b], pis[b], sinT[:, gp, :], ALU.mult, eng="pool")
            k.tt(are[b], q1[b], q2[b], ALU.add)
            k.tt(q3[b], pis[b], cosT[:, gp, :], ALU.mult)
            k.tt(q4[b], prs[b], sinT[:, gp, :], ALU.mult, eng="pool")
            k.tt(aim[b], q3[b], q4[b], ALU.subtract)
            k.scan(Wre[:, gp, :], rhoT[:, gp, :], are[b], ini_re[:, gp:gp + 1])
            k.scan(Wim[:, gp, :], rhoT[:, gp, :], aim[b], ini_im[:, gp:gp + 1])
            if own:
                k.tt(q1[b], Wre[:, gp, :], cosT[:, gp, :], ALU.mult)
                k.tt(q2[b], Wim[:, gp, :], sinT[:, gp, :], ALU.mult, eng="pool")
                k.tt(xr[b], q1[b], q2[b], ALU.subtract)
                k.tt(q3[b], Wre[:, gp, :], sinT[:, gp, :], ALU.mult, eng="pool")
                k.tt(q4[b], Wim[:, gp, :], cosT[:, gp, :], ALU.mult)
                k.tt(xi[b], q3[b], q4[b], ALU.add)
                py = pss[4 + c]
                k.mm(py, LC[:, gp, 0, :], xr[b], start=(kq == 0), stop=False)
                k.mm(py, LC[:, gp, 1, :], xi[b], start=False, stop=(kq == 3))
        if s + 1 < NS:
            k.tt(c1, c512, Wre[:, :, 511], ALU.mult)
            k.tt(c2, s512, Wim[:, :, 511], ALU.mult)
            k.tt(ini_re, c1, c2, ALU.subtract)
            k.tt(c1, s512, Wre[:, :, 511], ALU.mult)
            k.tt(c2, c512, Wim[:, :, 511], ALU.mult)
            k.tt(ini_im, c1, c2, ALU.add)
        if own:
            gi = s // 2
            cols = slice(gi * 512, (gi + 1) * 512)
            for c in range(2):
                k.stt(yv[:, c, :], uT[:, c, :], dX[:, c:c + 1], pss[4 + c], ALU.mult, ALU.add)
                k.tt(y2, yv[:, c, :], yv[:, c, :], ALU.mult)
                k.ts(y2, y2, 0.044715, ALU.mult, 1.0, ALU.add)
                k.tt(y2, y2, yv[:, c, :], ALU.mult)
                k.act(sg, y2, AF.Sigmoid, scale=GELU_C)
                k.tt(ygT[:, c, :], yv[:, c, :], sg, ALU.mult)
            yst = C.yst[gi % 2]
            for fc in range(2):
                ps = pss[fc]
                k.mm(ps, wg_b[:, 0, fc * 128:(fc + 1) * 128], ygT[:, 0, :], start=True, stop=False)
                k.mm(ps, wg_b[:, 1, fc * 128:(fc + 1) * 128], ygT[:, 1, :], start=False, stop=True)
                k.act(sg, ps, AF.Sigmoid)
                k.tt(yst[:, fc, :], ygT[:, fc, :], sg, ALU.mult)
            k.dma(yT_out[:, cols].re("(c p) t -> p c t", p=128), yst, key=yT_out.tok)


def host_s5_params(a_re, a_im, log_dt, b_re, b_im, c_re, c_im, d, wglu):
    f = np.float32
    def Xl(a):
        a = a.reshape(2, 8, 64)
        return np.ascontiguousarray(np.broadcast_to(a.transpose(1, 0, 2)[:, None], (8, 16, 2, 64)).reshape(128, 128), dtype=f)
    ldt2 = np.broadcast_to(log_dt[:, None], (16, 64))
    def Bl(b):
        b = b.reshape(2, 8, 64, 16)
        return np.ascontiguousarray(b.transpose(1, 3, 0, 2).reshape(128, 128), dtype=f)
    def Yl(a):
        a = a.reshape(8, 2, 64)
        return np.ascontiguousarray(a.transpose(1, 2, 0).reshape(128, 8), dtype=f)
    def Cl(cm):
        cm = cm.reshape(2, 8, 16, 64)
        t = cm.transpose(3, 0, 1, 2).reshape(64, 2, 128)
        return np.ascontiguousarray(np.concatenate([t, t], 0), dtype=f)
    maskB = np.zeros((128, 4, 128), f)
    for kq in range(4):
        for g2 in range(2):
            gl = 2 * kq + g2
            maskB[gl * 16:(gl + 1) * 16, kq, g2 * 64:(g2 + 1) * 64] = 1.0
    maskC = np.ascontiguousarray(maskB.transpose(2, 1, 0))
    return {"arX": Xl(a_re), "aiX": Xl(a_im), "ldtX": Xl(ldt2), "brX": Bl(b_re), "biX": Bl(b_im),
            "arY": Yl(a_re), "aiY": Yl(a_im), "ldtY": Yl(ldt2), "CdupR": Cl(c_re), "CdupI": Cl(c_im),
            "maskB": maskB, "maskC": maskC, "jrow": JROW,
            "dX": np.ascontiguousarray(d.reshape(2, 128).T, dtype=f), "wglu": np.ascontiguousarray(wglu, dtype=f)}


S5_SHAPES = {"arX": [128, 128], "aiX": [128, 128], "ldtX": [128, 128], "brX": [128, 128], "biX": [128, 128],
             "arY": [128, 8], "aiY": [128, 8], "ldtY": [128, 8], "CdupR": [128, 2, 128], "CdupI": [128, 2, 128],
             "maskB": [128, 4, 128], "maskC": [128, 4, 128], "jrow": [128, 512], "dX": [128, 2], "wglu": [256, 256]}


def build_B_mix2_test(NG):
    T, NK = NG * 512, 2 * NG * 512
    k = KB()
    ins = {}
    for name, shape, dt in (("kfeat", [NFR, NK], BF16), ("ktok", [NK, NTC], BF16), ("dqT", [256, T], BF16),
                            ("identd", [128, 128], BF16), ("antid", [128, 128], BF16),
                            ("ident32d", [128, 128], F32), ("tri32d", [128, 128], F32),
                            ("padbd", [128, 1], F32), ("bfd", [128, 4], F32),
                            ("mfoxd", [128, 4, 512], BF16), ("mmlad", [128, 4, 512], BF16),
                            ("relb", [4, 513], F32), ("mchkd", [128, 8, 512], F32)):
        ins[name] = k.dram(name, shape, dt, "ExternalInput")
    sp = {nm: k.dram("s5_" + nm, sh, F32, "ExternalInput") for nm, sh in S5_SHAPES.items()}
    Ed = k.dram("Ed", [4, 1536], F32, "Internal")
    yD = k.dram("yD", [256, T], BF16, "ExternalOutput")
    yA = k.dram("yA", [256, T], BF16, "ExternalOutput")
    C = common_B(k, ins)
    att_common(k, C, ins)
    with k.phase():
        chunk_phase(k, NG, C, ins["kfeat"], ins["ktok"], ins["dqT"], ins["relb"], ins["mchkd"], Ed, yD)
    with k.phase():
        s5_phase(k, NG, C, ins["kfeat"], sp, yA)
    return k.finish()


CV_F = 2048


def convert_weight(k, cv, name, src, shape):
    n = int(np.prod(shape))
    assert n % (128 * CV_F) == 0, (name, shape)
    dst = k.dram("wc_" + name, list(shape), BF16, "Internal")
    letters = "abcd"[:len(shape)]
    flat = "%s -> (%s)" % (" ".join(letters), " ".join(letters))
    sv = src.re(flat).re("(n p f) -> n p f", p=128, f=CV_F)
    dv = dst.re(flat).re("(n p f) -> n p f", p=128, f=CV_F)
    k.P.bg_mode = True
    try:
        _convert_tiles(k, cv, sv, dv, dst, n // (128 * CV_F))
    finally:
        k.P.bg_mode = False
    return dst


def _convert_tiles(k, cv, sv, dv, dst, ntiles):
    for i in range(ntiles):
        st, cb = cv["st"][cv["n"] % 2], cv["cb"][cv["n"] % 2]
        cv["n"] += 1
        k.dma(st, sv[i], q="pool")
        k.copy(cb, st, eng="pool")
        k.dma(dv[i], cb, q="pool", key=dst.tok)
    return dst


class WS:
    def __init__(self, k, nbf=5):
        self.k = k
        self.bf = [k.sb("w_bf%d" % i, [128, 8, 512], BF16) for i in range(nbf)]
        self.n = 0

    def load(self, src, KC, ncols):
        k = self.k
        bf = self.bf[self.n % len(self.bf)]
        self.n += 1
        k.dma(bf[:, 0:KC, 0:ncols], src.re("(kc p) c -> p kc c", p=128))
        return bf[:, 0:KC, 0:ncols]


def local_phase(k, NG, C, kind, final, x_in, p_in, yTs, W, x_out):
    T = NG * 512
    ws = WS(k)
    pss, pst = C.pss, C.pst
    ident = C.identb
    gts = {}
    for nm in ("g_mix", "g_ffn", "g_ple"):
        gts[nm] = k.sb("l_" + nm, [128, 8], F32)
        k.dma(gts[nm], W[nm])
    bg = k.sb("l_bg", [128, 4, 8], F32)
    k.dma(bg, W["b_gate"])
    scr = alloc_norm_scratch(k, "l")
    xgs = [k.sb("l_xg%d" % i, [128, 4, 1024], F32) for i in range(2)]
    hT = k.sb("l_hT", [128, 8, 512], BF16)
    YT = k.sb("l_YT", [128, 4, 2, 512], BF16)
    macc = k.sb("l_macc", [128, 4, 512], F32)
    mT = k.sb("l_mT", [128, 8, 512], BF16)
    gs = [k.sb("l_gs%d" % i, [128, 512], F32) for i in range(2)]
    tmp = [k.sb("l_tmp%d" % i, [128, 512], F32) for i in range(2)]
    nfc = 22 if kind == "ffn" else 11
    hid = k.sb("l_hid", [128, nfc, 512], BF16)
    pf = k.sb("l_pf", [128, 4, 256], F32)
    pb = k.sb("l_pb", [128, 4, 256], BF16)
    pT = k.sb("l_pT", [128, 2, 512], BF16)
    if final:
        gfin = k.sb("l_gfin", [128, 1024], F32)
        k.dma(gfin, W["g_final"])
    if kind == "moe":
        xhT = k.sb("l_xhT", [128, 8, 512], BF16)
        xlT = k.sb("l_xlT", [128, 8, 512], BF16)
        xn32 = k.sb("l_xn32", [128, 1024], F32)
        xnf = k.sb("l_xnf", [128, 1024], F32)
        xlo = k.sb("l_xlo", [128, 4, 1024], BF16)
        wr_s = k.sb("l_wr_s", [128, 8, 8], F32)
        wg32 = k.sb("l_wg32", [128, 8, 8], F32)
        wgf = k.sb("l_wgf", [128, 8, 8], F32)
        wg_hi = k.sb("l_wg_hi", [128, 8, 8], BF16)
        wg_lo = k.sb("l_wg_lo", [128, 8, 8], BF16)
        brt = k.sb("l_brt", [128, 8], F32)
        k.dma(wr_s, W["w_router"].re("(kc p) e -> p kc e", p=128))
        k.dma(brt, W["b_router"])
        for kc in range(8):
            k.ts(wg32[:, kc, :], wr_s[:, kc, :], gts["g_ffn"][:, kc:kc + 1], ALU.mult)
        k.copy(wg_hi, wg32)
        k.copy(wgf, wg_hi)
        k.tt(wg32, wg32, wgf, ALU.subtract)
        k.copy(wg_lo, wg32)
        lg = k.sb("l_lg", [128, 4, 8], F32)
        lg2 = k.sb("l_lg2", [128, 4, 8], F32)
        eq1 = k.sb("l_eq1", [128, 4, 8], F32)
        eq2 = k.sb("l_eq2", [128, 4, 8], F32)
        wts = k.sb("l_wts", [128, 4, 8], F32)
        m1 = k.sb("l_m1", [128, 4], F32)
        m2 = k.sb("l_m2", [128, 4], F32)
        g1 = k.sb("l_g1", [128, 4], F32)
        g2 = k.sb("l_g2", [128, 4], F32)

    def xadd(xg, tb, half, ps):
        xs = xg[:, tb, half * 512:(half + 1) * 512]
        k.tt(xs, xs, ps, ALU.add)

    k.dma(xgs[0], x_in[0:512, :].re("(b p) f -> p b f", p=128))
    for gi in range(NG):
        xg = xgs[gi % 2]
        cols = slice(gi * 512, (gi + 1) * 512)
        if gi + 1 < NG:
            k.dma(xgs[(gi + 1) % 2], x_in[(gi + 1) * 512:(gi + 2) * 512, :].re("(b p) f -> p b f", p=128), q="pool")
        for mi in range(4):
            k.dma(YT[:, mi, :, :].sub("S:l_YT%d" % mi), yTs[mi][:, cols].re("(c p) t -> p c t", p=128),
                  q="pool", key="l_YT%d" % mi)
        k.dma(pf, p_in[cols, :].re("(b p) c -> p b c", p=128), q="pool")
        norm_hT(k, xg, gts["g_mix"], hT, scr, pst, ident)
        for fc4 in range(2):
            for br in range(4):
                gtile = ws.load(W["w_gate"][br][:, fc4 * 512:(fc4 + 1) * 512], 8, 512)
                btile = ws.load(W["w_branch"][br][:, fc4 * 512:(fc4 + 1) * 512], 2, 512)
                for fl in range(4):
                    fc = fc4 * 4 + fl
                    p1, p2 = pss[(2 * fl) % 6], pss[(2 * fl + 1) % 6]
                    for kc in range(8):
                        k.mm(p1, gtile[:, kc, fl * 128:(fl + 1) * 128], hT[:, kc, :], start=(kc == 0), stop=(kc == 7))
                    g_ = gs[fl % 2]
                    k.act(g_, p1, AF.Sigmoid, bias=bg[:, br, fc:fc + 1])
                    for c in range(2):
                        k.mm(p2, btile[:, c, fl * 128:(fl + 1) * 128], YT[:, br, c, :].sub("S:l_YT%d" % br),
                             start=(c == 0), stop=(c == 1))
                    if br == 0:
                        k.tt(macc[:, fl, :], g_, p2, ALU.mult)
                    else:
                        t_ = tmp[fl % 2]
                        k.tt(t_, g_, p2, ALU.mult)
                        if br < 3:
                            k.tt(macc[:, fl, :], macc[:, fl, :], t_, ALU.add, eng="pool")
                        else:
                            k.tt(mT[:, fc, :], macc[:, fl, :], t_, ALU.add, eng="pool")
        for half in range(2):
            wt = ws.load(W["w_o"][:, half * 512:(half + 1) * 512], 8, 512)
            for tb in range(4):
                ps = pss[(half * 4 + tb) % 6]
                for kc in range(8):
                    k.mm(ps, mT[:, kc, tb * 128:(tb + 1) * 128], wt[:, kc, :], start=(kc == 0), stop=(kc == 7))
                xadd(xg, tb, half, ps)
        import os
        upto = int(os.environ.get("LOCAL_UPTO", "9"))
        if upto < 1:
            k.dma(x_out[cols, :].re("(b p) f -> p b f", p=128), xg, key=x_out.tok)
            continue
        if kind == "ffn":
            norm_hT(k, xg, gts["g_ffn"], hT, scr, pst, ident, flip=1)
            ffn_expert(k, ws, pss, hT, hid, gs, W["w1"], W["w3"], W["w2"], 2816,
                       lambda tb, half, ps: xadd(xg, tb, half, ps))
        else:
            norm_hT(k, xg, gts["g_ffn"], hT, scr, pst, ident, flip=1, raw_out=xhT)
            import os
            mstop = int(os.environ.get("MOE_STOP", "9"))
            for b in range(4 if mstop >= 1 else 0):
                k.ts(xn32, xg[:, b, :], scr.rstd[:, b:b + 1], ALU.mult)
                k.copy(xnf, scr.xn[:, b, :], eng="pool")
                k.tt(xlo[:, b, :], xn32, xnf, ALU.subtract)
            for c in range(8 if mstop >= 2 else 0):
                pt = pst[c % 2]
                for b in range(4):
                    k.tr(pt[:, b * 128:(b + 1) * 128], xlo[:, b, c * 128:(c + 1) * 128], ident)
                k.copy(xlT[:, c, :], pt[:, 0:512], eng=("act" if c % 2 else "dve"))
            for tb in range(4 if mstop >= 3 else 0):
                ps = pss[tb % 6]
                tsl = slice(tb * 128, (tb + 1) * 128)
                n = 0
                for (a_, w_) in ((xhT, wg_hi), (xlT, wg_hi), (xhT, wg_lo)):
                    for kc in range(8):
                        k.mm(ps[:, 0:8], a_[:, kc, tsl], w_[:, kc, :], start=(n == 0), stop=(n == 23))
                        n += 1
                k.tt(lg[:, tb, :], ps[:, 0:8], brt, ALU.add)
            if mstop < 4:
                k.dma(x_out[cols, :].re("(b p) f -> p b f", p=128), xg, key=x_out.tok)
                continue
            k.reduce(m1, lg, ALU.max)
            for tb in range(4):
                k.ts(eq1[:, tb, :], lg[:, tb, :], m1[:, tb:tb + 1], ALU.is_equal)
            k.stt(lg2, eq1, NEG, lg, ALU.mult, ALU.add)
            k.reduce(m2, lg2, ALU.max)
            for tb in range(4):
                k.ts(eq2[:, tb, :], lg2[:, tb, :], m2[:, tb:tb + 1], ALU.is_equal)
            k.tt(g1, m1, m2, ALU.subtract)
            k.act(g1, g1, AF.Sigmoid)
            k.ts(g2, g1, -1.0, ALU.mult, 1.0, ALU.add)
            for tb in range(4):
                k.ts(eq1[:, tb, :], eq1[:, tb, :], g1[:, tb:tb + 1], ALU.mult)
                k.stt(wts[:, tb, :], eq2[:, tb, :], g2[:, tb:tb + 1], eq1[:, tb, :], ALU.mult, ALU.add)
            import os
            for e in range(int(os.environ.get("MOE_NE", "8"))):
                def addw(tb, half, ps, e=e):
                    xs = xg[:, tb, half * 512:(half + 1) * 512]
                    k.stt(xs, ps, wts[:, tb, e:e + 1], xs, ALU.mult, ALU.add)
                ffn_expert(k, ws, pss, hT, hid, gs, W["w1"][e], W["w3"][e], W["w2"][e], 1408, addw)
        if upto < 2:
            k.dma(x_out[cols, :].re("(b p) f -> p b f", p=128), xg, key=x_out.tok)
            continue
        norm_hT(k, xg, gts["g_ple"], hT, scr, pst, ident)
        k.copy(pb, pf)
        for c in range(2):
            pt = pst[c % 2]
            for b in range(4):
                k.tr(pt[:, b * 128:(b + 1) * 128], pb[:, b, c * 128:(c + 1) * 128], ident)
            k.copy(pT[:, c, :], pt[:, 0:512], eng=("act" if c % 2 else "dve"))
        for half in range(2):
            wpg = ws.load(W["ple_w_gate"][:, half * 512:(half + 1) * 512], 8, 512)
            wup = ws.load(W["ple_w_up"][:, half * 512:(half + 1) * 512], 2, 512)
            for tb in range(4):
                p1, p2 = pss[(2 * tb) % 6], pss[(2 * tb + 1) % 6]
                tsl = slice(tb * 128, (tb + 1) * 128)
                for kc in range(8):
                    k.mm(p1, hT[:, kc, tsl], wpg[:, kc, :], start=(kc == 0), stop=(kc == 7))
                g_ = gs[tb % 2]
                k.act(g_, p1, AF.Sigmoid)
                for c in range(2):
                    k.mm(p2, pT[:, c, tsl], wup[:, c, :], start=(c == 0), stop=(c == 1))
                t_ = tmp[tb % 2]
                k.tt(t_, g_, p2, ALU.mult)
                xadd(xg, tb, half, t_)
        if final:
            for b in range(4):
                k.act(scr.junk.ap, xg[:, b, :], AF.Square, accum_out=scr.ssq[:, b:b + 1])
            k.ts(scr.rstd, scr.ssq, 1.0 / 1024, ALU.mult, RMS_EPS, ALU.add)
            k.act(scr.rstd, scr.rstd, AF.Sqrt)
            k.recip(scr.rstd, scr.rstd)
            for b in range(4):
                k.stt(xg[:, b, :], xg[:, b, :], scr.rstd[:, b:b + 1], gfin, ALU.mult, ALU.mult,
                      )
        k.dma(x_out[cols, :].re("(b p) f -> p b f", p=128), xg, key=x_out.tok)


def ffn_expert(k, ws, pss, hT, hid, gs, w1, w3, w2, dff, addfn):
    nfc = dff // 128
    c0 = 0
    fc = 0
    while c0 < dff:
        nc_ = min(512, dff - c0)
        t1 = ws.load(w1[:, c0:c0 + nc_], 8, nc_)
        t3 = ws.load(w3[:, c0:c0 + nc_], 8, nc_)
        for fl in range(nc_ // 128):
            p1, p3 = pss[(2 * fl) % 6], pss[(2 * fl + 1) % 6]
            for kc in range(8):
                k.mm(p1, t1[:, kc, fl * 128:(fl + 1) * 128], hT[:, kc, :], start=(kc == 0), stop=(kc == 7))
            for kc in range(8):
                k.mm(p3, t3[:, kc, fl * 128:(fl + 1) * 128], hT[:, kc, :], start=(kc == 0), stop=(kc == 7))
            g_ = gs[fl % 2]
            import os
            if os.environ.get("NOSILU"):
                k.act(g_, p1, AF.Sigmoid)
                k.tt(g_, g_, p1, ALU.mult)
            else:
                k.act(g_, p1, AF.Silu)
            k.tt(hid[:, fc, :], g_, p3, ALU.mult)
            fc += 1
        c0 += nc_
    for half in range(2):
        k0 = 0
        while k0 < nfc:
            kn = min(8, nfc - k0)
            wt = ws.load(w2[k0 * 128:(k0 + kn) * 128, half * 512:(half + 1) * 512], kn, 512)
            for tb in range(4):
                ps = pss[tb]
                for kc in range(kn):
                    k.mm(ps, hid[:, k0 + kc, tb * 128:(tb + 1) * 128], wt[:, kc, :],
                         start=(k0 + kc == 0), stop=(k0 + kc == nfc - 1))
            k0 += kn
        for tb in range(4):
            addfn(tb, half, pss[tb])


B_CONST_SPECS = (("identd", [128, 128], BF16), ("antid", [128, 128], BF16), ("ident32d", [128, 128], F32),
                 ("tri32d", [128, 128], F32), ("padbd", [128, 1], F32), ("bfd", [128, 4], F32),
                 ("mfoxd", [128, 4, 512], BF16), ("mmlad", [128, 4, 512], BF16),
                 ("relb", [4, 513], F32), ("mchkd", [128, 8, 512], F32))


def build_B(NG, kind, final, stages=("fox", "mla", "chunk", "s5", "local")):
    T, NK = NG * 512, 2 * NG * 512
    k = KB()
    ins = {}
    for name, shape, dt in (("kfeat", [NFR, NK], BF16), ("ktok", [NK, NTC], BF16), ("kff", [NK, 4], F32),
                            ("qfT", [256, T], BF16), ("dqT", [256, T], BF16), ("qmT", [384, T], BF16),
                            ("x", [T, D], F32), ("p", [T, 256], F32)) + B_CONST_SPECS:
        ins[name] = k.dram(name, shape, dt, "ExternalInput")
    sp = {nm: k.dram("s5_" + nm, sh, F32, "ExternalInput") for nm, sh in S5_SHAPES.items()}
    W = {}
    for nm in ("g_mix", "g_ffn", "g_ple"):
        W[nm] = k.dram(nm, [128, 8], F32, "ExternalInput")
    W["b_gate"] = k.dram("b_gate", [128, 4, 8], F32, "ExternalInput")
    wgate = k.dram("w_gate", [4, D, D], F32, "ExternalInput")
    wbr = k.dram("w_branch", [4, 256, D], F32, "ExternalInput")
    W["w_gate"] = [wgate[i] for i in range(4)]
    W["w_branch"] = [wbr[i] for i in range(4)]
    W["w_o"] = k.dram("w_o", [D, D], F32, "ExternalInput")
    if kind == "ffn":
        W["w1"] = k.dram("w1", [D, 2816], F32, "ExternalInput")
        W["w3"] = k.dram("w3", [D, 2816], F32, "ExternalInput")
        W["w2"] = k.dram("w2", [2816, D], F32, "ExternalInput")
    else:
        w1 = k.dram("w1", [8, D, 1408], F32, "ExternalInput")
        w3 = k.dram("w3", [8, D, 1408], F32, "ExternalInput")
        w2 = k.dram("w2", [8, 1408, D], F32, "ExternalInput")
        W["w1"] = [w1[e] for e in range(8)]
        W["w3"] = [w3[e] for e in range(8)]
        W["w2"] = [w2[e] for e in range(8)]
        W["w_router"] = k.dram("w_router", [D, 8], F32, "ExternalInput")
        W["b_router"] = k.dram("b_router", [128, 8], F32, "ExternalInput")
    W["ple_w_gate"] = k.dram("ple_w_gate", [D, D], F32, "ExternalInput")
    W["ple_w_up"] = k.dram("ple_w_up", [256, D], F32, "ExternalInput")
    if final:
        W["g_final"] = k.dram("g_final", [128, D], F32, "ExternalInput")
    Ed = k.dram("Ed", [4, 1536], F32, "Internal")
    dbg = len(stages) < 5
    yT = [k.dram("yT%d" % i, [256, T], BF16, "ExternalOutput" if dbg else "Internal") for i in range(4)]
    xo = k.dram("xo", [T, D], F32, "ExternalOutput")
    C = common_B(k, ins)
    if "local" in stages:
        cv = {"st": [k.sb("cv_st%d" % i, [128, CV_F], F32) for i in range(2)],
              "cb": [k.sb("cv_cb%d" % i, [128, CV_F], BF16) for i in range(2)], "n": 0}
        g_ = convert_weight(k, cv, "w_gate", wgate, [4, D, D])
        b_ = convert_weight(k, cv, "w_branch", wbr, [4, 256, D])
        W["w_gate"] = [g_[i] for i in range(4)]
        W["w_branch"] = [b_[i] for i in range(4)]
        W["w_o"] = convert_weight(k, cv, "w_o", W["w_o"], [D, D])
        if kind == "ffn":
            W["w1"] = convert_weight(k, cv, "w1", W["w1"], [D, 2816])
            W["w3"] = convert_weight(k, cv, "w3", W["w3"], [D, 2816])
            W["w2"] = convert_weight(k, cv, "w2", W["w2"], [2816, D])
        else:
            c1 = convert_weight(k, cv, "w1", w1, [8, D, 1408])
            c3 = convert_weight(k, cv, "w3", w3, [8, D, 1408])
            c2 = convert_weight(k, cv, "w2", w2, [8, 1408, D])
            W["w1"] = [c1[e] for e in range(8)]
            W["w3"] = [c3[e] for e in range(8)]
            W["w2"] = [c2[e] for e in range(8)]
        W["ple_w_gate"] = convert_weight(k, cv, "ple_w_gate", W["ple_w_gate"], [D, D])
        W["ple_w_up"] = convert_weight(k, cv, "ple_w_up", W["ple_w_up"], [256, D])
    with k.phase():
        att_common(k, C, ins)
        if "fox" in stages:
            with k.phase():
                fox_phase(k, NG, C, ins["kfeat"], ins["ktok"], ins["kff"], ins["qfT"], yT[1])
        if "mla" in stages:
            with k.phase():
                mla_phase(k, NG, C, ins["kfeat"], ins["ktok"], ins["qmT"], yT[2])
        if "chunk" in stages:
            with k.phase():
                chunk_phase(k, NG, C, ins["kfeat"], ins["ktok"], ins["dqT"], ins["relb"], ins["mchkd"], Ed, yT[3])
        if "s5" in stages:
            with k.phase():
                s5_phase(k, NG, C, ins["kfeat"], sp, yT[0])
    if "local" in stages:
        with k.phase():
            local_phase(k, NG, C, kind, final, ins["x"], ins["p"], yT, W, xo)
    return k.finish()


_PROGS = {}


def _prog(key, fn):
    if key not in _PROGS:
        _PROGS[key] = fn()
    return _PROGS[key]


def _own_idx(par, NG):
    return np.concatenate([np.arange((par + 2 * i) * 512, (par + 2 * i + 1) * 512) for i in range(NG)])


def _c(a, dt=np.float32):
    return np.ascontiguousarray(a, dtype=dt)


def run_model(inp, NG, n_cores=8, stages=None):
    f = np.float32
    nb = n_cores // 2
    S = 1024 * NG
    x = _c(inp["x"])
    p = _c(inp["p"])
    pos = np.asarray(inp["positions"]).astype(np.int32)
    own = [_own_idx(par, NG) for par in range(2)]
    xs = [x[c // 2][own[c % 2]] for c in range(n_cores)]
    cores = list(range(n_cores))
    for l in range(2):
        kind = "ffn" if l % 2 == 0 else "moe"
        final = (l == 1)
        ncA = _prog(("A", NG), lambda: build_A(NG))
        insA = [host_A_inputs(xs[c], np.asarray(inp["g_mix"][l]), np.asarray(inp["w_in"][l]),
                              np.asarray(inp["mla_w_uq"][l]), np.asarray(inp["mla_w_ukv"][l]),
                              np.asarray(inp["mla_g_q"][l]), np.asarray(inp["mla_g_kv"][l]),
                              pos[c // 2][own[c % 2]]) for c in cores]
        ra = run_bass_kernel_spmd(ncA, insA, core_ids=cores).results
        ncB = _prog(("B", NG, kind, final, stages), lambda: (build_B(NG, kind, final, stages) if stages else build_B(NG, kind, final)))
        spd = host_s5_params(*[np.asarray(inp[n][l]) for n in ("ssm_a_re", "ssm_a_im", "ssm_log_dt", "ssm_b_re",
                                                                 "ssm_b_im", "ssm_c_re", "ssm_c_im", "ssm_d",
                                                                 "ssm_w_glu")])
        wd = {"g_mix": _c(np.asarray(inp["g_mix"][l]).reshape(8, 128).T),
              "g_ffn": _c(np.asarray(inp["g_ffn"][l]).reshape(8, 128).T),
              "g_ple": _c(np.asarray(inp["g_ple"][l]).reshape(8, 128).T),
              "b_gate": _c(np.asarray(inp["b_gate"][l]).reshape(4, 8, 128).transpose(2, 0, 1)),
              "w_gate": _c(inp["w_gate"][l]), "w_branch": _c(inp["w_branch"][l]), "w_o": _c(inp["w_o"][l]),
              "ple_w_gate": _c(inp["ple_w_gate"][l]), "ple_w_up": _c(inp["ple_w_up"][l]),
              "identd": IDENT, "antid": ANTI, "ident32d": IDENT32, "tri32d": TRI32,
              "bfd": _c(np.tile(np.asarray(inp["fox_b_f"][l])[None], (128, 1))),
              "mfoxd": MFOX, "mmlad": MMLA, "relb": _c(inp["chk_rel_bias"][l]), "mchkd": MCHK}
        if kind == "ffn":
            wd.update({"w1": _c(inp["ffn_w1"][l // 2]), "w3": _c(inp["ffn_w3"][l // 2]), "w2": _c(inp["ffn_w2"][l // 2])})
        else:
            j = l // 2
            wd.update({"w1": _c(inp["moe_w1"][j]), "w3": _c(inp["moe_w3"][j]), "w2": _c(inp["moe_w2"][j]),
                       "w_router": _c(inp["moe_w_router"][j]),
                       "b_router": _c(np.tile(np.asarray(inp["moe_b_router"][j])[None], (128, 1)))})
        if final:
            wd["g_final"] = _c(np.tile(np.asarray(inp["g_final"])[None], (128, 1)))
        for nm, v in spd.items():
            wd["s5_" + nm] = v
        insB = []
        for c in cores:
            b, par = c // 2, c % 2
            ra0, ra1 = ra[2 * b], ra[2 * b + 1]

            def glob(nm, axis):
                a0, a1 = np.asarray(ra0[nm]), np.asarray(ra1[nm])
                a0 = np.moveaxis(a0, axis, 0).reshape((NG, 512) + tuple(np.delete(a0.shape, axis)))
                a1 = np.moveaxis(a1, axis, 0).reshape((NG, 512) + tuple(np.delete(a1.shape, axis)))
                g_ = np.stack([a0, a1], 1).reshape((2 * NG * 512,) + a0.shape[2:])
                return np.moveaxis(g_, 0, axis)
            d = dict(wd)
            d["kfeat"] = to_slots(glob("featT", 1), par, NG, 1)
            d["ktok"] = to_slots(glob("tokM", 0), par, NG, 0)
            d["kff"] = to_slots(glob("ffM", 0), par, NG, 0)
            me = ra[c]
            d["qfT"], d["dqT"], d["qmT"] = np.asarray(me["qfT"]), np.asarray(me["dqT"]), np.asarray(me["qmT"])
            d["x"] = _c(xs[c])
            d["p"] = _c(p[l][b][own[par]])
            d["padbd"] = np.full((128, 1), NEG if par == 0 else 0.0, f)
            insB.append(d)
        rb = run_bass_kernel_spmd(ncB, insB, core_ids=cores).results
        print('layer', l, 'B done', flush=True)
        xs = [np.asarray(rb[c]["xo"]) for c in cores]
    out = np.zeros((nb, S, D), f)
    for c in cores:
        out[c // 2][own[c % 2]] = xs[c]
    return out


def kernel(**inputs):
    return run_model(inputs, 8)
```

```python
import contextlib
import math
import numpy as np
import ml_dtypes
import concourse.bass as bass
import concourse.mybir as mybir
from concourse.bass_utils import run_bass_kernel_spmd

F32 = mybir.dt.float32
BF16 = mybir.dt.bfloat16
I32 = mybir.dt.int32
AF = mybir.ActivationFunctionType
ALU = mybir.AluOpType
AX = mybir.AxisListType

ENGS = ("pe", "act", "dve", "pool", "sp")
CH = 30000
NEG = -30000.0


class Op:
    __slots__ = ("eng", "fn", "waits", "dma", "dsem", "dval", "signal", "sigval", "sigsem", "bg")

    def __init__(self, eng, fn, dma):
        self.eng = eng
        self.fn = fn
        self.dma = dma
        self.waits = []
        self.signal = False
        self.sigval = 0
        self.sigsem = 0
        self.dsem = None
        self.dval = 0
        self.bg = False


class Prog:
    def __init__(self, nc):
        self.nc = nc
        self.ops = {e: [] for e in ENGS}
        self.lastw = {}
        self.readers = {}
        self.dma_sems = {}
        self.last_dma = {}
        self.bg_mode = False
        self.selfwait = {"pe": False, "act": True, "dve": True, "pool": True, "sp": False}

    def op(self, eng, fn, reads=(), writes=(), dma_key=None):
        o = Op(eng, fn, dma_key is not None)
        o.bg = self.bg_mode
        deps = []
        for t in reads:
            w = self.lastw.get(t)
            if w is not None:
                deps.append((w, 0))
        for t in writes:
            w = self.lastw.get(t)
            if w is not None:
                deps.append((w, 1))
            for r in self.readers.get(t, ()):
                deps.append((r, 2))
        seen = set()
        for d, kind in deps:
            if d is o or id(d) in seen:
                continue
            if (not d.dma) and d.eng == eng:
                if not self.selfwait[eng]:
                    continue
            seen.add(id(d))
            o.waits.append(d)
        for t in reads:
            self.readers.setdefault(t, []).append(o)
        for t in writes:
            self.lastw[t] = o
            self.readers[t] = []
        if o.dma:
            ent = self.dma_sems.setdefault(dma_key, [0])
            ent[0] += 16
            o.dsem = dma_key
            o.dval = ent[0]
        self.ops[eng].append(o)
        if o.dma:
            self.last_dma[dma_key] = o
        return o

    def barrier(self):
        lasts = []
        for e in ENGS:
            for o in reversed(self.ops[e]):
                if not o.dma and not o.bg:
                    lasts.append(o)
                    break
        dl = [o for o in self.last_dma.values() if not o.bg]
        for e in ENGS:
            o = Op(e, lambda eng: eng.nop(), False)
            o.waits = [d for d in lasts if d.eng != e] + dl
            self.ops[e].append(o)
        self.lastw = {t: o for t, o in self.lastw.items() if o.bg}
        self.readers = {t: [r for r in rs if r.bg] for t, rs in self.readers.items()}

    def emit(self):
        nc = self.nc
        for e in ENGS:
            for o in self.ops[e]:
                for d in o.waits:
                    if not d.dma:
                        d.signal = True
        nsig = {}
        for e in ENGS:
            c = 0
            for o in self.ops[e]:
                if o.signal and not o.dma:
                    o.sigsem = c // CH
                    o.sigval = c % CH + 1
                    c += 1
            nsig[e] = c
        with contextlib.ExitStack() as st:
            esem = {e: [st.enter_context(nc.semaphore("s_%s%d" % (e, j)))
                        for j in range(nsig[e] // CH + 1)] for e in ENGS}
            dsem = {}
            for i, k in enumerate(self.dma_sems):
                dsem[k] = st.enter_context(nc.semaphore("d%d" % i))
            block = st.enter_context(nc.Block())
            hooks = {"pe": block.tensor, "act": block.scalar, "dve": block.vector,
                     "pool": block.gpsimd, "sp": block.sync}

            def make(e):
                def body(eng):
                    waited = {}
                    for o in self.ops[e]:
                        need = {}
                        for d in o.waits:
                            if d.dma:
                                s, v = dsem[d.dsem], d.dval
                            else:
                                s, v = esem[d.eng][d.sigsem], d.sigval
                            key = id(s)
                            if waited.get(key, 0) >= v:
                                continue
                            if key not in need or need[key][1] < v:
                                need[key] = (s, v)
                        for key, (s, v) in need.items():
                            eng.wait_ge(s, v)
                            waited[key] = v
                        inst = o.fn(eng)
                        if o.dma:
                            inst.then_inc(dsem[o.dsem], 16)
                        elif o.signal:
                            inst.then_inc(esem[e][o.sigsem], 1)
                    if e == "sp":
                        for k, ent in self.dma_sems.items():
                            eng.wait_ge(dsem[k], ent[0])
                return body

            for e in ENGS:
                if self.ops[e] or e == "sp":
                    hooks[e](make(e))


class V:
    __slots__ = ("ap", "tok")

    def __init__(self, ap, tok):
        self.ap = ap
        self.tok = tok

    def __getitem__(self, idx):
        return V(self.ap[idx], self.tok)

    def re(self, s, **kw):
        return V(self.ap.rearrange(s, **kw), self.tok)

    def sub(self, tok):
        return V(self.ap, tok)


def _ap(x):
    return x.ap if isinstance(x, V) else x


def _toks(*xs):
    return [x.tok for x in xs if isinstance(x, V)]


class KB:
    def __init__(self):
        self.nc = bass.Bass("TRN2", target_bir_lowering=False)
        self.P = Prog(self.nc)
        self.st = contextlib.ExitStack()
        self.nps = 0
        self.dq = 0

    @contextlib.contextmanager
    def phase(self):
        old = self.st
        self.st = contextlib.ExitStack()
        try:
            yield
        finally:
            self.P.barrier()
            self.st.close()
            self.st = old

    def dram(self, name, shape, dt, kind):
        return V(self.nc.dram_tensor(name, list(shape), dt, kind=kind).ap(), "D:" + name)

    def sb(self, name, shape, dt):
        t = self.st.enter_context(self.nc.sbuf_tensor(name, list(shape), dt))
        return V(t[:], "S:" + name)

    def ps(self, name, shape, dt):
        t = self.st.enter_context(self.nc.psum_tensor(name, list(shape), dt))
        return V(t[:], "P:" + name)

    def dma(self, out, in_, q="sp", key=None, **kw):
        o, i = _ap(out), _ap(in_)
        self.P.op(q, lambda e: e.dma_start(out=o, in_=i, **kw), reads=_toks(in_), writes=_toks(out),
                  dma_key=key or out.tok)

    def mm(self, out, lhsT, rhs, start=True, stop=True, extra_reads=()):
        o, l, r = _ap(out), _ap(lhsT), _ap(rhs)
        self.P.op("pe", lambda e: e.matmul(o, l, r, start=start, stop=stop),
                  reads=_toks(lhsT, rhs) + list(extra_reads), writes=_toks(out))

    def tr(self, out, in_, ident):
        o, i, d = _ap(out), _ap(in_), _ap(ident)
        self.P.op("pe", lambda e: e.transpose(o, i, d), reads=_toks(in_, ident), writes=_toks(out))

    def act(self, out, in_, func, bias=None, scale=1.0, accum_out=None, eng="act"):
        o, i = _ap(out), _ap(in_)
        b = _ap(bias)
        s = _ap(scale)
        a = _ap(accum_out)
        kw = {}
        if b is not None:
            kw["bias"] = b
        if a is not None:
            kw["accum_out"] = a
        self.P.op("act", lambda e: e.activation(out=o, in_=i, func=func, scale=s, **kw),
                  reads=_toks(in_, bias, scale), writes=_toks(out, accum_out))

    def tt(self, out, in0, in1, op, eng="dve"):
        o, a, b = _ap(out), _ap(in0), _ap(in1)
        self.P.op(eng, lambda e: e.tensor_tensor(out=o, in0=a, in1=b, op=op),
                  reads=_toks(in0, in1), writes=_toks(out))

    def ts(self, out, in0, s1, op0, s2=None, op1=None, eng="dve", accum_out=None):
        o, a = _ap(out), _ap(in0)
        x1, x2 = _ap(s1), _ap(s2)
        acc = _ap(accum_out)
        kw = {}
        if op1 is not None:
            kw["op1"] = op1
        if acc is not None:
            kw["accum_out"] = acc
        self.P.op(eng, lambda e: e.tensor_scalar(out=o, in0=a, scalar1=x1, scalar2=x2, op0=op0, **kw),
                  reads=_toks(in0, s1, s2), writes=_toks(out, accum_out))

    def stt(self, out, in0, scalar, in1, op0, op1, eng="dve"):
        o, a, s, b = _ap(out), _ap(in0), _ap(scalar), _ap(in1)
        self.P.op(eng, lambda e: e.scalar_tensor_tensor(out=o, in0=a, scalar=s, in1=b, op0=op0, op1=op1),
                  reads=_toks(in0, scalar, in1), writes=_toks(out))

    def copy(self, out, in_, eng="dve"):
        o, i = _ap(out), _ap(in_)
        if eng == "act":
            self.P.op("act", lambda e: e.copy(out=o, in_=i), reads=_toks(in_), writes=_toks(out))
        else:
            self.P.op(eng, lambda e: e.tensor_copy(out=o, in_=i), reads=_toks(in_), writes=_toks(out))

    def recip(self, out, in_):
        o, i = _ap(out), _ap(in_)
        self.P.op("dve", lambda e: e.reciprocal(out=o, in_=i), reads=_toks(in_), writes=_toks(out))

    def memset(self, out, val, eng="pool"):
        o = _ap(out)
        self.P.op(eng, lambda e: e.memset(o, val), writes=_toks(out))

    def scan(self, out, d0, d1, init, op0=ALU.mult, op1=ALU.add):
        o, a, b, i = _ap(out), _ap(d0), _ap(d1), _ap(init)
        self.P.op("dve", lambda e: e.tensor_tensor_scan(out=o, data0=a, data1=b, initial=i, op0=op0, op1=op1),
                  reads=_toks(d0, d1, init), writes=_toks(out))

    def reduce(self, out, in_, op, axis=AX.X):
        o, i = _ap(out), _ap(in_)
        self.P.op("dve", lambda e: e.tensor_reduce(out=o, in_=i, axis=axis, op=op),
                  reads=_toks(in_), writes=_toks(out))

    def finish(self):
        self.P.emit()
        self.st.close()
        return self.nc


D = 1024
RMS_EPS = 1e-6
C_U, C_FQ, C_FK, C_FV, C_FF, C_CQ, C_CKV, C_KPE, C_DQ, C_DK, C_DV = (
    0, 256, 512, 768, 1024, 1028, 1220, 1348, 1380, 1636, 1892)
D_IN = 2148
WA_COLS = np.concatenate([np.arange(C_U, C_U + 256), np.arange(C_FK, C_FK + 256), np.arange(C_DK, C_DK + 256),
                          np.arange(C_CKV, C_CKV + 128), np.arange(C_KPE, C_KPE + 32),
                          np.arange(C_FV, C_FV + 256), np.arange(C_DV, C_DV + 256), np.arange(C_FF, C_FF + 4)])
NWA = 1444
R_U, R_FK, R_DK, R_CKV, R_KPE, R_KSW, NFEAT = 0, 256, 512, 768, 896, 928, 960


class Scr:
    pass


def alloc_norm_scratch(k, tag=""):
    s = Scr()
    s.junk = k.sb("njunk" + tag, [128, 1024], BF16)
    s.ssq = k.sb("nssq" + tag, [128, 4], F32)
    s.rstd = k.sb("nrstd" + tag, [128, 4], F32)
    s.xn = k.sb("nxn" + tag, [128, 4, 1024], BF16)
    return s


def norm_hT(k, xg, gt, hT, s, pst, ident, flip=0, raw_out=None):
    for b in range(4):
        k.act(s.junk.ap, xg[:, b, :], AF.Square, accum_out=s.ssq[:, b:b + 1])
    k.ts(s.rstd, s.ssq, 1.0 / 1024, ALU.mult, RMS_EPS, ALU.add)
    k.act(s.rstd, s.rstd, AF.Sqrt)
    k.recip(s.rstd, s.rstd)
    for b in range(4):
        k.ts(s.xn[:, b, :], xg[:, b, :], s.rstd[:, b:b + 1], ALU.mult, eng=("dve" if b % 2 == 0 else "pool"))
    for c in range(8):
        pt = pst[c % len(pst)]
        for b in range(4):
            k.tr(pt[:, b * 128:(b + 1) * 128], s.xn[:, b, c * 128:(c + 1) * 128], ident)
        if raw_out is not None:
            k.copy(raw_out[:, c, :], pt[:, 0:512], eng=("act" if (c + flip) % 2 == 0 else "dve"))
            k.ts(hT[:, c, :], raw_out[:, c, :], gt[:, c:c + 1], ALU.mult, eng="pool")
        elif (c + flip) % 2 == 0:
            k.act(hT[:, c, :], pt[:, 0:512], AF.Copy, scale=gt[:, c:c + 1])
        else:
            k.ts(hT[:, c, :], pt[:, 0:512], gt[:, c:c + 1], ALU.mult)


S96 = 96 ** -0.5
TWO_PI = 2.0 * math.pi
RC1 = 6.28125
RC2 = float(np.float32(TWO_PI - 6.28125))
RC3 = float(TWO_PI - 6.28125 - np.float64(np.float32(TWO_PI - 6.28125)))
PI_LO = 3.1415925


def range_reduce(k, out, in_, tmp_i, tmp_f):
    k.ts(tmp_f, in_, 1.0 / TWO_PI, ALU.mult, 0.5, ALU.add)
    k.copy(tmp_i, tmp_f)
    k.copy(tmp_f, tmp_i)
    k.stt(out, tmp_f, -RC1, in_, ALU.mult, ALU.add)
    k.stt(out, tmp_f, -RC2, out, ALU.mult, ALU.add)
    k.stt(out, tmp_f, -RC3, out, ALU.mult, ALU.add)
    k.ts(tmp_f, out, -math.pi, ALU.is_lt)
    k.stt(out, tmp_f, TWO_PI, out, ALU.mult, ALU.add)
    k.ts(tmp_f, out, math.pi, ALU.is_gt)
    k.stt(out, tmp_f, -TWO_PI, out, ALU.mult, ALU.add)
    k.ts(out, out, -PI_LO, ALU.max, PI_LO, ALU.min)


FR_U, FR_FK, FR_DK, FR_MK, NFR = 0, 256, 512, 768, 1152
TC_FV, TC_DV, TC_MV, NTC = 0, 256, 512, 768
NWB = 2180


def build_A(NG):
    T = NG * 512
    k = KB()
    x = k.dram("x", [T, D], F32, "ExternalInput")
    g = k.dram("g", [128, 8], F32, "ExternalInput")
    win = k.dram("win", [D, D_IN], F32, "ExternalInput")
    wuq = k.dram("wuq", [192, 384], F32, "ExternalInput")
    wukv = k.dram("wukv", [128, 512], F32, "ExternalInput")
    gq = k.dram("gq", [128, 2], F32, "ExternalInput")
    gkv = k.dram("gkv", [128, 1], F32, "ExternalInput")
    pos = k.dram("pos", [1, T], I32, "ExternalInput")
    cst = k.dram("cst", [128, 2], F32, "ExternalInput")
    idn = k.dram("identd", [128, 128], BF16, "ExternalInput")
    featT = k.dram("featT", [NFR, T], BF16, "ExternalOutput")
    tokM = k.dram("tokM", [T, NTC], BF16, "ExternalOutput")
    ffM = k.dram("ffM", [T, 4], F32, "ExternalOutput")
    qfT = k.dram("qfT", [256, T], BF16, "ExternalOutput")
    dqT = k.dram("dqT", [256, T], BF16, "ExternalOutput")
    qmT = k.dram("qmT", [384, T], BF16, "ExternalOutput")
    build_A_body(k, NG, x, g, win, wuq, wukv, gq, gkv, pos, cst, idn, featT, tokM, ffM, qfT, dqT, qmT)
    return k.finish()


def build_A_body(k, NG, x, g, win, wuq, wukv, gq, gkv, pos, cst, idn, featT, tokM, ffM, qfT, dqT, qmT):
    ident = k.sb("ident", [128, 128], BF16)
    ones = k.sb("ones", [128, 128], BF16)
    gt = k.sb("gt", [128, 8], F32)
    gqt = k.sb("gqt", [128, 2], F32)
    gkvt = k.sb("gkvt", [128, 1], F32)
    cstt = k.sb("cstt", [128, 2], F32)
    wb = k.sb("wb", [128, 8, NWB], BF16)
    k.dma(ident, idn)
    k.dma(gt, g)
    k.dma(gqt, gq)
    k.dma(gkvt, gkv)
    k.dma(cstt, cst)
    k.memset(ones, 1.0)
    xgs = [k.sb("xg%d" % i, [128, 4, 1024], F32) for i in range(2)]
    k.dma(xgs[0], x[0:512, :].re("(b p) f -> p b f", p=128))
    wst = [k.sb("wst%d" % i, [128, D_IN], F32) for i in range(2)]
    wbt = ["S:wb%d" % kc for kc in range(8)]
    for kc in range(8):
        src = wst[kc % 2]
        k.dma(src, win[kc * 128:(kc + 1) * 128, :], q="pool" if kc % 2 else "sp")
        dst = wb[:, kc, :].sub(wbt[kc])
        e = ("pool", "dve")[kc % 2]
        k.copy(dst[:, 0:D_IN], src, eng=e)
        k.ts(dst[:, D_IN:D_IN + 16], src[:, C_KPE + 16:C_KPE + 32], -1.0, ALU.mult, eng=e)
        k.copy(dst[:, D_IN + 16:D_IN + 32], src[:, C_KPE:C_KPE + 16], eng=e)
    wuq_s = k.sb("wuq_s", [128, 2, 384], F32)
    wukv_s = k.sb("wukv_s", [128, 512], F32)
    k.dma(wuq_s[:, 0, :], wuq[0:128, :])
    k.dma(wuq_s[0:64, 1, :], wuq[128:192, :])
    k.dma(wukv_s, wukv)
    wuq_b = k.sb("wuq_b", [128, 2, 384], BF16)
    wuqsw_b = k.sb("wuqsw_b", [128, 2, 384], BF16)
    wkn_b = k.sb("wkn_b", [128, 4, 64], BF16)
    wv_b = k.sb("wv_b", [128, 4, 64], BF16)
    k.copy(wuq_b[:, 0, :], wuq_s[:, 0, :])
    k.copy(wuq_b[0:64, 1, :], wuq_s[0:64, 1, :])
    k.copy(wuqsw_b[:, 0, :], wuq_s[:, 0, :])
    k.copy(wuqsw_b[0:64, 1, :], wuq_s[0:64, 1, :])
    for c, np_ in ((0, 128), (1, 64)):
        for h in range(4):
            b0 = h * 96 + 64
            k.ts(wuqsw_b[0:np_, c, b0:b0 + 16], wuq_s[0:np_, c, b0 + 16:b0 + 32], -1.0, ALU.mult)
            k.copy(wuqsw_b[0:np_, c, b0 + 16:b0 + 32], wuq_s[0:np_, c, b0:b0 + 16])
    wk4 = wukv_s.re("k (h two d) -> k h two d", h=4, two=2)
    k.copy(wkn_b, wk4[:, :, 0, :])
    k.copy(wv_b, wk4[:, :, 1, :])

    hTs = [k.sb("hT%d" % i, [128, 8, 512], BF16) for i in range(2)]
    scr = alloc_norm_scratch(k)
    pst = [k.ps("pst%d" % i, [128, 1024], BF16) for i in range(2)]
    pss = [k.ps("ps%d" % i, [128, 512], F32) for i in range(6)]
    fst = [k.sb("fst%d" % i, [128, 6, 512], BF16) for i in range(2)]
    qst = [k.sb("qst%d" % i, [128, 4, 512], BF16) for i in range(2)]
    tst = [k.sb("tst%d" % i, [128, 4, NTC], BF16) for i in range(2)]
    ffs = [k.sb("ffs%d" % i, [128, 4, 4], F32) for i in range(2)]
    mkn = [k.sb("mkn%d" % i, [64, 4, 512], BF16) for i in range(2)]
    rk = [k.sb("rk%d" % i, [96, 512], BF16) for i in range(2)]
    qms = [k.sb("qms%d" % i, [96, 4, 512], BF16) for i in range(2)]
    posi = k.sb("posi", [96, 512], I32)
    posf = k.sb("posf", [96, 512], F32)
    ang = k.sb("ang", [96, 512], F32)
    ang2 = k.sb("ang2", [96, 512], F32)
    rtf = k.sb("rtf", [96, 512], F32)
    rti = k.sb("rti", [96, 512], I32)
    Ct = k.sb("Ct", [96, 512], F32)
    St = k.sb("St", [96, 512], F32)
    kpe_s = k.sb("kpe_s", [96, 512], F32)
    ksw_s = k.sb("ksw_s", [96, 512], F32)
    rt1 = k.sb("rt1", [96, 512], F32)
    rt2 = k.sb("rt2", [96, 512], F32)
    ckv_s = k.sb("ckv_s", [128, 512], F32)
    cq_s = k.sb("cq_s", [128, 2, 512], F32)
    sq = k.sb("sq", [128, 2, 512], BF16)
    rstd = k.sb("rstd", [128, 512], F32)
    ckvn = k.sb("ckvn", [128, 512], BF16)
    cqn = k.sb("cqn", [128, 2, 512], BF16)
    R = slice(64, 96)
    pc = [0]

    def nps():
        p_ = pss[pc[0] % 6]
        pc[0] += 1
        return p_

    def proj(c0, m, hT):
        ps = nps()
        for kc in range(8):
            k.mm(ps[0:m, :], wb[:, kc, c0:c0 + m].sub(wbt[kc]), hT[:, kc, :], start=(kc == 0), stop=(kc == 7))
        return ps

    for gi in range(NG):
        xg = xgs[gi % 2]
        if gi + 1 < NG:
            k.dma(xgs[(gi + 1) % 2], x[(gi + 1) * 512:(gi + 2) * 512, :].re("(b p) f -> p b f", p=128))
        hT = hTs[gi % 2]
        norm_hT(k, xg, gt, hT, scr, pst, ident)
        cols = slice(gi * 512, (gi + 1) * 512)
        fs, qs, tsb, ff = fst[gi % 2], qst[gi % 2], tst[gi % 2], ffs[gi % 2]
        k.dma(posi[R, :], pos[0:1, cols].re("o t -> (o) t").ap.partition_broadcast(32)
              if False else V(pos.ap[0:1, cols].partition_broadcast(32), pos.tok))
        k.copy(posf[R, :], posi[R, :])
        k.ts(ang[R, :], posf[R, :], cstt[R, 0:1], ALU.mult)
        k.ts(ang2[R, :], ang[R, :], math.pi / 2, ALU.add)
        range_reduce(k, ang[R, :], ang[R, :], rti[R, :], rtf[R, :])
        range_reduce(k, ang2[R, :], ang2[R, :], rti[R, :], rtf[R, :])
        k.act(St[R, :], ang[R, :], AF.Sin)
        k.act(Ct[R, :], ang2[R, :], AF.Sin)
        for ci, c0 in enumerate((C_U, C_U + 128, C_FK, C_FK + 128, C_DK, C_DK + 128)):
            ps = proj(c0, 128, hT)
            k.copy(fs[:, ci, :], ps, eng=("act" if ci % 2 else "dve"))
        k.dma(featT[0:768, cols].re("(c p) t -> p c t", p=128), fs, key="featT")
        for ci, c0 in enumerate((C_FQ, C_FQ + 128, C_DQ, C_DQ + 128)):
            ps = proj(c0, 128, hT)
            if ci % 2:
                k.act(qs[:, ci, :], ps, AF.Copy, scale=0.125)
            else:
                k.ts(qs[:, ci, :], ps, 0.125, ALU.mult)
        k.dma(qfT[:, cols].re("(c p) t -> p c t", p=128), qs[:, 0:2, :], key="qfT")
        k.dma(dqT[:, cols].re("(c p) t -> p c t", p=128), qs[:, 2:4, :], key="dqT")
        for tb in range(4):
            for (c0, n, o0) in ((C_FV, 256, TC_FV), (C_DV, 256, TC_DV)):
                ps = nps()
                for kc in range(8):
                    k.mm(ps[:, 0:n], hT[:, kc, tb * 128:(tb + 1) * 128], wb[:, kc, c0:c0 + n].sub(wbt[kc]),
                         start=(kc == 0), stop=(kc == 7))
                k.copy(tsb[:, tb, o0:o0 + n], ps[:, 0:n], eng=("act" if tb % 2 else "dve"))
            ps = nps()
            for kc in range(8):
                k.mm(ps[:, 0:4], hT[:, kc, tb * 128:(tb + 1) * 128], wb[:, kc, C_FF:C_FF + 4].sub(wbt[kc]),
                     start=(kc == 0), stop=(kc == 7))
            k.copy(ff[:, tb, :], ps[:, 0:4])
        k.dma(ffM[cols, :].re("(b p) c -> p b c", p=128), ff, key="ffM")
        ps = proj(C_CKV, 128, hT)
        k.copy(ckv_s, ps, eng="act")
        k.act(sq[:, 0, :], ckv_s, AF.Square)
        ps = nps()
        k.mm(ps, ones, sq[:, 0, :])
        k.ts(rstd, ps, 1.0 / 128, ALU.mult, RMS_EPS, ALU.add)
        k.act(rstd, rstd, AF.Sqrt)
        k.recip(rstd, rstd)
        k.stt(ckvn, ckv_s, gkvt[:, 0:1], rstd, ALU.mult, ALU.mult)
        mk = mkn[gi % 2]
        for h in range(4):
            ps = nps()
            k.mm(ps[0:64, :], wkn_b[:, h, :], ckvn)
            k.copy(mk[:, h, :], ps[0:64, :], eng=("act" if h % 2 else "dve"))
        for tb in range(4):
            ps = nps()
            k.mm(ps[:, 0:256], ckvn[:, tb * 128:(tb + 1) * 128], wv_b.re("k h d -> k (h d)"))
            k.copy(tsb[:, tb, TC_MV:TC_MV + 256], ps[:, 0:256], eng=("act" if tb % 2 else "dve"))
        k.dma(tokM[cols, :].re("(b p) c -> p b c", p=128), tsb, key="tokM")
        psA = proj(C_KPE - 64, 96, hT)
        psB = proj(D_IN - 64, 96, hT)
        k.tt(rt1[R, :], psA[R, :], Ct[R, :], ALU.mult)
        k.tt(rt2[R, :], psB[R, :], St[R, :], ALU.mult)
        rkk = rk[gi % 2]
        k.tt(rkk[R, :], rt1[R, :], rt2[R, :], ALU.add)
        for h in range(4):
            r0 = FR_MK + h * 96
            k.dma(featT[r0:r0 + 64, cols], mk[:, h, :], key="featT")
            k.dma(featT[r0 + 64:r0 + 96, cols], rkk[R, :], key="featT")
        ps0 = proj(C_CQ, 128, hT)
        ps1 = proj(C_CQ + 128, 64, hT)
        k.copy(cq_s[:, 0, :], ps0, eng="act")
        k.copy(cq_s[0:64, 1, :], ps1[0:64, :], eng="act")
        k.act(sq[:, 0, :], cq_s[:, 0, :], AF.Square)
        k.act(sq[0:64, 1, :], cq_s[0:64, 1, :], AF.Square)
        ps = nps()
        k.mm(ps, ones, sq[:, 0, :], start=True, stop=False)
        k.mm(ps, ones[0:64, :], sq[0:64, 1, :], start=False, stop=True)
        k.ts(rstd, ps, 1.0 / 192, ALU.mult, RMS_EPS, ALU.add)
        k.act(rstd, rstd, AF.Sqrt)
        k.recip(rstd, rstd)
        k.stt(cqn[:, 0, :], cq_s[:, 0, :], gqt[:, 0:1], rstd, ALU.mult, ALU.mult)
        k.stt(cqn[0:64, 1, :], cq_s[0:64, 1, :], gqt[0:64, 1:2], rstd[0:64, :], ALU.mult, ALU.mult)
        qm = qms[gi % 2]
        for h in range(4):
            pa = nps()
            k.mm(pa[0:96, :], wuq_b[:, 0, h * 96:(h + 1) * 96], cqn[:, 0, :], start=True, stop=False)
            k.mm(pa[0:96, :], wuq_b[0:64, 1, h * 96:(h + 1) * 96], cqn[0:64, 1, :], start=False, stop=True)
            pb = nps()
            k.mm(pb[0:96, :], wuqsw_b[:, 0, h * 96:(h + 1) * 96], cqn[:, 0, :], start=True, stop=False)
            k.mm(pb[0:96, :], wuqsw_b[0:64, 1, h * 96:(h + 1) * 96], cqn[0:64, 1, :], start=False, stop=True)
            k.ts(qm[0:64, h, :], pa[0:64, :], S96, ALU.mult)
            k.stt(rt1[R, :], pa[R, :], S96, Ct[R, :], ALU.mult, ALU.mult)
            k.stt(rt2[R, :], pb[R, :], S96, St[R, :], ALU.mult, ALU.mult)
            k.tt(qm[R, h, :], rt1[R, :], rt2[R, :], ALU.add)
        k.dma(qmT[:, cols].re("(h r) t -> r h t", r=96), qm, key="qmT")


INV_FREQ = (10000.0 ** (-np.arange(0, 32, 2, dtype=np.float32) / 32)).astype(np.float32)
IDENT = np.eye(128, dtype=np.float32).astype(ml_dtypes.bfloat16)


def host_A_inputs(x_own, g, w_in, wuq, wukv, gq, gkv, pos_own):
    cst = np.zeros((128, 2), np.float32)
    cst[64:80, 0] = INV_FREQ
    cst[80:96, 0] = INV_FREQ
    gqp = np.zeros((256,), np.float32)
    gqp[:192] = gq
    return {"x": np.ascontiguousarray(x_own, dtype=np.float32),
            "g": np.ascontiguousarray(g.reshape(8, 128).T),
            "win": np.ascontiguousarray(w_in), "wuq": np.ascontiguousarray(wuq),
            "wukv": np.ascontiguousarray(wukv),
            "gq": np.ascontiguousarray(gqp.reshape(2, 128).T), "gkv": np.ascontiguousarray(gkv.reshape(128, 1)),
            "pos": np.ascontiguousarray(pos_own.reshape(1, -1).astype(np.int32)),
            "cst": cst, "identd": IDENT}


def attn_head(k, nkb, kt_fn, qt, v_fn, bias_fn, mask_fn, pss, pts, ones_b, yrow_out, rec_t, rows):
    O, S = pss[3], pss[4]
    scb = (0, 1, 2, 5)
    LAG = 2
    pend = []

    def drain(last):
        pi, pp = pend.pop(0)
        k.mm(O, v_fn(pi), pp, start=(pi == 0), stop=last)
        k.mm(S, ones_b, pp, start=(pi == 0), stop=last)

    for idx in range(nkb):
        sc = pss[scb[idx % 4]]
        m = mask_fn(idx)
        k.mm(sc, kt_fn(idx), qt, start=True, stop=(m is None))
        if m is not None:
            k.mm(sc, m[0], m[1], start=False, stop=True)
        pt = pts[idx % len(pts)]
        b = bias_fn(idx)
        if b is None:
            k.act(pt, sc, AF.Exp)
        else:
            k.act(pt, sc, AF.Exp, bias=b)
        pend.append((idx, pt))
        if len(pend) > LAG:
            drain(False)
    while pend:
        drain(len(pend) == 1)
    k.recip(rec_t[rows, :], S[rows, :])
    k.tt(yrow_out, O[rows, :], rec_t[rows, :], ALU.mult)


def fox_phase(k, NG, C, kfeat, ktok, kff, qfT, yT_out):
    NS, NK = 2 * NG, 2 * NG * 512
    NKB = NK // 128
    T = NG * 512
    KT = [k.sb("fKT%d" % h, [67, NK], BF16) for h in range(4)]
    QT = [k.sb("fQT%d" % h, [67, T], BF16) for h in range(4)]
    Vt = k.sb("fV", [128, NKB, 256], BF16)
    for h in range(4):
        k.dma(KT[h][0:64, :], kfeat[FR_FK + h * 64:FR_FK + (h + 1) * 64, :])
        k.memset(KT[h][64:67, :], 1.0, eng="dve")
        k.dma(QT[h][0:64, :], qfT[h * 64:(h + 1) * 64, :])
    k.dma(Vt, ktok[:, TC_FV:TC_FV + 256].re("(kb p) c -> p kb c", p=128))
    ff = k.sb("f_ff", [128, NKB, 4], F32)
    t1 = k.sb("f_t1", [128, NKB, 4], F32)
    t2 = k.sb("f_t2", [128, NKB, 4], F32)
    lf = k.sb("f_lf", [128, NKB, 4], F32)
    cum = k.sb("f_cum", [128, NKB, 4], F32)
    nb = k.sb("f_nb", [128, NKB, 4], F32)
    k.dma(ff, kff.re("(kb p) h -> p kb h", p=128))
    for h in range(4):
        k.ts(ff[:, :, h], ff[:, :, h], C.bft[:, h:h + 1], ALU.add)
    k.ts(t1, ff, -1.0, ALU.mult)
    k.tt(t1, t1, ff, ALU.max)
    k.act(t1, t1, AF.Exp, scale=-1.0)
    k.act(t1, t1, AF.Ln, bias=C.one_col[:, 0:1])
    k.ts(t2, ff, 0.0, ALU.min)
    k.tt(lf, t2, t1, ALU.subtract)
    lf2 = lf.re("p kb h -> p (kb h)")
    pw, ptot = C.pss[0], C.pss[1]
    k.mm(pw[:, 0:NKB * 4], C.tri32, lf2)
    k.mm(ptot[:, 0:NKB * 4], C.ones32, lf2)
    k.copy(t1.re("p kb h -> p (kb h)"), ptot[:, 0:NKB * 4])
    for h in range(4):
        k.scan(t2[:, :, h], C.onesrow[:, 0:NKB], t1[:, :, h], 0.0)
    k.tt(t2, t2, t1, ALU.subtract)
    k.tt(cum.re("p kb h -> p (kb h)"), pw[:, 0:NKB * 4], t2.re("p kb h -> p (kb h)"), ALU.add)
    k.ts(nb, cum, -1.0, ALU.mult)
    for h in range(4):
        k.ts(nb[:, 0:4, h], nb[:, 0:4, h], C.padb[:, 0:1], ALU.add)
    cq = k.sb("f_cq", [4, 512], F32)
    cqh = k.sb("f_cqh", [4, 512], BF16)
    cqm = k.sb("f_cqm", [4, 512], BF16)
    cql = k.sb("f_cql", [4, 512], BF16)
    cqf = k.sb("f_cqf", [4, 512], F32)
    for gi in range(NG):
        pq = C.pss[2 + gi % 2]
        for j in range(4):
            kb = (2 * gi + 1) * 4 + j
            k.tr(pq[0:4, j * 128:(j + 1) * 128], cum[:, kb, :], C.ident32)
        k.copy(cq, pq[0:4, :])
        k.copy(cqh, cq)
        k.copy(cqf, cqh)
        k.tt(cq, cq, cqf, ALU.subtract)
        k.copy(cqm, cq)
        k.copy(cqf, cqm)
        k.tt(cq, cq, cqf, ALU.subtract)
        k.copy(cql, cq)
        cols = slice(gi * 512, (gi + 1) * 512)
        for h in range(4):
            k.dma(QT[h][64:65, cols], cqh[h:h + 1, :], key="fq%d" % h)
            k.dma(QT[h][65:66, cols], cqm[h:h + 1, :], key="fq%d" % h)
            k.dma(QT[h][66:67, cols], cql[h:h + 1, :], key="fq%d" % h)
    for gi in range(NG):
        nkb = (2 * gi + 2) * 4
        cols = slice(gi * 512, (gi + 1) * 512)
        yst = C.yst[gi % 2]
        for h in range(4):
            rows = slice((h % 2) * 64, (h % 2) * 64 + 64)
            pair = h // 2
            attn_head(k, nkb,
                      lambda kb, h=h: KT[h][:, kb * 128:(kb + 1) * 128],
                      QT[h][:, cols],
                      lambda kb, pair=pair: Vt[:, kb, pair * 128:(pair + 1) * 128],
                      lambda kb, h=h: nb[:, kb, h:h + 1],
                      lambda kb, nkb=nkb: ((C.identb, C.mfox[:, kb - (nkb - 4), :]) if kb >= nkb - 4 else None),
                      C.pss, C.pts, C.onesb, yst[rows, pair, :], C.rec, rows)
        k.dma(yT_out[:, cols].re("(c p) t -> p c t", p=128), yst, key=yT_out.tok)


def mla_phase(k, NG, C, kfeat, ktok, qmT, yT_out):
    NS, NK = 2 * NG, 2 * NG * 512
    NKB = NK // 128
    T = NG * 512
    KT = [k.sb("mKT%d" % h, [96, NK], BF16) for h in range(4)]
    QT = [k.sb("mQT%d" % h, [96, T], BF16) for h in range(4)]
    Vt = k.sb("mV", [128, NKB, 256], BF16)
    for h in range(4):
        k.dma(KT[h], kfeat[FR_MK + h * 96:FR_MK + (h + 1) * 96, :])
        k.dma(QT[h], qmT[h * 96:(h + 1) * 96, :])
    k.dma(Vt, ktok[:, TC_MV:TC_MV + 256].re("(kb p) c -> p kb c", p=128))
    for gi in range(NG):
        nkb = (2 * gi + 2) * 4
        cols = slice(gi * 512, (gi + 1) * 512)
        yst = C.yst[gi % 2]
        for h in range(4):
            rows = slice((h % 2) * 64, (h % 2) * 64 + 64)
            pair = h // 2
            attn_head(k, nkb,
                      lambda kb, h=h: KT[h][:, kb * 128:(kb + 1) * 128],
                      QT[h][:, cols],
                      lambda kb, pair=pair: Vt[:, kb, pair * 128:(pair + 1) * 128],
                      lambda kb: (C.padb[:, 0:1] if kb < 4 else None),
                      lambda kb, nkb=nkb: ((C.identb, C.mmla[:, kb - (nkb - 4), :]) if kb >= nkb - 4 else None),
                      C.pss, C.pts, C.onesb, yst[rows, pair, :], C.rec, rows)
        k.dma(yT_out[:, cols].re("(c p) t -> p c t", p=128), yst, key=yT_out.tok)


class Cm:
    pass


def common_B(k, cons):
    C = Cm()
    C.identb = k.sb("identb", [128, 128], BF16)
    C.antib = k.sb("antib", [128, 128], BF16)
    C.onesb = k.sb("onesb", [128, 128], BF16)
    C.ident32 = k.sb("ident32", [128, 128], F32)
    C.tri32 = k.sb("tri32", [128, 128], F32)
    C.ones32 = k.sb("ones32", [128, 128], F32)
    C.onesrow = k.sb("onesrow", [128, 512], F32)
    C.one_col = k.sb("one_col", [128, 1], F32)
    C.padb = k.sb("padb", [128, 1], F32)
    C.bft = k.sb("bft", [128, 4], F32)
    k.dma(C.identb, cons["identd"])
    k.dma(C.antib, cons["antid"])
    k.dma(C.ident32, cons["ident32d"])
    k.dma(C.tri32, cons["tri32d"])
    k.dma(C.padb, cons["padbd"])
    k.dma(C.bft, cons["bfd"])
    k.memset(C.onesb, 1.0)
    k.memset(C.ones32, 1.0)
    k.memset(C.onesrow, 1.0)
    k.memset(C.one_col, 1.0)
    C.pss = [k.ps("ps%d" % i, [128, 512], F32) for i in range(6)]
    C.pst = [k.ps("pst%d" % i, [128, 1024], BF16) for i in range(2)]
    return C


def att_common(k, C, cons):
    C.mfox = k.sb("mfox", [128, 4, 512], BF16)
    C.mmla = k.sb("mmla", [128, 4, 512], BF16)
    k.dma(C.mfox, cons["mfoxd"])
    k.dma(C.mmla, cons["mmlad"])
    C.pts = [k.sb("pT%d" % i, [128, 512], BF16) for i in range(4)]
    C.yst = [k.sb("yst%d" % i, [128, 2, 512], BF16) for i in range(2)]
    C.rec = k.sb("rec", [128, 512], F32)


def host_masks():
    kk = np.arange(128)[:, None, None]
    j = np.arange(4)[None, :, None]
    q = np.arange(512)[None, None, :]
    kidx = j * 128 + kk
    mfox = np.where(kidx <= q, 0.0, NEG).astype(np.float32)
    mmla = np.where(kidx // 64 <= q // 64, 0.0, NEG).astype(np.float32)
    return mfox.astype(ml_dtypes.bfloat16), mmla.astype(ml_dtypes.bfloat16)


ANTI = np.ascontiguousarray(np.eye(128, dtype=np.float32)[::-1]).astype(ml_dtypes.bfloat16)
IDENT32 = np.eye(128, dtype=np.float32)
TRI32 = np.triu(np.ones((128, 128), np.float32))
MFOX, MMLA = host_masks()


def slot_tiles(par, NG):
    return [-1] + list(range(2 * NG - 1)) if par == 0 else list(range(2 * NG))


def to_slots(arr_tiles, par, NG, axis):
    a = np.moveaxis(arr_tiles, axis, 0)
    a = a.reshape((2 * NG, 512) + a.shape[1:])
    out = np.zeros_like(a)
    for s, t in enumerate(slot_tiles(par, NG)):
        if t >= 0:
            out[s] = a[t]
    out = out.reshape((2 * NG * 512,) + a.shape[2:])
    return np.ascontiguousarray(np.moveaxis(out, 0, axis))


def build_B_attn_test(NG):
    T, NK = NG * 512, 2 * NG * 512
    k = KB()
    ins = {}
    for name, shape, dt in (("kfeat", [NFR, NK], BF16), ("ktok", [NK, NTC], BF16), ("kff", [NK, 4], F32),
                            ("qfT", [256, T], BF16), ("qmT", [384, T], BF16),
                            ("identd", [128, 128], BF16), ("antid", [128, 128], BF16),
                            ("ident32d", [128, 128], F32), ("tri32d", [128, 128], F32),
                            ("padbd", [128, 1], F32), ("bfd", [128, 4], F32),
                            ("mfoxd", [128, 4, 512], BF16), ("mmlad", [128, 4, 512], BF16)):
        ins[name] = k.dram(name, shape, dt, "ExternalInput")
    yB = k.dram("yB", [256, T], BF16, "ExternalOutput")
    yC = k.dram("yC", [256, T], BF16, "ExternalOutput")
    C = common_B(k, ins)
    att_common(k, C, ins)
    fox_phase(k, NG, C, ins["kfeat"], ins["ktok"], ins["kff"], ins["qfT"], yB)
    mla_phase(k, NG, C, ins["kfeat"], ins["ktok"], ins["qmT"], yC)
    return k.finish()


def host_chunk_mask():
    kkp = np.arange(128)[:, None, None]
    j = np.arange(8)[None, :, None]
    q = np.arange(512)[None, None, :]
    kidx = j * 128 + (127 - kkp)
    dc = (q + 512) // 64 - kidx // 64
    return np.where((dc >= 0) & (dc <= 8), 0.0, NEG).astype(np.float32)


MCHK = host_chunk_mask()
JROW = np.tile(np.arange(512, dtype=np.float32)[None], (128, 1))


def chunk_phase(k, NG, C, kfeat, ktok, dqT, relb, mchkd, Ed, yT_out):
    T = NG * 512
    rb = k.sb("c_rb", [4, 513], F32)
    E = k.sb("c_E", [4, 1536], F32)
    k.dma(rb, relb)
    k.copy(E[:, 255:768], rb)
    k.ts(E[:, 0:255], C.onesrow[0:4, 0:255], rb[:, 0:1], ALU.mult)
    k.ts(E[:, 768:1536], C.onesrow[0:4, 0:256].ap.unsqueeze(1).to_broadcast([4, 3, 256]) if False else
         V(C.onesrow.ap[0:4, 0:256], C.onesrow.tok), rb[:, 512:513], ALU.mult) if False else None
    for j3 in range(3):
        k.ts(E[:, 768 + j3 * 256:768 + (j3 + 1) * 256], C.onesrow[0:4, 0:256], rb[:, 512:513], ALU.mult)
    k.dma(Ed, E)
    Tb = k.sb("c_Tb", [128, 4, 8, 512], BF16)
    mc = k.sb("c_mc", [128, 8, 512], F32)
    k.dma(mc, mchkd)
    tfs = [k.sb("c_tf%d" % i, [128, 512], F32) for i in range(2)]
    n = 0
    for h in range(4):
        for j in range(8):
            tf = tfs[n % 2]
            src = bass.AP(Ed.ap.tensor, h * 1536 + 896 - 128 * j, [[1, 128], [1, 512]])
            k.dma(tf, V(src, Ed.tok), q=("sp", "pool")[n % 2])
            k.tt(Tb[:, h, j, :], tf, mc[:, j, :], ALU.add, eng=("dve", "pool")[n % 2])
            n += 1
    KD = [k.sb("c_KD%d" % i, [128, 2, 1024], BF16) for i in range(2)]
    QD = [k.sb("c_QD%d" % i, [128, 2, 512], BF16) for i in range(2)]
    VD = [k.sb("c_VD%d" % i, [128, 8, 256], BF16) for i in range(2)]
    for gi in range(NG):
        cols = slice(gi * 512, (gi + 1) * 512)
        kc = slice(2 * gi * 512, (2 * gi + 2) * 512)
        kd, qd, vd = KD[gi % 2], QD[gi % 2], VD[gi % 2]
        k.dma(kd, kfeat[FR_DK:FR_DK + 256, kc].re("(c p) t -> p c t", p=128))
        k.dma(qd, dqT[:, cols].re("(c p) t -> p c t", p=128), q="pool")
        k.dma(vd, ktok[kc, TC_DV:TC_DV + 256].re("(kb p) c -> p kb c", p=128))
        yst = C.yst[gi % 2]
        for h in range(4):
            rows = slice((h % 2) * 64, (h % 2) * 64 + 64)
            pair = h // 2
            attn_head(k, 8,
                      lambda kb, pair=pair, rows=rows: kd[rows, pair, kb * 128:(kb + 1) * 128],
                      qd[rows, pair, :],
                      lambda kb, pair=pair: vd[:, kb, pair * 128:(pair + 1) * 128],
                      lambda kb, gi=gi: (C.padb[:, 0:1] if (gi == 0 and kb < 4) else None),
                      lambda kb, h=h: (C.antib, Tb[:, h, kb, :]),
                      C.pss, C.pts, C.onesb, yst[rows, pair, :], C.rec, rows)
        k.dma(yT_out[:, cols].re("(c p) t -> p c t", p=128), yst, key=yT_out.tok)


GELU_C = 2.0 * math.sqrt(2.0 / math.pi)


def s5_phase(k, NG, C, kfeat, sp, yT_out):
    NS = 2 * NG
    f32t = lambda n, sh: k.sb("s_" + n, sh, F32)
    X = {}
    for nm in ("arX", "aiX", "ldtX", "brX", "biX"):
        X[nm] = f32t(nm, [128, 128])
        k.dma(X[nm], sp[nm])
    dt = f32t("dt", [128, 128])
    mag = f32t("mag", [128, 128])
    th = f32t("th", [128, 128])
    th2 = f32t("th2", [128, 128])
    tf = f32t("tf", [128, 128])
    ti = k.sb("s_ti", [128, 128], I32)
    sn = f32t("sn", [128, 128])
    cs = f32t("cs", [128, 128])
    a1 = f32t("a1", [128, 128])
    a2 = f32t("a2", [128, 128])
    a3 = f32t("a3", [128, 128])
    a4 = f32t("a4", [128, 128])
    bbr = f32t("bbr", [128, 128])
    bbi = f32t("bbi", [128, 128])
    k.act(dt, X["ldtX"], AF.Exp)
    k.tt(mag, X["arX"], dt, ALU.mult)
    k.act(mag, mag, AF.Exp)
    k.tt(th, X["aiX"], dt, ALU.mult)
    k.ts(th2, th, math.pi / 2, ALU.add)
    range_reduce(k, th, th, ti, tf)
    range_reduce(k, th2, th2, ti, tf)
    k.act(sn, th, AF.Sin)
    k.act(cs, th2, AF.Sin)
    k.tt(a1, mag, cs, ALU.mult)
    k.tt(a2, mag, sn, ALU.mult)
    k.ts(a1, a1, -1.0, ALU.add)
    k.tt(a3, X["arX"], X["arX"], ALU.mult)
    k.tt(a4, X["aiX"], X["aiX"], ALU.mult)
    k.tt(a3, a3, a4, ALU.add)
    k.recip(a3, a3)
    k.tt(a4, a1, X["arX"], ALU.mult)
    k.tt(tf, a2, X["aiX"], ALU.mult)
    k.tt(a4, a4, tf, ALU.add)
    k.tt(a4, a4, a3, ALU.mult)
    k.tt(tf, a2, X["arX"], ALU.mult)
    k.tt(a1, a1, X["aiX"], ALU.mult)
    k.tt(tf, tf, a1, ALU.subtract)
    k.tt(tf, tf, a3, ALU.mult)
    k.tt(bbr, a4, X["brX"], ALU.mult)
    k.tt(a1, tf, X["biX"], ALU.mult)
    k.tt(bbr, bbr, a1, ALU.subtract)
    k.tt(bbi, a4, X["biX"], ALU.mult)
    k.tt(a1, tf, X["brX"], ALU.mult)
    k.tt(bbi, bbi, a1, ALU.add)
    mB = f32t("mB", [128, 4, 128])
    k.dma(mB, sp["maskB"])
    cdr = f32t("cdr", [128, 2, 128])
    cdi = f32t("cdi", [128, 2, 128])
    k.dma(cdr, sp["CdupR"])
    k.dma(cdi, sp["CdupI"])
    mCt = f32t("mC", [128, 4, 128])
    k.dma(mCt, sp["maskC"])
    LB = k.sb("s_LB", [128, 8, 2, 128], BF16)
    LC = k.sb("s_LC", [128, 8, 2, 128], BF16)
    for gp in range(8):
        c, kq = gp // 4, gp % 4
        for ri, src in enumerate((bbr, bbi)):
            for g2 in range(2):
                k.tt(LB[:, gp, ri, g2 * 64:(g2 + 1) * 64], src[:, c * 64:(c + 1) * 64],
                     mB[:, kq, g2 * 64:(g2 + 1) * 64], ALU.mult, eng=("dve", "pool")[g2])
        k.tt(LC[:, gp, 0, :], cdr[:, c, :], mCt[:, kq, :], ALU.mult)
        k.stt(LC[:, gp, 1, :], cdi[:, c, :], -1.0, mCt[:, kq, :], ALU.mult, ALU.mult)
    Y = {}
    for nm in ("arY", "aiY", "ldtY"):
        Y[nm] = f32t(nm, [128, 8])
        k.dma(Y[nm], sp[nm])
    dtY = f32t("dtY", [128, 8])
    rho = f32t("rho", [128, 8])
    thY = f32t("thY", [128, 8])
    k.act(dtY, Y["ldtY"], AF.Exp)
    k.tt(rho, Y["arY"], dtY, ALU.mult)
    k.act(rho, rho, AF.Exp)
    k.tt(thY, Y["aiY"], dtY, ALU.mult)
    t5 = f32t("t5", [128, 8])
    t5b = f32t("t5b", [128, 8])
    t5f = f32t("t5f", [128, 8])
    t5i = k.sb("s_t5i", [128, 8], I32)
    s512 = f32t("s512", [128, 8])
    c512 = f32t("c512", [128, 8])
    k.ts(t5, thY, 512.0, ALU.mult)
    k.ts(t5b, t5, math.pi / 2, ALU.add)
    range_reduce(k, t5, t5, t5i, t5f)
    range_reduce(k, t5b, t5b, t5i, t5f)
    k.act(s512, t5, AF.Sin)
    k.act(c512, t5b, AF.Sin)
    jrow = f32t("jrow", [128, 512])
    k.dma(jrow, sp["jrow"])
    cosT = f32t("cosT", [128, 8, 512])
    sinT = f32t("sinT", [128, 8, 512])
    rhoT = f32t("rhoT", [128, 8, 512])
    ag = f32t("ag", [128, 512])
    ag2 = f32t("ag2", [128, 512])
    agf = f32t("agf", [128, 512])
    agi = k.sb("s_agi", [128, 512], I32)
    for gp in range(8):
        k.ts(ag, jrow, thY[:, gp:gp + 1], ALU.mult)
        k.ts(ag2, ag, math.pi / 2, ALU.add)
        range_reduce(k, ag, ag, agi, agf)
        range_reduce(k, ag2, ag2, agi, agf)
        k.act(sinT[:, gp, :], ag, AF.Sin)
        k.act(cosT[:, gp, :], ag2, AF.Sin)
        k.ts(rhoT[:, gp, :], C.onesrow, rho[:, gp:gp + 1], ALU.mult, eng="pool")
    dX = f32t("dX", [128, 2])
    k.dma(dX, sp["dX"])
    wg_s = f32t("wg_s", [128, 2, 256])
    k.dma(wg_s, sp["wglu"].re("(c p) f -> p c f", p=128))
    wg_b = k.sb("s_wg_b", [128, 2, 256], BF16)
    k.copy(wg_b, wg_s)
    Wre = f32t("Wre", [128, 8, 512])
    Wim = f32t("Wim", [128, 8, 512])
    ini_re = f32t("ini_re", [128, 8])
    ini_im = f32t("ini_im", [128, 8])
    k.memset(ini_re, 0.0)
    k.memset(ini_im, 0.0)
    c1 = f32t("c1", [128, 8])
    c2 = f32t("c2", [128, 8])
    uTs = [k.sb("s_uT%d" % i, [128, 2, 512], BF16) for i in range(2)]
    prs = [f32t("prs%d" % i, [128, 512]) for i in range(2)]
    pis = [f32t("pis%d" % i, [128, 512]) for i in range(2)]
    q1 = [f32t("q1_%d" % i, [128, 512]) for i in range(2)]
    q2 = [f32t("q2_%d" % i, [128, 512]) for i in range(2)]
    q3, q4 = q1, q2
    are = [f32t("are%d" % i, [128, 512]) for i in range(2)]
    aim = [f32t("aim%d" % i, [128, 512]) for i in range(2)]
    xr = [k.sb("s_xr%d" % i, [128, 512], BF16) for i in range(2)]
    xi = [k.sb("s_xi%d" % i, [128, 512], BF16) for i in range(2)]
    yv = f32t("yv", [128, 2, 512])
    y2 = f32t("y2", [128, 512])
    ygT = k.sb("s_ygT", [128, 2, 512], BF16)
    sg = f32t("sg", [128, 512])
    pss = C.pss
    for s in range(NS):
        uT = uTs[s % 2]
        k.dma(uT, kfeat[FR_U:FR_U + 256, s * 512:(s + 1) * 512].re("(c p) t -> p c t", p=128))
        own = (s % 2 == 1)
        for gp in range(8):
            c, kq = gp // 4, gp % 4
            b = gp % 2
            pa, pb = pss[(2 * gp) % 4], pss[(2 * gp + 1) % 4]
            k.mm(pa, LB[:, gp, 0, :], uT[:, c, :])
            k.mm(pb, LB[:, gp, 1, :], uT[:, c, :])
            k.copy(prs[b], pa, eng="act")
            k.copy(pis[b], pb, eng="act")
            k.tt(q1[b], prs[b], cosT[:, gp, :], ALU.mult)
            k.tt(q2[b], pis[b], sinT[:, gp, :], ALU.mult, eng="pool")
            k.tt(are[b], q1[b], q2[b], ALU.add)
            k.tt(q3[b], pis[b], cosT[:, gp, :], ALU.mult)
            k.tt(q4[b], prs[b], sinT[:, gp, :], ALU.mult, eng="pool")
            k.tt(aim[b], q3[b], q4[b], ALU.subtract)
            k.scan(Wre[:, gp, :], rhoT[:, gp, :], are[b], ini_re[:, gp:gp + 1])
            k.scan(Wim[:, gp, :], rhoT[:, gp, :], aim[b], ini_im[:, gp:gp + 1])
            if own:
                k.tt(q1[b], Wre[:, gp, :], cosT[:, gp, :], ALU.mult)
                k.tt(q2[b], Wim[:, gp, :], sinT[:, gp, :], ALU.mult, eng="pool")
                k.tt(xr[b], q1[b], q2[b], ALU.subtract)
                k.tt(q3[b], Wre[:, gp, :], sinT[:, gp, :], ALU.mult, eng="pool")
                k.tt(q4[b], Wim[:, gp, :], cosT[:, gp, :], ALU.mult)
                k.tt(xi[b], q3[b], q4[b], ALU.add)
                py = pss[4 + c]
                k.mm(py, LC[:, gp, 0, :], xr[b], start=(kq == 0), stop=False)
                k.mm(py, LC[:, gp, 1, :], xi[b], start=False, stop=(kq == 3))
        if s + 1 < NS:
            k.tt(c1, c512, Wre[:, :, 511], ALU.mult)
            k.tt(c2, s512, Wim[:, :, 511], ALU.mult)
            k.tt(ini_re, c1, c2, ALU.subtract)
            k.tt(c1, s512, Wre[:, :, 511], ALU.mult)
            k.tt(c2, c512, Wim[:, :, 511], ALU.mult)
            k.tt(ini_im, c1, c2, ALU.add)
        if own:
            gi = s // 2
            cols = slice(gi * 512, (gi + 1) * 512)
            for c in range(2):
                k.stt(yv[:, c, :], uT[:, c, :], dX[:, c:c + 1], pss[4 + c], ALU.mult, ALU.add)
                k.tt(y2, yv[:, c, :], yv[:, c, :], ALU.mult)
                k.ts(y2, y2, 0.044715, ALU.mult, 1.0, ALU.add)
                k.tt(y2, y2, yv[:, c, :], ALU.mult)
                k.act(sg, y2, AF.Sigmoid, scale=GELU_C)
                k.tt(ygT[:, c, :], yv[:, c, :], sg, ALU.mult)
            yst = C.yst[gi % 2]
            for fc in range(2):
                ps = pss[fc]
                k.mm(ps, wg_b[:, 0, fc * 128:(fc + 1) * 128], ygT[:, 0, :], start=True, stop=False)
                k.mm(ps, wg_b[:, 1, fc * 128:(fc + 1) * 128], ygT[:, 1, :], start=False, stop=True)
                k.act(sg, ps, AF.Sigmoid)
                k.tt(yst[:, fc, :], ygT[:, fc, :], sg, ALU.mult)
            k.dma(yT_out[:, cols].re("(c p) t -> p c t", p=128), yst, key=yT_out.tok)


def host_s5_params(a_re, a_im, log_dt, b_re, b_im, c_re, c_im, d, wglu):
    f = np.float32
    def Xl(a):
        a = a.reshape(2, 8, 64)
        return np.ascontiguousarray(np.broadcast_to(a.transpose(1, 0, 2)[:, None], (8, 16, 2, 64)).reshape(128, 128), dtype=f)
    ldt2 = np.broadcast_to(log_dt[:, None], (16, 64))
    def Bl(b):
        b = b.reshape(2, 8, 64, 16)
        return np.ascontiguousarray(b.transpose(1, 3, 0, 2).reshape(128, 128), dtype=f)
    def Yl(a):
        a = a.reshape(8, 2, 64)
        return np.ascontiguousarray(a.transpose(1, 2, 0).reshape(128, 8), dtype=f)
    def Cl(cm):
        cm = cm.reshape(2, 8, 16, 64)
        t = cm.transpose(3, 0, 1, 2).reshape(64, 2, 128)
        return np.ascontiguousarray(np.concatenate([t, t], 0), dtype=f)
    maskB = np.zeros((128, 4, 128), f)
    for kq in range(4):
        for g2 in range(2):
            gl = 2 * kq + g2
            maskB[gl * 16:(gl + 1) * 16, kq, g2 * 64:(g2 + 1) * 64] = 1.0
    maskC = np.ascontiguousarray(maskB.transpose(2, 1, 0))
    return {"arX": Xl(a_re), "aiX": Xl(a_im), "ldtX": Xl(ldt2), "brX": Bl(b_re), "biX": Bl(b_im),
            "arY": Yl(a_re), "aiY": Yl(a_im), "ldtY": Yl(ldt2), "CdupR": Cl(c_re), "CdupI": Cl(c_im),
            "maskB": maskB, "maskC": maskC, "jrow": JROW,
            "dX": np.ascontiguousarray(d.reshape(2, 128).T, dtype=f), "wglu": np.ascontiguousarray(wglu, dtype=f)}


S5_SHAPES = {"arX": [128, 128], "aiX": [128, 128], "ldtX": [128, 128], "brX": [128, 128], "biX": [128, 128],
             "arY": [128, 8], "aiY": [128, 8], "ldtY": [128, 8], "CdupR": [128, 2, 128], "CdupI": [128, 2, 128],
             "maskB": [128, 4, 128], "maskC": [128, 4, 128], "jrow": [128, 512], "dX": [128, 2], "wglu": [256, 256]}


def build_B_mix2_test(NG):
    T, NK = NG * 512, 2 * NG * 512
    k = KB()
    ins = {}
    for name, shape, dt in (("kfeat", [NFR, NK], BF16), ("ktok", [NK, NTC], BF16), ("dqT", [256, T], BF16),
                            ("identd", [128, 128], BF16), ("antid", [128, 128], BF16),
                            ("ident32d", [128, 128], F32), ("tri32d", [128, 128], F32),
                            ("padbd", [128, 1], F32), ("bfd", [128, 4], F32),
                            ("mfoxd", [128, 4, 512], BF16), ("mmlad", [128, 4, 512], BF16),
                            ("relb", [4, 513], F32), ("mchkd", [128, 8, 512], F32)):
        ins[name] = k.dram(name, shape, dt, "ExternalInput")
    sp = {nm: k.dram("s5_" + nm, sh, F32, "ExternalInput") for nm, sh in S5_SHAPES.items()}
    Ed = k.dram("Ed", [4, 1536], F32, "Internal")
    yD = k.dram("yD", [256, T], BF16, "ExternalOutput")
    yA = k.dram("yA", [256, T], BF16, "ExternalOutput")
    C = common_B(k, ins)
    att_common(k, C, ins)
    with k.phase():
        chunk_phase(k, NG, C, ins["kfeat"], ins["ktok"], ins["dqT"], ins["relb"], ins["mchkd"], Ed, yD)
    with k.phase():
        s5_phase(k, NG, C, ins["kfeat"], sp, yA)
    return k.finish()


CV_F = 2048


def convert_weight(k, cv, name, src, shape):
    n = int(np.prod(shape))
    assert n % (128 * CV_F) == 0, (name, shape)
    dst = k.dram("wc_" + name, list(shape), BF16, "Internal")
    letters = "abcd"[:len(shape)]
    flat = "%s -> (%s)" % (" ".join(letters), " ".join(letters))
    sv = src.re(flat).re("(n p f) -> n p f", p=128, f=CV_F)
    dv = dst.re(flat).re("(n p f) -> n p f", p=128, f=CV_F)
    k.P.bg_mode = True
    try:
        _convert_tiles(k, cv, sv, dv, dst, n // (128 * CV_F))
    finally:
        k.P.bg_mode = False
    return dst


def _convert_tiles(k, cv, sv, dv, dst, ntiles):
    for i in range(ntiles):
        st, cb = cv["st"][cv["n"] % 2], cv["cb"][cv["n"] % 2]
        cv["n"] += 1
        k.dma(st, sv[i], q="pool")
        k.copy(cb, st, eng="pool")
        k.dma(dv[i], cb, q="pool", key=dst.tok)
    return dst


class WS:
    def __init__(self, k, nbf=5):
        self.k = k
        self.bf = [k.sb("w_bf%d" % i, [128, 8, 512], BF16) for i in range(nbf)]
        self.n = 0

    def load(self, src, KC, ncols):
        k = self.k
        bf = self.bf[self.n % len(self.bf)]
        self.n += 1
        k.dma(bf[:, 0:KC, 0:ncols], src.re("(kc p) c -> p kc c", p=128))
        return bf[:, 0:KC, 0:ncols]


def local_phase(k, NG, C, kind, final, x_in, p_in, yTs, W, x_out):
    T = NG * 512
    ws = WS(k)
    pss, pst = C.pss, C.pst
    ident = C.identb
    gts = {}
    for nm in ("g_mix", "g_ffn", "g_ple"):
        gts[nm] = k.sb("l_" + nm, [128, 8], F32)
        k.dma(gts[nm], W[nm])
    bg = k.sb("l_bg", [128, 4, 8], F32)
    k.dma(bg, W["b_gate"])
    scr = alloc_norm_scratch(k, "l")
    xgs = [k.sb("l_xg%d" % i, [128, 4, 1024], F32) for i in range(2)]
    hT = k.sb("l_hT", [128, 8, 512], BF16)
    YT = k.sb("l_YT", [128, 4, 2, 512], BF16)
    macc = k.sb("l_macc", [128, 4, 512], F32)
    mT = k.sb("l_mT", [128, 8, 512], BF16)
    gs = [k.sb("l_gs%d" % i, [128, 512], F32) for i in range(2)]
    tmp = [k.sb("l_tmp%d" % i, [128, 512], F32) for i in range(2)]
    nfc = 22 if kind == "ffn" else 11
    hid = k.sb("l_hid", [128, nfc, 512], BF16)
    pf = k.sb("l_pf", [128, 4, 256], F32)
    pb = k.sb("l_pb", [128, 4, 256], BF16)
    pT = k.sb("l_pT", [128, 2, 512], BF16)
    if final:
        gfin = k.sb("l_gfin", [128, 1024], F32)
        k.dma(gfin, W["g_final"])
    if kind == "moe":
        xhT = k.sb("l_xhT", [128, 8, 512], BF16)
        xlT = k.sb("l_xlT", [128, 8, 512], BF16)
        xn32 = k.sb("l_xn32", [128, 1024], F32)
        xnf = k.sb("l_xnf", [128, 1024], F32)
        xlo = k.sb("l_xlo", [128, 4, 1024], BF16)
        wr_s = k.sb("l_wr_s", [128, 8, 8], F32)
        wg32 = k.sb("l_wg32", [128, 8, 8], F32)
        wgf = k.sb("l_wgf", [128, 8, 8], F32)
        wg_hi = k.sb("l_wg_hi", [128, 8, 8], BF16)
        wg_lo = k.sb("l_wg_lo", [128, 8, 8], BF16)
        brt = k.sb("l_brt", [128, 8], F32)
        k.dma(wr_s, W["w_router"].re("(kc p) e -> p kc e", p=128))
        k.dma(brt, W["b_router"])
        for kc in range(8):
            k.ts(wg32[:, kc, :], wr_s[:, kc, :], gts["g_ffn"][:, kc:kc + 1], ALU.mult)
        k.copy(wg_hi, wg32)
        k.copy(wgf, wg_hi)
        k.tt(wg32, wg32, wgf, ALU.subtract)
        k.copy(wg_lo, wg32)
        lg = k.sb("l_lg", [128, 4, 8], F32)
        lg2 = k.sb("l_lg2", [128, 4, 8], F32)
        eq1 = k.sb("l_eq1", [128, 4, 8], F32)
        eq2 = k.sb("l_eq2", [128, 4, 8], F32)
        wts = k.sb("l_wts", [128, 4, 8], F32)
        m1 = k.sb("l_m1", [128, 4], F32)
        m2 = k.sb("l_m2", [128, 4], F32)
        g1 = k.sb("l_g1", [128, 4], F32)
        g2 = k.sb("l_g2", [128, 4], F32)

    def xadd(xg, tb, half, ps):
        xs = xg[:, tb, half * 512:(half + 1) * 512]
        k.tt(xs, xs, ps, ALU.add)

    k.dma(xgs[0], x_in[0:512, :].re("(b p) f -> p b f", p=128))
    for gi in range(NG):
        xg = xgs[gi % 2]
        cols = slice(gi * 512, (gi + 1) * 512)
        if gi + 1 < NG:
            k.dma(xgs[(gi + 1) % 2], x_in[(gi + 1) * 512:(gi + 2) * 512, :].re("(b p) f -> p b f", p=128), q="pool")
        for mi in range(4):
            k.dma(YT[:, mi, :, :].sub("S:l_YT%d" % mi), yTs[mi][:, cols].re("(c p) t -> p c t", p=128),
                  q="pool", key="l_YT%d" % mi)
        k.dma(pf, p_in[cols, :].re("(b p) c -> p b c", p=128), q="pool")
        norm_hT(k, xg, gts["g_mix"], hT, scr, pst, ident)
        for fc4 in range(2):
            for br in range(4):
                gtile = ws.load(W["w_gate"][br][:, fc4 * 512:(fc4 + 1) * 512], 8, 512)
                btile = ws.load(W["w_branch"][br][:, fc4 * 512:(fc4 + 1) * 512], 2, 512)
                for fl in range(4):
                    fc = fc4 * 4 + fl
                    p1, p2 = pss[(2 * fl) % 6], pss[(2 * fl + 1) % 6]
                    for kc in range(8):
                        k.mm(p1, gtile[:, kc, fl * 128:(fl + 1) * 128], hT[:, kc, :], start=(kc == 0), stop=(kc == 7))
                    g_ = gs[fl % 2]
                    k.act(g_, p1, AF.Sigmoid, bias=bg[:, br, fc:fc + 1])
                    for c in range(2):
                        k.mm(p2, btile[:, c, fl * 128:(fl + 1) * 128], YT[:, br, c, :].sub("S:l_YT%d" % br),
                             start=(c == 0), stop=(c == 1))
                    if br == 0:
                        k.tt(macc[:, fl, :], g_, p2, ALU.mult)
                    else:
                        t_ = tmp[fl % 2]
                        k.tt(t_, g_, p2, ALU.mult)
                        if br < 3:
                            k.tt(macc[:, fl, :], macc[:, fl, :], t_, ALU.add, eng="pool")
                        else:
                            k.tt(mT[:, fc, :], macc[:, fl, :], t_, ALU.add, eng="pool")
        for half in range(2):
            wt = ws.load(W["w_o"][:, half * 512:(half + 1) * 512], 8, 512)
            for tb in range(4):
                ps = pss[(half * 4 + tb) % 6]
                for kc in range(8):
                    k.mm(ps, mT[:, kc, tb * 128:(tb + 1) * 128], wt[:, kc, :], start=(kc == 0), stop=(kc == 7))
                xadd(xg, tb, half, ps)
        import os
        upto = int(os.environ.get("LOCAL_UPTO", "9"))
        if upto < 1:
            k.dma(x_out[cols, :].re("(b p) f -> p b f", p=128), xg, key=x_out.tok)
            continue
        if kind == "ffn":
            norm_hT(k, xg, gts["g_ffn"], hT, scr, pst, ident, flip=1)
            ffn_expert(k, ws, pss, hT, hid, gs, W["w1"], W["w3"], W["w2"], 2816,
                       lambda tb, half, ps: xadd(xg, tb, half, ps))
        else:
            norm_hT(k, xg, gts["g_ffn"], hT, scr, pst, ident, flip=1, raw_out=xhT)
            import os
            mstop = int(os.environ.get("MOE_STOP", "9"))
            for b in range(4 if mstop >= 1 else 0):
                k.ts(xn32, xg[:, b, :], scr.rstd[:, b:b + 1], ALU.mult)
                k.copy(xnf, scr.xn[:, b, :], eng="pool")
                k.tt(xlo[:, b, :], xn32, xnf, ALU.subtract)
            for c in range(8 if mstop >= 2 else 0):
                pt = pst[c % 2]
                for b in range(4):
                    k.tr(pt[:, b * 128:(b + 1) * 128], xlo[:, b, c * 128:(c + 1) * 128], ident)
                k.copy(xlT[:, c, :], pt[:, 0:512], eng=("act" if c % 2 else "dve"))
            for tb in range(4 if mstop >= 3 else 0):
                ps = pss[tb % 6]
                tsl = slice(tb * 128, (tb + 1) * 128)
                n = 0
                for (a_, w_) in ((xhT, wg_hi), (xlT, wg_hi), (xhT, wg_lo)):
                    for kc in range(8):
                        k.mm(ps[:, 0:8], a_[:, kc, tsl], w_[:, kc, :], start=(n == 0), stop=(n == 23))
                        n += 1
                k.tt(lg[:, tb, :], ps[:, 0:8], brt, ALU.add)
            if mstop < 4:
                k.dma(x_out[cols, :].re("(b p) f -> p b f", p=128), xg, key=x_out.tok)
                continue
            k.reduce(m1, lg, ALU.max)
            for tb in range(4):
                k.ts(eq1[:, tb, :], lg[:, tb, :], m1[:, tb:tb + 1], ALU.is_equal)
            k.stt(lg2, eq1, NEG, lg, ALU.mult, ALU.add)
            k.reduce(m2, lg2, ALU.max)
            for tb in range(4):
                k.ts(eq2[:, tb, :], lg2[:, tb, :], m2[:, tb:tb + 1], ALU.is_equal)
            k.tt(g1, m1, m2, ALU.subtract)
            k.act(g1, g1, AF.Sigmoid)
            k.ts(g2, g1, -1.0, ALU.mult, 1.0, ALU.add)
            for tb in range(4):
                k.ts(eq1[:, tb, :], eq1[:, tb, :], g1[:, tb:tb + 1], ALU.mult)
                k.stt(wts[:, tb, :], eq2[:, tb, :], g2[:, tb:tb + 1], eq1[:, tb, :], ALU.mult, ALU.add)
            import os
            for e in range(int(os.environ.get("MOE_NE", "8"))):
                def addw(tb, half, ps, e=e):
                    xs = xg[:, tb, half * 512:(half + 1) * 512]
                    k.stt(xs, ps, wts[:, tb, e:e + 1], xs, ALU.mult, ALU.add)
                ffn_expert(k, ws, pss, hT, hid, gs, W["w1"][e], W["w3"][e], W["w2"][e], 1408, addw)
        if upto < 2:
            k.dma(x_out[cols, :].re("(b p) f -> p b f", p=128), xg, key=x_out.tok)
            continue
        norm_hT(k, xg, gts["g_ple"], hT, scr, pst, ident)
        k.copy(pb, pf)
        for c in range(2):
            pt = pst[c % 2]
            for b in range(4):
                k.tr(pt[:, b * 128:(b + 1) * 128], pb[:, b, c * 128:(c + 1) * 128], ident)
            k.copy(pT[:, c, :], pt[:, 0:512], eng=("act" if c % 2 else "dve"))
        for half in range(2):
            wpg = ws.load(W["ple_w_gate"][:, half * 512:(half + 1) * 512], 8, 512)
            wup = ws.load(W["ple_w_up"][:, half * 512:(half + 1) * 512], 2, 512)
            for tb in range(4):
                p1, p2 = pss[(2 * tb) % 6], pss[(2 * tb + 1) % 6]
                tsl = slice(tb * 128, (tb + 1) * 128)
                for kc in range(8):
                    k.mm(p1, hT[:, kc, tsl], wpg[:, kc, :], start=(kc == 0), stop=(kc == 7))
                g_ = gs[tb % 2]
                k.act(g_, p1, AF.Sigmoid)
                for c in range(2):
                    k.mm(p2, pT[:, c, tsl], wup[:, c, :], start=(c == 0), stop=(c == 1))
                t_ = tmp[tb % 2]
                k.tt(t_, g_, p2, ALU.mult)
                xadd(xg, tb, half, t_)
        if final:
            for b in range(4):
                k.act(scr.junk.ap, xg[:, b, :], AF.Square, accum_out=scr.ssq[:, b:b + 1])
            k.ts(scr.rstd, scr.ssq, 1.0 / 1024, ALU.mult, RMS_EPS, ALU.add)
            k.act(scr.rstd, scr.rstd, AF.Sqrt)
            k.recip(scr.rstd, scr.rstd)
            for b in range(4):
                k.stt(xg[:, b, :], xg[:, b, :], scr.rstd[:, b:b + 1], gfin, ALU.mult, ALU.mult,
                      )
        k.dma(x_out[cols, :].re("(b p) f -> p b f", p=128), xg, key=x_out.tok)


def ffn_expert(k, ws, pss, hT, hid, gs, w1, w3, w2, dff, addfn):
    nfc = dff // 128
    c0 = 0
    fc = 0
    while c0 < dff:
        nc_ = min(512, dff - c0)
        t1 = ws.load(w1[:, c0:c0 + nc_], 8, nc_)
        t3 = ws.load(w3[:, c0:c0 + nc_], 8, nc_)
        for fl in range(nc_ // 128):
            p1, p3 = pss[(2 * fl) % 6], pss[(2 * fl + 1) % 6]
            for kc in range(8):
                k.mm(p1, t1[:, kc, fl * 128:(fl + 1) * 128], hT[:, kc, :], start=(kc == 0), stop=(kc == 7))
            for kc in range(8):
                k.mm(p3, t3[:, kc, fl * 128:(fl + 1) * 128], hT[:, kc, :], start=(kc == 0), stop=(kc == 7))
            g_ = gs[fl % 2]
            import os
            if os.environ.get("NOSILU"):
                k.act(g_, p1, AF.Sigmoid)
                k.tt(g_, g_, p1, ALU.mult)
            else:
                k.act(g_, p1, AF.Silu)
            k.tt(hid[:, fc, :], g_, p3, ALU.mult)
            fc += 1
        c0 += nc_
    for half in range(2):
        k0 = 0
        while k0 < nfc:
            kn = min(8, nfc - k0)
            wt = ws.load(w2[k0 * 128:(k0 + kn) * 128, half * 512:(half + 1) * 512], kn, 512)
            for tb in range(4):
                ps = pss[tb]
                for kc in range(kn):
                    k.mm(ps, hid[:, k0 + kc, tb * 128:(tb + 1) * 128], wt[:, kc, :],
                         start=(k0 + kc == 0), stop=(k0 + kc == nfc - 1))
            k0 += kn
        for tb in range(4):
            addfn(tb, half, pss[tb])


B_CONST_SPECS = (("identd", [128, 128], BF16), ("antid", [128, 128], BF16), ("ident32d", [128, 128], F32),
                 ("tri32d", [128, 128], F32), ("padbd", [128, 1], F32), ("bfd", [128, 4], F32),
                 ("mfoxd", [128, 4, 512], BF16), ("mmlad", [128, 4, 512], BF16),
                 ("relb", [4, 513], F32), ("mchkd", [128, 8, 512], F32))


def build_B(NG, kind, final, stages=("fox", "mla", "chunk", "s5", "local")):
    T, NK = NG * 512, 2 * NG * 512
    k = KB()
    ins = {}
    for name, shape, dt in (("kfeat", [NFR, NK], BF16), ("ktok", [NK, NTC], BF16), ("kff", [NK, 4], F32),
                            ("qfT", [256, T], BF16), ("dqT", [256, T], BF16), ("qmT", [384, T], BF16),
                            ("x", [T, D], F32), ("p", [T, 256], F32)) + B_CONST_SPECS:
        ins[name] = k.dram(name, shape, dt, "ExternalInput")
    sp = {nm: k.dram("s5_" + nm, sh, F32, "ExternalInput") for nm, sh in S5_SHAPES.items()}
    W = {}
    for nm in ("g_mix", "g_ffn", "g_ple"):
        W[nm] = k.dram(nm, [128, 8], F32, "ExternalInput")
    W["b_gate"] = k.dram("b_gate", [128, 4, 8], F32, "ExternalInput")
    wgate = k.dram("w_gate", [4, D, D], F32, "ExternalInput")
    wbr = k.dram("w_branch", [4, 256, D], F32, "ExternalInput")
    W["w_gate"] = [wgate[i] for i in range(4)]
    W["w_branch"] = [wbr[i] for i in range(4)]
    W["w_o"] = k.dram("w_o", [D, D], F32, "ExternalInput")
    if kind == "ffn":
        W["w1"] = k.dram("w1", [D, 2816], F32, "ExternalInput")
        W["w3"] = k.dram("w3", [D, 2816], F32, "ExternalInput")
        W["w2"] = k.dram("w2", [2816, D], F32, "ExternalInput")
    else:
        w1 = k.dram("w1", [8, D, 1408], F32, "ExternalInput")
        w3 = k.dram("w3", [8, D, 1408], F32, "ExternalInput")
        w2 = k.dram("w2", [8, 1408, D], F32, "ExternalInput")
        W["w1"] = [w1[e] for e in range(8)]
        W["w3"] = [w3[e] for e in range(8)]
        W["w2"] = [w2[e] for e in range(8)]
        W["w_router"] = k.dram("w_router", [D, 8], F32, "ExternalInput")
        W["b_router"] = k.dram("b_router", [128, 8], F32, "ExternalInput")
    W["ple_w_gate"] = k.dram("ple_w_gate", [D, D], F32, "ExternalInput")
    W["ple_w_up"] = k.dram("ple_w_up", [256, D], F32, "ExternalInput")
    if final:
        W["g_final"] = k.dram("g_final", [128, D], F32, "ExternalInput")
    Ed = k.dram("Ed", [4, 1536], F32, "Internal")
    dbg = len(stages) < 5
    yT = [k.dram("yT%d" % i, [256, T], BF16, "ExternalOutput" if dbg else "Internal") for i in range(4)]
    xo = k.dram("xo", [T, D], F32, "ExternalOutput")
    C = common_B(k, ins)
    if "local" in stages:
        cv = {"st": [k.sb("cv_st%d" % i, [128, CV_F], F32) for i in range(2)],
              "cb": [k.sb("cv_cb%d" % i, [128, CV_F], BF16) for i in range(2)], "n": 0}
        g_ = convert_weight(k, cv, "w_gate", wgate, [4, D, D])
        b_ = convert_weight(k, cv, "w_branch", wbr, [4, 256, D])
        W["w_gate"] = [g_[i] for i in range(4)]
        W["w_branch"] = [b_[i] for i in range(4)]
        W["w_o"] = convert_weight(k, cv, "w_o", W["w_o"], [D, D])
        if kind == "ffn":
            W["w1"] = convert_weight(k, cv, "w1", W["w1"], [D, 2816])
            W["w3"] = convert_weight(k, cv, "w3", W["w3"], [D, 2816])
            W["w2"] = convert_weight(k, cv, "w2", W["w2"], [2816, D])
        else:
            c1 = convert_weight(k, cv, "w1", w1, [8, D, 1408])
            c3 = convert_weight(k, cv, "w3", w3, [8, D, 1408])
            c2 = convert_weight(k, cv, "w2", w2, [8, 1408, D])
            W["w1"] = [c1[e] for e in range(8)]
            W["w3"] = [c3[e] for e in range(8)]
            W["w2"] = [c2[e] for e in range(8)]
        W["ple_w_gate"] = convert_weight(k, cv, "ple_w_gate", W["ple_w_gate"], [D, D])
        W["ple_w_up"] = convert_weight(k, cv, "ple_w_up", W["ple_w_up"], [256, D])
    with k.phase():
        att_common(k, C, ins)
        if "fox" in stages:
            with k.phase():
                fox_phase(k, NG, C, ins["kfeat"], ins["ktok"], ins["kff"], ins["qfT"], yT[1])
        if "mla" in stages:
            with k.phase():
                mla_phase(k, NG, C, ins["kfeat"], ins["ktok"], ins["qmT"], yT[2])
        if "chunk" in stages:
            with k.phase():
                chunk_phase(k, NG, C, ins["kfeat"], ins["ktok"], ins["dqT"], ins["relb"], ins["mchkd"], Ed, yT[3])
        if "s5" in stages:
            with k.phase():
                s5_phase(k, NG, C, ins["kfeat"], sp, yT[0])
    if "local" in stages:
        with k.phase():
            local_phase(k, NG, C, kind, final, ins["x"], ins["p"], yT, W, xo)
    return k.finish()


_PROGS = {}


def _prog(key, fn):
    if key not in _PROGS:
        _PROGS[key] = fn()
    return _PROGS[key]


def _own_idx(par, NG):
    return np.concatenate([np.arange((par + 2 * i) * 512, (par + 2 * i + 1) * 512) for i in range(NG)])


def _c(a, dt=np.float32):
    return np.ascontiguousarray(a, dtype=dt)


def run_model(inp, NG, n_cores=8, stages=None):
    f = np.float32
    nb = n_cores // 2
    S = 1024 * NG
    x = _c(inp["x"])
    p = _c(inp["p"])
    pos = np.asarray(inp["positions"]).astype(np.int32)
    own = [_own_idx(par, NG) for par in range(2)]
    xs = [x[c // 2][own[c % 2]] for c in range(n_cores)]
    cores = list(range(n_cores))
    for l in range(2):
        kind = "ffn" if l % 2 == 0 else "moe"
        final = (l == 1)
        ncA = _prog(("A", NG), lambda: build_A(NG))
        insA = [host_A_inputs(xs[c], np.asarray(inp["g_mix"][l]), np.asarray(inp["w_in"][l]),
                              np.asarray(inp["mla_w_uq"][l]), np.asarray(inp["mla_w_ukv"][l]),
                              np.asarray(inp["mla_g_q"][l]), np.asarray(inp["mla_g_kv"][l]),
                              pos[c // 2][own[c % 2]]) for c in cores]
        ra = run_bass_kernel_spmd(ncA, insA, core_ids=cores).results
        ncB = _prog(("B", NG, kind, final, stages), lambda: (build_B(NG, kind, final, stages) if stages else build_B(NG, kind, final)))
        spd = host_s5_params(*[np.asarray(inp[n][l]) for n in ("ssm_a_re", "ssm_a_im", "ssm_log_dt", "ssm_b_re",
                                                                 "ssm_b_im", "ssm_c_re", "ssm_c_im", "ssm_d",
                                                                 "ssm_w_glu")])
        wd = {"g_mix": _c(np.asarray(inp["g_mix"][l]).reshape(8, 128).T),
              "g_ffn": _c(np.asarray(inp["g_ffn"][l]).reshape(8, 128).T),
              "g_ple": _c(np.asarray(inp["g_ple"][l]).reshape(8, 128).T),
              "b_gate": _c(np.asarray(inp["b_gate"][l]).reshape(4, 8, 128).transpose(2, 0, 1)),
              "w_gate": _c(inp["w_gate"][l]), "w_branch": _c(inp["w_branch"][l]), "w_o": _c(inp["w_o"][l]),
              "ple_w_gate": _c(inp["ple_w_gate"][l]), "ple_w_up": _c(inp["ple_w_up"][l]),
              "identd": IDENT, "antid": ANTI, "ident32d": IDENT32, "tri32d": TRI32,
              "bfd": _c(np.tile(np.asarray(inp["fox_b_f"][l])[None], (128, 1))),
              "mfoxd": MFOX, "mmlad": MMLA, "relb": _c(inp["chk_rel_bias"][l]), "mchkd": MCHK}
        if kind == "ffn":
            wd.update({"w1": _c(inp["ffn_w1"][l // 2]), "w3": _c(inp["ffn_w3"][l // 2]), "w2": _c(inp["ffn_w2"][l // 2])})
        else:
            j = l // 2
            wd.update({"w1": _c(inp["moe_w1"][j]), "w3": _c(inp["moe_w3"][j]), "w2": _c(inp["moe_w2"][j]),
                       "w_router": _c(inp["moe_w_router"][j]),
                       "b_router": _c(np.tile(np.asarray(inp["moe_b_router"][j])[None], (128, 1)))})
        if final:
            wd["g_final"] = _c(np.tile(np.asarray(inp["g_final"])[None], (128, 1)))
        for nm, v in spd.items():
            wd["s5_" + nm] = v
        insB = []
        for c in cores:
            b, par = c // 2, c % 2
            ra0, ra1 = ra[2 * b], ra[2 * b + 1]

            def glob(nm, axis):
                a0, a1 = np.asarray(ra0[nm]), np.asarray(ra1[nm])
                a0 = np.moveaxis(a0, axis, 0).reshape((NG, 512) + tuple(np.delete(a0.shape, axis)))
                a1 = np.moveaxis(a1, axis, 0).reshape((NG, 512) + tuple(np.delete(a1.shape, axis)))
                g_ = np.stack([a0, a1], 1).reshape((2 * NG * 512,) + a0.shape[2:])
                return np.moveaxis(g_, 0, axis)
            d = dict(wd)
            d["kfeat"] = to_slots(glob("featT", 1), par, NG, 1)
            d["ktok"] = to_slots(glob("tokM", 0), par, NG, 0)
            d["kff"] = to_slots(glob("ffM", 0), par, NG, 0)
            me = ra[c]
            d["qfT"], d["dqT"], d["qmT"] = np.asarray(me["qfT"]), np.asarray(me["dqT"]), np.asarray(me["qmT"])
            d["x"] = _c(xs[c])
            d["p"] = _c(p[l][b][own[par]])
            d["padbd"] = np.full((128, 1), NEG if par == 0 else 0.0, f)
            insB.append(d)
        rb = run_bass_kernel_spmd(ncB, insB, core_ids=cores).results
        print('layer', l, 'B done', flush=True)
        xs = [np.asarray(rb[c]["xo"]) for c in cores]
    out = np.zeros((nb, S, D), f)
    for c in cores:
        out[c // 2][own[c % 2]] = xs[c]
    return out


def kernel(**inputs):
    return run_model(inputs, 8)
```

```python
import contextlib
import math
import numpy as np
import ml_dtypes
import concourse.bass as bass
import concourse.mybir as mybir
from concourse.bass_utils import run_bass_kernel_spmd

F32 = mybir.dt.float32
BF16 = mybir.dt.bfloat16
I32 = mybir.dt.int32
AF = mybir.ActivationFunctionType
ALU = mybir.AluOpType
AX = mybir.AxisListType

ENGS = ("pe", "act", "dve", "pool", "sp")
CH = 30000
NEG = -30000.0


class Op:
    __slots__ = ("eng", "fn", "waits", "dma", "dsem", "dval", "signal", "sigval", "sigsem", "bg")

    def __init__(self, eng, fn, dma):
        self.eng = eng
        self.fn = fn
        self.dma = dma
        self.waits = []
        self.signal = False
        self.sigval = 0
        self.sigsem = 0
        self.dsem = None
        self.dval = 0
        self.bg = False


class Prog:
    def __init__(self, nc):
        self.nc = nc
        self.ops = {e: [] for e in ENGS}
        self.lastw = {}
        self.readers = {}
        self.dma_sems = {}
        self.last_dma = {}
        self.bg_mode = False
        self.selfwait = {"pe": False, "act": True, "dve": True, "pool": True, "sp": False}

    def op(self, eng, fn, reads=(), writes=(), dma_key=None):
        o = Op(eng, fn, dma_key is not None)
        o.bg = self.bg_mode
        deps = []
        for t in reads:
            w = self.lastw.get(t)
            if w is not None:
                deps.append((w, 0))
        for t in writes:
            w = self.lastw.get(t)
            if w is not None:
                deps.append((w, 1))
            for r in self.readers.get(t, ()):
                deps.append((r, 2))
        seen = set()
        for d, kind in deps:
            if d is o or id(d) in seen:
                continue
            if (not d.dma) and d.eng == eng:
                if not self.selfwait[eng]:
                    continue
            seen.add(id(d))
            o.waits.append(d)
        for t in reads:
            self.readers.setdefault(t, []).append(o)
        for t in writes:
            self.lastw[t] = o
            self.readers[t] = []
        if o.dma:
            ent = self.dma_sems.setdefault(dma_key, [0])
            ent[0] += 16
            o.dsem = dma_key
            o.dval = ent[0]
        self.ops[eng].append(o)
        if o.dma:
            self.last_dma[dma_key] = o
        return o

    def barrier(self):
        lasts = []
        for e in ENGS:
            for o in reversed(self.ops[e]):
                if not o.dma and not o.bg:
                    lasts.append(o)
                    break
        dl = [o for o in self.last_dma.values() if not o.bg]
        for e in ENGS:
            o = Op(e, lambda eng: eng.nop(), False)
            o.waits = [d for d in lasts if d.eng != e] + dl
            self.ops[e].append(o)
        self.lastw = {t: o for t, o in self.lastw.items() if o.bg}
        self.readers = {t: [r for r in rs if r.bg] for t, rs in self.readers.items()}

    def emit(self):
        nc = self.nc
        for e in ENGS:
            for o in self.ops[e]:
                for d in o.waits:
                    if not d.dma:
                        d.signal = True
        nsig = {}
        for e in ENGS:
            c = 0
            for o in self.ops[e]:
                if o.signal and not o.dma:
                    o.sigsem = c // CH
                    o.sigval = c % CH + 1
                    c += 1
            nsig[e] = c
        with contextlib.ExitStack() as st:
            esem = {e: [st.enter_context(nc.semaphore("s_%s%d" % (e, j)))
                        for j in range(nsig[e] // CH + 1)] for e in ENGS}
            dsem = {}
            for i, k in enumerate(self.dma_sems):
                dsem[k] = st.enter_context(nc.semaphore("d%d" % i))
            block = st.enter_context(nc.Block())
            hooks = {"pe": block.tensor, "act": block.scalar, "dve": block.vector,
                     "pool": block.gpsimd, "sp": block.sync}

            def make(e):
                def body(eng):
                    waited = {}
                    for o in self.ops[e]:
                        need = {}
                        for d in o.waits:
                            if d.dma:
                                s, v = dsem[d.dsem], d.dval
                            else:
                                s, v = esem[d.eng][d.sigsem], d.sigval
                            key = id(s)
                            if waited.get(key, 0) >= v:
                                continue
                            if key not in need or need[key][1] < v:
                                need[key] = (s, v)
                        for key, (s, v) in need.items():
                            eng.wait_ge(s, v)
                            waited[key] = v
                        inst = o.fn(eng)
                        if o.dma:
                            inst.then_inc(dsem[o.dsem], 16)
                        elif o.signal:
                            inst.then_inc(esem[e][o.sigsem], 1)
                    if e == "sp":
                        for k, ent in self.dma_sems.items():
                            eng.wait_ge(dsem[k], ent[0])
                return body

            for e in ENGS:
                if self.ops[e] or e == "sp":
                    hooks[e](make(e))


class V:
    __slots__ = ("ap", "tok")

    def __init__(self, ap, tok):
        self.ap = ap
        self.tok = tok

    def __getitem__(self, idx):
        return V(self.ap[idx], self.tok)

    def re(self, s, **kw):
        return V(self.ap.rearrange(s, **kw), self.tok)

    def sub(self, tok):
        return V(self.ap, tok)


def _ap(x):
    return x.ap if isinstance(x, V) else x


def _toks(*xs):
    return [x.tok for x in xs if isinstance(x, V)]


class KB:
    def __init__(self):
        self.nc = bass.Bass("TRN2", target_bir_lowering=False)
        self.P = Prog(self.nc)
        self.st = contextlib.ExitStack()
        self.nps = 0
        self.dq = 0

    @contextlib.contextmanager
    def phase(self):
        old = self.st
        self.st = contextlib.ExitStack()
        try:
            yield
        finally:
            self.P.barrier()
            self.st.close()
            self.st = old

    def dram(self, name, shape, dt, kind):
        return V(self.nc.dram_tensor(name, list(shape), dt, kind=kind).ap(), "D:" + name)

    def sb(self, name, shape, dt):
        t = self.st.enter_context(self.nc.sbuf_tensor(name, list(shape), dt))
        return V(t[:], "S:" + name)

    def ps(self, name, shape, dt):
        t = self.st.enter_context(self.nc.psum_tensor(name, list(shape), dt))
        return V(t[:], "P:" + name)

    def dma(self, out, in_, q="sp", key=None, **kw):
        o, i = _ap(out), _ap(in_)
        self.P.op(q, lambda e: e.dma_start(out=o, in_=i, **kw), reads=_toks(in_), writes=_toks(out),
                  dma_key=key or out.tok)

    def mm(self, out, lhsT, rhs, start=True, stop=True, extra_reads=()):
        o, l, r = _ap(out), _ap(lhsT), _ap(rhs)
        self.P.op("pe", lambda e: e.matmul(o, l, r, start=start, stop=stop),
                  reads=_toks(lhsT, rhs) + list(extra_reads), writes=_toks(out))

    def tr(self, out, in_, ident):
        o, i, d = _ap(out), _ap(in_), _ap(ident)
        self.P.op("pe", lambda e: e.transpose(o, i, d), reads=_toks(in_, ident), writes=_toks(out))

    def act(self, out, in_, func, bias=None, scale=1.0, accum_out=None, eng="act"):
        o, i = _ap(out), _ap(in_)
        b = _ap(bias)
        s = _ap(scale)
        a = _ap(accum_out)
        kw = {}
        if b is not None:
            kw["bias"] = b
        if a is not None:
            kw["accum_out"] = a
        self.P.op("act", lambda e: e.activation(out=o, in_=i, func=func, scale=s, **kw),
                  reads=_toks(in_, bias, scale), writes=_toks(out, accum_out))

    def tt(self, out, in0, in1, op, eng="dve"):
        o, a, b = _ap(out), _ap(in0), _ap(in1)
        self.P.op(eng, lambda e: e.tensor_tensor(out=o, in0=a, in1=b, op=op),
                  reads=_toks(in0, in1), writes=_toks(out))

    def ts(self, out, in0, s1, op0, s2=None, op1=None, eng="dve", accum_out=None):
        o, a = _ap(out), _ap(in0)
        x1, x2 = _ap(s1), _ap(s2)
        acc = _ap(accum_out)
        kw = {}
        if op1 is not None:
            kw["op1"] = op1
        if acc is not None:
            kw["accum_out"] = acc
        self.P.op(eng, lambda e: e.tensor_scalar(out=o, in0=a, scalar1=x1, scalar2=x2, op0=op0, **kw),
                  reads=_toks(in0, s1, s2), writes=_toks(out, accum_out))

    def stt(self, out, in0, scalar, in1, op0, op1, eng="dve"):
        o, a, s, b = _ap(out), _ap(in0), _ap(scalar), _ap(in1)
        self.P.op(eng, lambda e: e.scalar_tensor_tensor(out=o, in0=a, scalar=s, in1=b, op0=op0, op1=op1),
                  reads=_toks(in0, scalar, in1), writes=_toks(out))

    def copy(self, out, in_, eng="dve"):
        o, i = _ap(out), _ap(in_)
        if eng == "act":
            self.P.op("act", lambda e: e.copy(out=o, in_=i), reads=_toks(in_), writes=_toks(out))
        else:
            self.P.op(eng, lambda e: e.tensor_copy(out=o, in_=i), reads=_toks(in_), writes=_toks(out))

    def recip(self, out, in_):
        o, i = _ap(out), _ap(in_)
        self.P.op("dve", lambda e: e.reciprocal(out=o, in_=i), reads=_toks(in_), writes=_toks(out))

    def memset(self, out, val, eng="pool"):
        o = _ap(out)
        self.P.op(eng, lambda e: e.memset(o, val), writes=_toks(out))

    def scan(self, out, d0, d1, init, op0=ALU.mult, op1=ALU.add):
        o, a, b, i = _ap(out), _ap(d0), _ap(d1), _ap(init)
        self.P.op("dve", lambda e: e.tensor_tensor_scan(out=o, data0=a, data1=b, initial=i, op0=op0, op1=op1),
                  reads=_toks(d0, d1, init), writes=_toks(out))

    def reduce(self, out, in_, op, axis=AX.X):
        o, i = _ap(out), _ap(in_)
        self.P.op("dve", lambda e: e.tensor_reduce(out=o, in_=i, axis=axis, op=op),
                  reads=_toks(in_), writes=_toks(out))

    def finish(self):
        self.P.emit()
        self.st.close()
        return self.nc


D = 1024
RMS_EPS = 1e-6
C_U, C_FQ, C_FK, C_FV, C_FF, C_CQ, C_CKV, C_KPE, C_DQ, C_DK, C_DV = (
    0, 256, 512, 768, 1024, 1028, 1220, 1348, 1380, 1636, 1892)
D_IN = 2148
WA_COLS = np.concatenate([np.arange(C_U, C_U + 256), np.arange(C_FK, C_FK + 256), np.arange(C_DK, C_DK + 256),
                          np.arange(C_CKV, C_CKV + 128), np.arange(C_KPE, C_KPE + 32),
                          np.arange(C_FV, C_FV + 256), np.arange(C_DV, C_DV + 256), np.arange(C_FF, C_FF + 4)])
NWA = 1444
R_U, R_FK, R_DK, R_CKV, R_KPE, R_KSW, NFEAT = 0, 256, 512, 768, 896, 928, 960


class Scr:
    pass


def alloc_norm_scratch(k, tag=""):
    s = Scr()
    s.junk = k.sb("njunk" + tag, [128, 1024], BF16)
    s.ssq = k.sb("nssq" + tag, [128, 4], F32)
    s.rstd = k.sb("nrstd" + tag, [128, 4], F32)
    s.xn = k.sb("nxn" + tag, [128, 4, 1024], BF16)
    return s


def norm_hT(k, xg, gt, hT, s, pst, ident, flip=0, raw_out=None):
    for b in range(4):
        k.act(s.junk.ap, xg[:, b, :], AF.Square, accum_out=s.ssq[:, b:b + 1])
    k.ts(s.rstd, s.ssq, 1.0 / 1024, ALU.mult, RMS_EPS, ALU.add)
    k.act(s.rstd, s.rstd, AF.Sqrt)
    k.recip(s.rstd, s.rstd)
    for b in range(4):
        k.ts(s.xn[:, b, :], xg[:, b, :], s.rstd[:, b:b + 1], ALU.mult, eng=("dve" if b % 2 == 0 else "pool"))
    for c in range(8):
        pt = pst[c % len(pst)]
        for b in range(4):
            k.tr(pt[:, b * 128:(b + 1) * 128], s.xn[:, b, c * 128:(c + 1) * 128], ident)
        if raw_out is not None:
            k.copy(raw_out[:, c, :], pt[:, 0:512], eng=("act" if (c + flip) % 2 == 0 else "dve"))
            k.ts(hT[:, c, :], raw_out[:, c, :], gt[:, c:c + 1], ALU.mult, eng="pool")
        elif (c + flip) % 2 == 0:
            k.act(hT[:, c, :], pt[:, 0:512], AF.Copy, scale=gt[:, c:c + 1])
        else:
            k.ts(hT[:, c, :], pt[:, 0:512], gt[:, c:c + 1], ALU.mult)


S96 = 96 ** -0.5
TWO_PI = 2.0 * math.pi
RC1 = 6.28125
RC2 = float(np.float32(TWO_PI - 6.28125))
RC3 = float(TWO_PI - 6.28125 - np.float64(np.float32(TWO_PI - 6.28125)))
PI_LO = 3.1415925


def range_reduce(k, out, in_, tmp_i, tmp_f):
    k.ts(tmp_f, in_, 1.0 / TWO_PI, ALU.mult, 0.5, ALU.add)
    k.copy(tmp_i, tmp_f)
    k.copy(tmp_f, tmp_i)
    k.stt(out, tmp_f, -RC1, in_, ALU.mult, ALU.add)
    k.stt(out, tmp_f, -RC2, out, ALU.mult, ALU.add)
    k.stt(out, tmp_f, -RC3, out, ALU.mult, ALU.add)
    k.ts(tmp_f, out, -math.pi, ALU.is_lt)
    k.stt(out, tmp_f, TWO_PI, out, ALU.mult, ALU.add)
    k.ts(tmp_f, out, math.pi, ALU.is_gt)
    k.stt(out, tmp_f, -TWO_PI, out, ALU.mult, ALU.add)
    k.ts(out, out, -PI_LO, ALU.max, PI_LO, ALU.min)


FR_U, FR_FK, FR_DK, FR_MK, NFR = 0, 256, 512, 768, 1152
TC_FV, TC_DV, TC_MV, NTC = 0, 256, 512, 768
NWB = 2180


def build_A(NG):
    T = NG * 512
    k = KB()
    x = k.dram("x", [T, D], F32, "ExternalInput")
    g = k.dram("g", [128, 8], F32, "ExternalInput")
    win = k.dram("win", [D, D_IN], F32, "ExternalInput")
    wuq = k.dram("wuq", [192, 384], F32, "ExternalInput")
    wukv = k.dram("wukv", [128, 512], F32, "ExternalInput")
    gq = k.dram("gq", [128, 2], F32, "ExternalInput")
    gkv = k.dram("gkv", [128, 1], F32, "ExternalInput")
    pos = k.dram("pos", [1, T], I32, "ExternalInput")
    cst = k.dram("cst", [128, 2], F32, "ExternalInput")
    idn = k.dram("identd", [128, 128], BF16, "ExternalInput")
    featT = k.dram("featT", [NFR, T], BF16, "ExternalOutput")
    tokM = k.dram("tokM", [T, NTC], BF16, "ExternalOutput")
    ffM = k.dram("ffM", [T, 4], F32, "ExternalOutput")
    qfT = k.dram("qfT", [256, T], BF16, "ExternalOutput")
    dqT = k.dram("dqT", [256, T], BF16, "ExternalOutput")
    qmT = k.dram("qmT", [384, T], BF16, "ExternalOutput")
    build_A_body(k, NG, x, g, win, wuq, wukv, gq, gkv, pos, cst, idn, featT, tokM, ffM, qfT, dqT, qmT)
    return k.finish()


def build_A_body(k, NG, x, g, win, wuq, wukv, gq, gkv, pos, cst, idn, featT, tokM, ffM, qfT, dqT, qmT):
    ident = k.sb("ident", [128, 128], BF16)
    ones = k.sb("ones", [128, 128], BF16)
    gt = k.sb("gt", [128, 8], F32)
    gqt = k.sb("gqt", [128, 2], F32)
    gkvt = k.sb("gkvt", [128, 1], F32)
    cstt = k.sb("cstt", [128, 2], F32)
    wb = k.sb("wb", [128, 8, NWB], BF16)
    k.dma(ident, idn)
    k.dma(gt, g)
    k.dma(gqt, gq)
    k.dma(gkvt, gkv)
    k.dma(cstt, cst)
    k.memset(ones, 1.0)
    xgs = [k.sb("xg%d" % i, [128, 4, 1024], F32) for i in range(2)]
    k.dma(xgs[0], x[0:512, :].re("(b p) f -> p b f", p=128))
    wst = [k.sb("wst%d" % i, [128, D_IN], F32) for i in range(2)]
    wbt = ["S:wb%d" % kc for kc in range(8)]
    for kc in range(8):
        src = wst[kc % 2]
        k.dma(src, win[kc * 128:(kc + 1) * 128, :], q="pool" if kc % 2 else "sp")
        dst = wb[:, kc, :].sub(wbt[kc])
        e = ("pool", "dve")[kc % 2]
        k.copy(dst[:, 0:D_IN], src, eng=e)
        k.ts(dst[:, D_IN:D_IN + 16], src[:, C_KPE + 16:C_KPE + 32], -1.0, ALU.mult, eng=e)
        k.copy(dst[:, D_IN + 16:D_IN + 32], src[:, C_KPE:C_KPE + 16], eng=e)
    wuq_s = k.sb("wuq_s", [128, 2, 384], F32)
    wukv_s = k.sb("wukv_s", [128, 512], F32)
    k.dma(wuq_s[:, 0, :], wuq[0:128, :])
    k.dma(wuq_s[0:64, 1, :], wuq[128:192, :])
    k.dma(wukv_s, wukv)
    wuq_b = k.sb("wuq_b", [128, 2, 384], BF16)
    wuqsw_b = k.sb("wuqsw_b", [128, 2, 384], BF16)
    wkn_b = k.sb("wkn_b", [128, 4, 64], BF16)
    wv_b = k.sb("wv_b", [128, 4, 64], BF16)
    k.copy(wuq_b[:, 0, :], wuq_s[:, 0, :])
    k.copy(wuq_b[0:64, 1, :], wuq_s[0:64, 1, :])
    k.copy(wuqsw_b[:, 0, :], wuq_s[:, 0, :])
    k.copy(wuqsw_b[0:64, 1, :], wuq_s[0:64, 1, :])
    for c, np_ in ((0, 128), (1, 64)):
        for h in range(4):
            b0 = h * 96 + 64
            k.ts(wuqsw_b[0:np_, c, b0:b0 + 16], wuq_s[0:np_, c, b0 + 16:b0 + 32], -1.0, ALU.mult)
            k.copy(wuqsw_b[0:np_, c, b0 + 16:b0 + 32], wuq_s[0:np_, c, b0:b0 + 16])
    wk4 = wukv_s.re("k (h two d) -> k h two d", h=4, two=2)
    k.copy(wkn_b, wk4[:, :, 0, :])
    k.copy(wv_b, wk4[:, :, 1, :])

    hTs = [k.sb("hT%d" % i, [128, 8, 512], BF16) for i in range(2)]
    scr = alloc_norm_scratch(k)
    pst = [k.ps("pst%d" % i, [128, 1024], BF16) for i in range(2)]
    pss = [k.ps("ps%d" % i, [128, 512], F32) for i in range(6)]
    fst = [k.sb("fst%d" % i, [128, 6, 512], BF16) for i in range(2)]
    qst = [k.sb("qst%d" % i, [128, 4, 512], BF16) for i in range(2)]
    tst = [k.sb("tst%d" % i, [128, 4, NTC], BF16) for i in range(2)]
    ffs = [k.sb("ffs%d" % i, [128, 4, 4], F32) for i in range(2)]
    mkn = [k.sb("mkn%d" % i, [64, 4, 512], BF16) for i in range(2)]
    rk = [k.sb("rk%d" % i, [96, 512], BF16) for i in range(2)]
    qms = [k.sb("qms%d" % i, [96, 4, 512], BF16) for i in range(2)]
    posi = k.sb("posi", [96, 512], I32)
    posf = k.sb("posf", [96, 512], F32)
    ang = k.sb("ang", [96, 512], F32)
    ang2 = k.sb("ang2", [96, 512], F32)
    rtf = k.sb("rtf", [96, 512], F32)
    rti = k.sb("rti", [96, 512], I32)
    Ct = k.sb("Ct", [96, 512], F32)
    St = k.sb("St", [96, 512], F32)
    kpe_s = k.sb("kpe_s", [96, 512], F32)
    ksw_s = k.sb("ksw_s", [96, 512], F32)
    rt1 = k.sb("rt1", [96, 512], F32)
    rt2 = k.sb("rt2", [96, 512], F32)
    ckv_s = k.sb("ckv_s", [128, 512], F32)
    cq_s = k.sb("cq_s", [128, 2, 512], F32)
    sq = k.sb("sq", [128, 2, 512], BF16)
    rstd = k.sb("rstd", [128, 512], F32)
    ckvn = k.sb("ckvn", [128, 512], BF16)
    cqn = k.sb("cqn", [128, 2, 512], BF16)
    R = slice(64, 96)
    pc = [0]

    def nps():
        p_ = pss[pc[0] % 6]
        pc[0] += 1
        return p_

    def proj(c0, m, hT):
        ps = nps()
        for kc in range(8):
            k.mm(ps[0:m, :], wb[:, kc, c0:c0 + m].sub(wbt[kc]), hT[:, kc, :], start=(kc == 0), stop=(kc == 7))
        return ps

    for gi in range(NG):
        xg = xgs[gi % 2]
        if gi + 1 < NG:
            k.dma(xgs[(gi + 1) % 2], x[(gi + 1) * 512:(gi + 2) * 512, :].re("(b p) f -> p b f", p=128))
        hT = hTs[gi % 2]
        norm_hT(k, xg, gt, hT, scr, pst, ident)
        cols = slice(gi * 512, (gi + 1) * 512)
        fs, qs, tsb, ff = fst[gi % 2], qst[gi % 2], tst[gi % 2], ffs[gi % 2]
        k.dma(posi[R, :], pos[0:1, cols].re("o t -> (o) t").ap.partition_broadcast(32)
              if False else V(pos.ap[0:1, cols].partition_broadcast(32), pos.tok))
        k.copy(posf[R, :], posi[R, :])
        k.ts(ang[R, :], posf[R, :], cstt[R, 0:1], ALU.mult)
        k.ts(ang2[R, :], ang[R, :], math.pi / 2, ALU.add)
        range_reduce(k, ang[R, :], ang[R, :], rti[R, :], rtf[R, :])
        range_reduce(k, ang2[R, :], ang2[R, :], rti[R, :], rtf[R, :])
        k.act(St[R, :], ang[R, :], AF.Sin)
        k.act(Ct[R, :], ang2[R, :], AF.Sin)
        for ci, c0 in enumerate((C_U, C_U + 128, C_FK, C_FK + 128, C_DK, C_DK + 128)):
            ps = proj(c0, 128, hT)
            k.copy(fs[:, ci, :], ps, eng=("act" if ci % 2 else "dve"))
        k.dma(featT[0:768, cols].re("(c p) t -> p c t", p=128), fs, key="featT")
        for ci, c0 in enumerate((C_FQ, C_FQ + 128, C_DQ, C_DQ + 128)):
            ps = proj(c0, 128, hT)
            if ci % 2:
                k.act(qs[:, ci, :], ps, AF.Copy, scale=0.125)
            else:
                k.ts(qs[:, ci, :], ps, 0.125, ALU.mult)
        k.dma(qfT[:, cols].re("(c p) t -> p c t", p=128), qs[:, 0:2, :], key="qfT")
        k.dma(dqT[:, cols].re("(c p) t -> p c t", p=128), qs[:, 2:4, :], key="dqT")
        for tb in range(4):
            for (c0, n, o0) in ((C_FV, 256, TC_FV), (C_DV, 256, TC_DV)):
                ps = nps()
                for kc in range(8):
                    k.mm(ps[:, 0:n], hT[:, kc, tb * 128:(tb + 1) * 128], wb[:, kc, c0:c0 + n].sub(wbt[kc]),
                         start=(kc == 0), stop=(kc == 7))
                k.copy(tsb[:, tb, o0:o0 + n], ps[:, 0:n], eng=("act" if tb % 2 else "dve"))
            ps = nps()
            for kc in range(8):
                k.mm(ps[:, 0:4], hT[:, kc, tb * 128:(tb + 1) * 128], wb[:, kc, C_FF:C_FF + 4].sub(wbt[kc]),
                     start=(kc == 0), stop=(kc == 7))
            k.copy(ff[:, tb, :], ps[:, 0:4])
        k.dma(ffM[cols, :].re("(b p) c -> p b c", p=128), ff, key="ffM")
        ps = proj(C_CKV, 128, hT)
        k.copy(ckv_s, ps, eng="act")
        k.act(sq[:, 0, :], ckv_s, AF.Square)
        ps = nps()
        k.mm(ps, ones, sq[:, 0, :])
        k.ts(rstd, ps, 1.0 / 128, ALU.mult, RMS_EPS, ALU.add)
        k.act(rstd, rstd, AF.Sqrt)
        k.recip(rstd, rstd)
        k.stt(ckvn, ckv_s, gkvt[:, 0:1], rstd, ALU.mult, ALU.mult)
        mk = mkn[gi % 2]
        for h in range(4):
            ps = nps()
            k.mm(ps[0:64, :], wkn_b[:, h, :], ckvn)
            k.copy(mk[:, h, :], ps[0:64, :], eng=("act" if h % 2 else "dve"))
        for tb in range(4):
            ps = nps()
            k.mm(ps[:, 0:256], ckvn[:, tb * 128:(tb + 1) * 128], wv_b.re("k h d -> k (h d)"))
            k.copy(tsb[:, tb, TC_MV:TC_MV + 256], ps[:, 0:256], eng=("act" if tb % 2 else "dve"))
        k.dma(tokM[cols, :].re("(b p) c -> p b c", p=128), tsb, key="tokM")
        psA = proj(C_KPE - 64, 96, hT)
        psB = proj(D_IN - 64, 96, hT)
        k.tt(rt1[R, :], psA[R, :], Ct[R, :], ALU.mult)
        k.tt(rt2[R, :], psB[R, :], St[R, :], ALU.mult)
        rkk = rk[gi % 2]
        k.tt(rkk[R, :], rt1[R, :], rt2[R, :], ALU.add)
        for h in range(4):
            r0 = FR_MK + h * 96
            k.dma(featT[r0:r0 + 64, cols], mk[:, h, :], key="featT")
            k.dma(featT[r0 + 64:r0 + 96, cols], rkk[R, :], key="featT")
        ps0 = proj(C_CQ, 128, hT)
        ps1 = proj(C_CQ + 128, 64, hT)
        k.copy(cq_s[:, 0, :], ps0, eng="act")
        k.copy(cq_s[0:64, 1, :], ps1[0:64, :], eng="act")
        k.act(sq[:, 0, :], cq_s[:, 0, :], AF.Square)
        k.act(sq[0:64, 1, :], cq_s[0:64, 1, :], AF.Square)
        ps = nps()
        k.mm(ps, ones, sq[:, 0, :], start=True, stop=False)
        k.mm(ps, ones[0:64, :], sq[0:64, 1, :], start=False, stop=True)
        k.ts(rstd, ps, 1.0 / 192, ALU.mult, RMS_EPS, ALU.add)
        k.act(rstd, rstd, AF.Sqrt)
        k.recip(rstd, rstd)
        k.stt(cqn[:, 0, :], cq_s[:, 0, :], gqt[:, 0:1], rstd, ALU.mult, ALU.mult)
        k.stt(cqn[0:64, 1, :], cq_s[0:64, 1, :], gqt[0:64, 1:2], rstd[0:64, :], ALU.mult, ALU.mult)
        qm = qms[gi % 2]
        for h in range(4):
            pa = nps()
            k.mm(pa[0:96, :], wuq_b[:, 0, h * 96:(h + 1) * 96], cqn[:, 0, :], start=True, stop=False)
            k.mm(pa[0:96, :], wuq_b[0:64, 1, h * 96:(h + 1) * 96], cqn[0:64, 1, :], start=False, stop=True)
            pb = nps()
            k.mm(pb[0:96, :], wuqsw_b[:, 0, h * 96:(h + 1) * 96], cqn[:, 0, :], start=True, stop=False)
            k.mm(pb[0:96, :], wuqsw_b[0:64, 1, h * 96:(h + 1) * 96], cqn[0:64, 1, :], start=False, stop=True)
            k.ts(qm[0:64, h, :], pa[0:64, :], S96, ALU.mult)
            k.stt(rt1[R, :], pa[R, :], S96, Ct[R, :], ALU.mult, ALU.mult)
            k.stt(rt2[R, :], pb[R, :], S96, St[R, :], ALU.mult, ALU.mult)
            k.tt(qm[R, h, :], rt1[R, :], rt2[R, :], ALU.add)
        k.dma(qmT[:, cols].re("(h r) t -> r h t", r=96), qm, key="qmT")


INV_FREQ = (10000.0 ** (-np.arange(0, 32, 2, dtype=np.float32) / 32)).astype(np.float32)
IDENT = np.eye(128, dtype=np.float32).astype(ml_dtypes.bfloat16)


def host_A_inputs(x_own, g, w_in, wuq, wukv, gq, gkv, pos_own):
    cst = np.zeros((128, 2), np.float32)
    cst[64:80, 0] = INV_FREQ
    cst[80:96, 0] = INV_FREQ
    gqp = np.zeros((256,), np.float32)
    gqp[:192] = gq
    return {"x": np.ascontiguousarray(x_own, dtype=np.float32),
            "g": np.ascontiguousarray(g.reshape(8, 128).T),
            "win": np.ascontiguousarray(w_in), "wuq": np.ascontiguousarray(wuq),
            "wukv": np.ascontiguousarray(wukv),
            "gq": np.ascontiguousarray(gqp.reshape(2, 128).T), "gkv": np.ascontiguousarray(gkv.reshape(128, 1)),
            "pos": np.ascontiguousarray(pos_own.reshape(1, -1).astype(np.int32)),
            "cst": cst, "identd": IDENT}


def attn_head(k, nkb, kt_fn, qt, v_fn, bias_fn, mask_fn, pss, pts, ones_b, yrow_out, rec_t, rows):
    O, S = pss[3], pss[4]
    scb = (0, 1, 2, 5)
    LAG = 3
    pend = []

    def drain(last):
        pi, pp = pend.pop(0)
        k.mm(O, v_fn(pi), pp, start=(pi == 0), stop=last)
        k.mm(S, ones_b, pp, start=(pi == 0), stop=last)

    for idx in range(nkb):
        sc = pss[scb[idx % 4]]
        m = mask_fn(idx)
        k.mm(sc, kt_fn(idx), qt, start=True, stop=(m is None))
        if m is not None:
            k.mm(sc, m[0], m[1], start=False, stop=True)
        pt = pts[idx % len(pts)]
        b = bias_fn(idx)
        if b is None:
            k.act(pt, sc, AF.Exp)
        else:
            k.act(pt, sc, AF.Exp, bias=b)
        pend.append((idx, pt))
        if len(pend) > LAG:
            drain(False)
    while pend:
        drain(len(pend) == 1)
    k.recip(rec_t[rows, :], S[rows, :])
    k.tt(yrow_out, O[rows, :], rec_t[rows, :], ALU.mult)


def fox_phase(k, NG, C, kfeat, ktok, kff, qfT, yT_out):
    NS, NK = 2 * NG, 2 * NG * 512
    NKB = NK // 128
    T = NG * 512
    KT = [k.sb("fKT%d" % h, [67, NK], BF16) for h in range(4)]
    QT = [k.sb("fQT%d" % h, [67, T], BF16) for h in range(4)]
    Vt = k.sb("fV", [128, NKB, 256], BF16)
    for h in range(4):
        k.dma(KT[h][0:64, :], kfeat[FR_FK + h * 64:FR_FK + (h + 1) * 64, :])
        k.memset(KT[h][64:67, :], 1.0, eng="dve")
        k.dma(QT[h][0:64, :], qfT[h * 64:(h + 1) * 64, :])
    k.dma(Vt, ktok[:, TC_FV:TC_FV + 256].re("(kb p) c -> p kb c", p=128))
    ff = k.sb("f_ff", [128, NKB, 4], F32)
    t1 = k.sb("f_t1", [128, NKB, 4], F32)
    t2 = k.sb("f_t2", [128, NKB, 4], F32)
    lf = k.sb("f_lf", [128, NKB, 4], F32)
    cum = k.sb("f_cum", [128, NKB, 4], F32)
    nb = k.sb("f_nb", [128, NKB, 4], F32)
    k.dma(ff, kff.re("(kb p) h -> p kb h", p=128))
    for h in range(4):
        k.ts(ff[:, :, h], ff[:, :, h], C.bft[:, h:h + 1], ALU.add)
    k.ts(t1, ff, -1.0, ALU.mult)
    k.tt(t1, t1, ff, ALU.max)
    k.act(t1, t1, AF.Exp, scale=-1.0)
    k.act(t1, t1, AF.Ln, bias=C.one_col[:, 0:1])
    k.ts(t2, ff, 0.0, ALU.min)
    k.tt(lf, t2, t1, ALU.subtract)
    lf2 = lf.re("p kb h -> p (kb h)")
    pw, ptot = C.pss[0], C.pss[1]
    k.mm(pw[:, 0:NKB * 4], C.tri32, lf2)
    k.mm(ptot[:, 0:NKB * 4], C.ones32, lf2)
    k.copy(t1.re("p kb h -> p (kb h)"), ptot[:, 0:NKB * 4])
    for h in range(4):
        k.scan(t2[:, :, h], C.onesrow[:, 0:NKB], t1[:, :, h], 0.0)
    k.tt(t2, t2, t1, ALU.subtract)
    k.tt(cum.re("p kb h -> p (kb h)"), pw[:, 0:NKB * 4], t2.re("p kb h -> p (kb h)"), ALU.add)
    k.ts(nb, cum, -1.0, ALU.mult)
    for h in range(4):
        k.ts(nb[:, 0:4, h], nb[:, 0:4, h], C.padb[:, 0:1], ALU.add)
    cq = k.sb("f_cq", [4, 512], F32)
    cqh = k.sb("f_cqh", [4, 512], BF16)
    cqm = k.sb("f_cqm", [4, 512], BF16)
    cql = k.sb("f_cql", [4, 512], BF16)
    cqf = k.sb("f_cqf", [4, 512], F32)
    for gi in range(NG):
        pq = C.pss[2 + gi % 2]
        for j in range(4):
            kb = (2 * gi + 1) * 4 + j
            k.tr(pq[0:4, j * 128:(j + 1) * 128], cum[:, kb, :], C.ident32)
        k.copy(cq, pq[0:4, :])
        k.copy(cqh, cq)
        k.copy(cqf, cqh)
        k.tt(cq, cq, cqf, ALU.subtract)
        k.copy(cqm, cq)
        k.copy(cqf, cqm)
        k.tt(cq, cq, cqf, ALU.subtract)
        k.copy(cql, cq)
        cols = slice(gi * 512, (gi + 1) * 512)
        for h in range(4):
            k.dma(QT[h][64:65, cols], cqh[h:h + 1, :], key="fq%d" % h)
            k.dma(QT[h][65:66, cols], cqm[h:h + 1, :], key="fq%d" % h)
            k.dma(QT[h][66:67, cols], cql[h:h + 1, :], key="fq%d" % h)
    for gi in range(NG):
        nkb = (2 * gi + 2) * 4
        cols = slice(gi * 512, (gi + 1) * 512)
        yst = C.yst[gi % 2]
        for h in range(4):
            rows = slice((h % 2) * 64, (h % 2) * 64 + 64)
            pair = h // 2
            attn_head(k, nkb,
                      lambda kb, h=h: KT[h][:, kb * 128:(kb + 1) * 128],
                      QT[h][:, cols],
                      lambda kb, pair=pair: Vt[:, kb, pair * 128:(pair + 1) * 128],
                      lambda kb, h=h: nb[:, kb, h:h + 1],
                      lambda kb, nkb=nkb: ((C.identb, C.mfox[:, kb - (nkb - 4), :]) if kb >= nkb - 4 else None),
                      C.pss, C.pts, C.onesb, yst[rows, pair, :], C.rec, rows)
        k.dma(yT_out[:, cols].re("(c p) t -> p c t", p=128), yst, key=yT_out.tok)


def mla_phase(k, NG, C, kfeat, ktok, qmT, yT_out):
    NS, NK = 2 * NG, 2 * NG * 512
    NKB = NK // 128
    T = NG * 512
    KT = [k.sb("mKT%d" % h, [96, NK], BF16) for h in range(4)]
    QT = [k.sb("mQT%d" % h, [96, T], BF16) for h in range(4)]
    Vt = k.sb("mV", [128, NKB, 256], BF16)
    for h in range(4):
        k.dma(KT[h], kfeat[FR_MK + h * 96:FR_MK + (h + 1) * 96, :])
        k.dma(QT[h], qmT[h * 96:(h + 1) * 96, :])
    k.dma(Vt, ktok[:, TC_MV:TC_MV + 256].re("(kb p) c -> p kb c", p=128))
    for gi in range(NG):
        nkb = (2 * gi + 2) * 4
        cols = slice(gi * 512, (gi + 1) * 512)
        yst = C.yst[gi % 2]
        for h in range(4):
            rows = slice((h % 2) * 64, (h % 2) * 64 + 64)
            pair = h // 2
            attn_head(k, nkb,
                      lambda kb, h=h: KT[h][:, kb * 128:(kb + 1) * 128],
                      QT[h][:, cols],
                      lambda kb, pair=pair: Vt[:, kb, pair * 128:(pair + 1) * 128],
                      lambda kb: (C.padb[:, 0:1] if kb < 4 else None),
                      lambda kb, nkb=nkb: ((C.identb, C.mmla[:, kb - (nkb - 4), :]) if kb >= nkb - 4 else None),
                      C.pss, C.pts, C.onesb, yst[rows, pair, :], C.rec, rows)
        k.dma(yT_out[:, cols].re("(c p) t -> p c t", p=128), yst, key=yT_out.tok)


class Cm:
    pass


def common_B(k, cons):
    C = Cm()
    C.identb = k.sb("identb", [128, 128], BF16)
    C.antib = k.sb("antib", [128, 128], BF16)
    C.onesb = k.sb("onesb", [128, 128], BF16)
    C.ident32 = k.sb("ident32", [128, 128], F32)
    C.tri32 = k.sb("tri32", [128, 128], F32)
    C.ones32 = k.sb("ones32", [128, 128], F32)
    C.onesrow = k.sb("onesrow", [128, 512], F32)
    C.one_col = k.sb("one_col", [128, 1], F32)
    C.padb = k.sb("padb", [128, 1], F32)
    C.bft = k.sb("bft", [128, 4], F32)
    k.dma(C.identb, cons["identd"])
    k.dma(C.antib, cons["antid"])
    k.dma(C.ident32, cons["ident32d"])
    k.dma(C.tri32, cons["tri32d"])
    k.dma(C.padb, cons["padbd"])
    k.dma(C.bft, cons["bfd"])
    k.memset(C.onesb, 1.0)
    k.memset(C.ones32, 1.0)
    k.memset(C.onesrow, 1.0)
    k.memset(C.one_col, 1.0)
    C.pss = [k.ps("ps%d" % i, [128, 512], F32) for i in range(6)]
    C.pst = [k.ps("pst%d" % i, [128, 1024], BF16) for i in range(2)]
    return C


def att_common(k, C, cons):
    C.mfox = k.sb("mfox", [128, 4, 512], BF16)
    C.mmla = k.sb("mmla", [128, 4, 512], BF16)
    k.dma(C.mfox, cons["mfoxd"])
    k.dma(C.mmla, cons["mmlad"])
    C.pts = [k.sb("pT%d" % i, [128, 512], BF16) for i in range(4)]
    C.yst = [k.sb("yst%d" % i, [128, 2, 512], BF16) for i in range(2)]
    C.rec = k.sb("rec", [128, 512], F32)


def host_masks():
    kk = np.arange(128)[:, None, None]
    j = np.arange(4)[None, :, None]
    q = np.arange(512)[None, None, :]
    kidx = j * 128 + kk
    mfox = np.where(kidx <= q, 0.0, NEG).astype(np.float32)
    mmla = np.where(kidx // 64 <= q // 64, 0.0, NEG).astype(np.float32)
    return mfox.astype(ml_dtypes.bfloat16), mmla.astype(ml_dtypes.bfloat16)


ANTI = np.ascontiguousarray(np.eye(128, dtype=np.float32)[::-1]).astype(ml_dtypes.bfloat16)
IDENT32 = np.eye(128, dtype=np.float32)
TRI32 = np.triu(np.ones((128, 128), np.float32))
MFOX, MMLA = host_masks()


def slot_tiles(par, NG):
    return [-1] + list(range(2 * NG - 1)) if par == 0 else list(range(2 * NG))


def to_slots(arr_tiles, par, NG, axis):
    a = np.moveaxis(arr_tiles, axis, 0)
    a = a.reshape((2 * NG, 512) + a.shape[1:])
    out = np.zeros_like(a)
    for s, t in enumerate(slot_tiles(par, NG)):
        if t >= 0:
            out[s] = a[t]
    out = out.reshape((2 * NG * 512,) + a.shape[2:])
    return np.ascontiguousarray(np.moveaxis(out, 0, axis))


def build_B_attn_test(NG):
    T, NK = NG * 512, 2 * NG * 512
    k = KB()
    ins = {}
    for name, shape, dt in (("kfeat", [NFR, NK], BF16), ("ktok", [NK, NTC], BF16), ("kff", [NK, 4], F32),
                            ("qfT", [256, T], BF16), ("qmT", [384, T], BF16),
                            ("identd", [128, 128], BF16), ("antid", [128, 128], BF16),
                            ("ident32d", [128, 128], F32), ("tri32d", [128, 128], F32),
                            ("padbd", [128, 1], F32), ("bfd", [128, 4], F32),
                            ("mfoxd", [128, 4, 512], BF16), ("mmlad", [128, 4, 512], BF16)):
        ins[name] = k.dram(name, shape, dt, "ExternalInput")
    yB = k.dram("yB", [256, T], BF16, "ExternalOutput")
    yC = k.dram("yC", [256, T], BF16, "ExternalOutput")
    C = common_B(k, ins)
    att_common(k, C, ins)
    fox_phase(k, NG, C, ins["kfeat"], ins["ktok"], ins["kff"], ins["qfT"], yB)
    mla_phase(k, NG, C, ins["kfeat"], ins["ktok"], ins["qmT"], yC)
    return k.finish()


def host_chunk_mask():
    kkp = np.arange(128)[:, None, None]
    j = np.arange(8)[None, :, None]
    q = np.arange(512)[None, None, :]
    kidx = j * 128 + (127 - kkp)
    dc = (q + 512) // 64 - kidx // 64
    return np.where((dc >= 0) & (dc <= 8), 0.0, NEG).astype(np.float32)


MCHK = host_chunk_mask()
JROW = np.tile(np.arange(512, dtype=np.float32)[None], (128, 1))


def chunk_phase(k, NG, C, kfeat, ktok, dqT, relb, mchkd, Ed, yT_out):
    T = NG * 512
    rb = k.sb("c_rb", [4, 513], F32)
    E = k.sb("c_E", [4, 1536], F32)
    k.dma(rb, relb)
    k.copy(E[:, 255:768], rb)
    k.ts(E[:, 0:255], C.onesrow[0:4, 0:255], rb[:, 0:1], ALU.mult)
    k.ts(E[:, 768:1536], C.onesrow[0:4, 0:256].ap.unsqueeze(1).to_broadcast([4, 3, 256]) if False else
         V(C.onesrow.ap[0:4, 0:256], C.onesrow.tok), rb[:, 512:513], ALU.mult) if False else None
    for j3 in range(3):
        k.ts(E[:, 768 + j3 * 256:768 + (j3 + 1) * 256], C.onesrow[0:4, 0:256], rb[:, 512:513], ALU.mult)
    k.dma(Ed, E)
    Tb = k.sb("c_Tb", [128, 4, 8, 512], BF16)
    mc = k.sb("c_mc", [128, 8, 512], F32)
    k.dma(mc, mchkd)
    tfs = [k.sb("c_tf%d" % i, [128, 512], F32) for i in range(2)]
    n = 0
    for h in range(4):
        for j in range(8):
            tf = tfs[n % 2]
            src = bass.AP(Ed.ap.tensor, h * 1536 + 896 - 128 * j, [[1, 128], [1, 512]])
            k.dma(tf, V(src, Ed.tok), q=("sp", "pool")[n % 2])
            k.tt(Tb[:, h, j, :], tf, mc[:, j, :], ALU.add, eng=("dve", "pool")[n % 2])
            n += 1
    KD = [k.sb("c_KD%d" % i, [128, 2, 1024], BF16) for i in range(2)]
    QD = [k.sb("c_QD%d" % i, [128, 2, 512], BF16) for i in range(2)]
    VD = [k.sb("c_VD%d" % i, [128, 8, 256], BF16) for i in range(2)]
    for gi in range(NG):
        cols = slice(gi * 512, (gi + 1) * 512)
        kc = slice(2 * gi * 512, (2 * gi + 2) * 512)
        kd, qd, vd = KD[gi % 2], QD[gi % 2], VD[gi % 2]
        k.dma(kd, kfeat[FR_DK:FR_DK + 256, kc].re("(c p) t -> p c t", p=128))
        k.dma(qd, dqT[:, cols].re("(c p) t -> p c t", p=128), q="pool")
        k.dma(vd, ktok[kc, TC_DV:TC_DV + 256].re("(kb p) c -> p kb c", p=128))
        yst = C.yst[gi % 2]
        for h in range(4):
            rows = slice((h % 2) * 64, (h % 2) * 64 + 64)
            pair = h // 2
            attn_head(k, 8,
                      lambda kb, pair=pair, rows=rows: kd[rows, pair, kb * 128:(kb + 1) * 128],
                      qd[rows, pair, :],
                      lambda kb, pair=pair: vd[:, kb, pair * 128:(pair + 1) * 128],
                      lambda kb, gi=gi: (C.padb[:, 0:1] if (gi == 0 and kb < 4) else None),
                      lambda kb, h=h: (C.antib, Tb[:, h, kb, :]),
                      C.pss, C.pts, C.onesb, yst[rows, pair, :], C.rec, rows)
        k.dma(yT_out[:, cols].re("(c p) t -> p c t", p=128), yst, key=yT_out.tok)


GELU_C = 2.0 * math.sqrt(2.0 / math.pi)


def s5_phase(k, NG, C, kfeat, sp, yT_out):
    NS = 2 * NG
    f32t = lambda n, sh: k.sb("s_" + n, sh, F32)
    X = {}
    for nm in ("arX", "aiX", "ldtX", "brX", "biX"):
        X[nm] = f32t(nm, [128, 128])
        k.dma(X[nm], sp[nm])
    dt = f32t("dt", [128, 128])
    mag = f32t("mag", [128, 128])
    th = f32t("th", [128, 128])
    th2 = f32t("th2", [128, 128])
    tf = f32t("tf", [128, 128])
    ti = k.sb("s_ti", [128, 128], I32)
    sn = f32t("sn", [128, 128])
    cs = f32t("cs", [128, 128])
    a1 = f32t("a1", [128, 128])
    a2 = f32t("a2", [128, 128])
    a3 = f32t("a3", [128, 128])
    a4 = f32t("a4", [128, 128])
    bbr = f32t("bbr", [128, 128])
    bbi = f32t("bbi", [128, 128])
    k.act(dt, X["ldtX"], AF.Exp)
    k.tt(mag, X["arX"], dt, ALU.mult)
    k.act(mag, mag, AF.Exp)
    k.tt(th, X["aiX"], dt, ALU.mult)
    k.ts(th2, th, math.pi / 2, ALU.add)
    range_reduce(k, th, th, ti, tf)
    range_reduce(k, th2, th2, ti, tf)
    k.act(sn, th, AF.Sin)
    k.act(cs, th2, AF.Sin)
    k.tt(a1, mag, cs, ALU.mult)
    k.tt(a2, mag, sn, ALU.mult)
    k.ts(a1, a1, -1.0, ALU.add)
    k.tt(a3, X["arX"], X["arX"], ALU.mult)
    k.tt(a4, X["aiX"], X["aiX"], ALU.mult)
    k.tt(a3, a3, a4, ALU.add)
    k.recip(a3, a3)
    k.tt(a4, a1, X["arX"], ALU.mult)
    k.tt(tf, a2, X["aiX"], ALU.mult)
    k.tt(a4, a4, tf, ALU.add)
    k.tt(a4, a4, a3, ALU.mult)
    k.tt(tf, a2, X["arX"], ALU.mult)
    k.tt(a1, a1, X["aiX"], ALU.mult)
    k.tt(tf, tf, a1, ALU.subtract)
    k.tt(tf, tf, a3, ALU.mult)
    k.tt(bbr, a4, X["brX"], ALU.mult)
    k.tt(a1, tf, X["biX"], ALU.mult)
    k.tt(bbr, bbr, a1, ALU.subtract)
    k.tt(bbi, a4, X["biX"], ALU.mult)
    k.tt(a1, tf, X["brX"], ALU.mult)
    k.tt(bbi, bbi, a1, ALU.add)
    mB = f32t("mB", [128, 4, 128])
    k.dma(mB, sp["maskB"])
    cdr = f32t("cdr", [128, 2, 128])
    cdi = f32t("cdi", [128, 2, 128])
    k.dma(cdr, sp["CdupR"])
    k.dma(cdi, sp["CdupI"])
    mCt = f32t("mC", [128, 4, 128])
    k.dma(mCt, sp["maskC"])
    LB = k.sb("s_LB", [128, 8, 2, 128], BF16)
    LC = k.sb("s_LC", [128, 8, 2, 128], BF16)
    for gp in range(8):
        c, kq = gp // 4, gp % 4
        for ri, src in enumerate((bbr, bbi)):
            for g2 in range(2):
                k.tt(LB[:, gp, ri, g2 * 64:(g2 + 1) * 64], src[:, c * 64:(c + 1) * 64],
                     mB[:, kq, g2 * 64:(g2 + 1) * 64], ALU.mult, eng=("dve", "pool")[g2])
        k.tt(LC[:, gp, 0, :], cdr[:, c, :], mCt[:, kq, :], ALU.mult)
        k.stt(LC[:, gp, 1, :], cdi[:, c, :], -1.0, mCt[:, kq, :], ALU.mult, ALU.mult)
    Y = {}
    for nm in ("arY", "aiY", "ldtY"):
        Y[nm] = f32t(nm, [128, 8])
        k.dma(Y[nm], sp[nm])
    dtY = f32t("dtY", [128, 8])
    rho = f32t("rho", [128, 8])
    thY = f32t("thY", [128, 8])
    k.act(dtY, Y["ldtY"], AF.Exp)
    k.tt(rho, Y["arY"], dtY, ALU.mult)
    k.act(rho, rho, AF.Exp)
    k.tt(thY, Y["aiY"], dtY, ALU.mult)
    t5 = f32t("t5", [128, 8])
    t5b = f32t("t5b", [128, 8])
    t5f = f32t("t5f", [128, 8])
    t5i = k.sb("s_t5i", [128, 8], I32)
    s512 = f32t("s512", [128, 8])
    c512 = f32t("c512", [128, 8])
    k.ts(t5, thY, 512.0, ALU.mult)
    k.ts(t5b, t5, math.pi / 2, ALU.add)
    range_reduce(k, t5, t5, t5i, t5f)
    range_reduce(k, t5b, t5b, t5i, t5f)
    k.act(s512, t5, AF.Sin)
    k.act(c512, t5b, AF.Sin)
    jrow = f32t("jrow", [128, 512])
    k.dma(jrow, sp["jrow"])
    cosT = f32t("cosT", [128, 8, 512])
    sinT = f32t("sinT", [128, 8, 512])
    rhoT = f32t("rhoT", [128, 8, 512])
    ag = f32t("ag", [128, 512])
    ag2 = f32t("ag2", [128, 512])
    agf = f32t("agf", [128, 512])
    agi = k.sb("s_agi", [128, 512], I32)
    for gp in range(8):
        k.ts(ag, jrow, thY[:, gp:gp + 1], ALU.mult)
        k.ts(ag2, ag, math.pi / 2, ALU.add)
        range_reduce(k, ag, ag, agi, agf)
        range_reduce(k, ag2, ag2, agi, agf)
        k.act(sinT[:, gp, :], ag, AF.Sin)
        k.act(cosT[:, gp, :], ag2, AF.Sin)
        k.ts(rhoT[:, gp, :], C.onesrow, rho[:, gp:gp + 1], ALU.mult, eng="pool")
    dX = f32t("dX", [128, 2])
    k.dma(dX, sp["dX"])
    wg_s = f32t("wg_s", [128, 2, 256])
    k.dma(wg_s, sp["wglu"].re("(c p) f -> p c f", p=128))
    wg_b = k.sb("s_wg_b", [128, 2, 256], BF16)
    k.copy(wg_b, wg_s)
    Wre = f32t("Wre", [128, 8, 512])
    Wim = f32t("Wim", [128, 8, 512])
    ini_re = f32t("ini_re", [128, 8])
    ini_im = f32t("ini_im", [128, 8])
    k.memset(ini_re, 0.0)
    k.memset(ini_im, 0.0)
    c1 = f32t("c1", [128, 8])
    c2 = f32t("c2", [128, 8])
    uTs = [k.sb("s_uT%d" % i, [128, 2, 512], BF16) for i in range(2)]
    prs = [f32t("prs%d" % i, [128, 512]) for i in range(2)]
    pis = [f32t("pis%d" % i, [128, 512]) for i in range(2)]
    q1 = [f32t("q1_%d" % i, [128, 512]) for i in range(2)]
    q2 = [f32t("q2_%d" % i, [128, 512]) for i in range(2)]
    q3, q4 = q1, q2
    are = [f32t("are%d" % i, [128, 512]) for i in range(2)]
    aim = [f32t("aim%d" % i, [128, 512]) for i in range(2)]
    xr = [k.sb("s_xr%d" % i, [128, 512], BF16) for i in range(2)]
    xi = [k.sb("s_xi%d" % i, [128, 512], BF16) for i in range(2)]
    yv = f32t("yv", [128, 2, 512])
    y2 = f32t("y2", [128, 512])
    ygT = k.sb("s_ygT", [128, 2, 512], BF16)
    sg = f32t("sg", [128, 512])
    pss = C.pss
    for s in range(NS):
        uT = uTs[s % 2]
        k.dma(uT, kfeat[FR_U:FR_U + 256, s * 512:(s + 1) * 512].re("(c p) t -> p c t", p=128))
        own = (s % 2 == 1)
        for gp in range(8):
            c, kq = gp // 4, gp % 4
            b = gp % 2
            pa, pb = pss[(2 * gp) % 4], pss[(2 * gp + 1) % 4]
            k.mm(pa, LB[:, gp, 0, :], uT[:, c, :])
            k.mm(pb, LB[:, gp, 1, :], uT[:, c, :])
            k.copy(prs[b], pa, eng="act")
            k.copy(pis[b], pb, eng="act")
            k.tt(q1[b], prs[b], cosT[:, gp, :], ALU.mult)
            k.tt(q2[b], pis[b], sinT[:, gp, :], ALU.mult, eng="pool")
            k.tt(are[b], q1[b], q2[b], ALU.add)
            k.tt(q3[b], pis[b], cosT[:, gp, :], ALU.mult)
            k.tt(q4[b], prs[b], sinT[:, gp, :], ALU.mult, eng="pool")
            k.tt(aim[b], q3[b], q4[b], ALU.subtract)
            k.scan(Wre[:, gp, :], rhoT[:, gp, :], are[b], ini_re[:, gp:gp + 1])
            k.scan(Wim[:, gp, :], rhoT[:, gp, :], aim[b], ini_im[:, gp:gp + 1])
            if own:
                k.tt(q1[b], Wre[:, gp, :], cosT[:, gp, :], ALU.mult)
                k.tt(q2[b], Wim[:, gp, :], sinT[:, gp, :], ALU.mult, eng="pool")
                k.tt(xr[b], q1[b], q2[b], ALU.subtract)
                k.tt(q3[b], Wre[:, gp, :], sinT[:, gp, :], ALU.mult, eng="pool")
                k.tt(q4[b], Wim[:, gp, :], cosT[:, gp, :], ALU.mult)
                k.tt(xi[b], q3[b], q4[b], ALU.add)
                py = pss[4 + c]
                k.mm(py, LC[:, gp, 0, :], xr[b], start=(kq == 0), stop=False)
                k.mm(py, LC[:, gp, 1, :], xi[b], start=False, stop=(kq == 3))
        if s + 1 < NS:
            k.tt(c1, c512, Wre[:, :, 511], ALU.mult)
            k.tt(c2, s512, Wim[:, :, 511], ALU.mult)
            k.tt(ini_re, c1, c2, ALU.subtract)
            k.tt(c1, s512, Wre[:, :, 511], ALU.mult)
            k.tt(c2, c512, Wim[:, :, 511], ALU.mult)
            k.tt(ini_im, c1, c2, ALU.add)
        if own:
            gi = s // 2
            cols = slice(gi * 512, (gi + 1) * 512)
            for c in range(2):
                k.stt(yv[:, c, :], uT[:, c, :], dX[:, c:c + 1], pss[4 + c], ALU.mult, ALU.add)
                k.tt(y2, yv[:, c, :], yv[:, c, :], ALU.mult)
                k.ts(y2, y2, 0.044715, ALU.mult, 1.0, ALU.add)
                k.tt(y2, y2, yv[:, c, :], ALU.mult)
                k.act(sg, y2, AF.Sigmoid, scale=GELU_C)
                k.tt(ygT[:, c, :], yv[:, c, :], sg, ALU.mult)
            yst = C.yst[gi % 2]
            for fc in range(2):
                ps = pss[fc]
                k.mm(ps, wg_b[:, 0, fc * 128:(fc + 1) * 128], ygT[:, 0, :], start=True, stop=False)
                k.mm(ps, wg_b[:, 1, fc * 128:(fc + 1) * 128], ygT[:, 1, :], start=False, stop=True)
                k.act(sg, ps, AF.Sigmoid)
                k.tt(yst[:, fc, :], ygT[:, fc, :], sg, ALU.mult)
            k.dma(yT_out[:, cols].re("(c p) t -> p c t", p=128), yst, key=yT_out.tok)


def host_s5_params(a_re, a_im, log_dt, b_re, b_im, c_re, c_im, d, wglu):
    f = np.float32
    def Xl(a):
        a = a.reshape(2, 8, 64)
        return np.ascontiguousarray(np.broadcast_to(a.transpose(1, 0, 2)[:, None], (8, 16, 2, 64)).reshape(128, 128), dtype=f)
    ldt2 = np.broadcast_to(log_dt[:, None], (16, 64))
    def Bl(b):
        b = b.reshape(2, 8, 64, 16)
        return np.ascontiguousarray(b.transpose(1, 3, 0, 2).reshape(128, 128), dtype=f)
    def Yl(a):
        a = a.reshape(8, 2, 64)
        return np.ascontiguousarray(a.transpose(1, 2, 0).reshape(128, 8), dtype=f)
    def Cl(cm):
        cm = cm.reshape(2, 8, 16, 64)
        t = cm.transpose(3, 0, 1, 2).reshape(64, 2, 128)
        return np.ascontiguousarray(np.concatenate([t, t], 0), dtype=f)
    maskB = np.zeros((128, 4, 128), f)
    for kq in range(4):
        for g2 in range(2):
            gl = 2 * kq + g2
            maskB[gl * 16:(gl + 1) * 16, kq, g2 * 64:(g2 + 1) * 64] = 1.0
    maskC = np.ascontiguousarray(maskB.transpose(2, 1, 0))
    return {"arX": Xl(a_re), "aiX": Xl(a_im), "ldtX": Xl(ldt2), "brX": Bl(b_re), "biX": Bl(b_im),
            "arY": Yl(a_re), "aiY": Yl(a_im), "ldtY": Yl(ldt2), "CdupR": Cl(c_re), "CdupI": Cl(c_im),
            "maskB": maskB, "maskC": maskC, "jrow": JROW,
            "dX": np.ascontiguousarray(d.reshape(2, 128).T, dtype=f), "wglu": np.ascontiguousarray(wglu, dtype=f)}


S5_SHAPES = {"arX": [128, 128], "aiX": [128, 128], "ldtX": [128, 128], "brX": [128, 128], "biX": [128, 128],
             "arY": [128, 8], "aiY": [128, 8], "ldtY": [128, 8], "CdupR": [128, 2, 128], "CdupI": [128, 2, 128],
             "maskB": [128, 4, 128], "maskC": [128, 4, 128], "jrow": [128, 512], "dX": [128, 2], "wglu": [256, 256]}


def build_B_mix2_test(NG):
    T, NK = NG * 512, 2 * NG * 512
    k = KB()
    ins = {}
    for name, shape, dt in (("kfeat", [NFR, NK], BF16), ("ktok", [NK, NTC], BF16), ("dqT", [256, T], BF16),
                            ("identd", [128, 128], BF16), ("antid", [128, 128], BF16),
                            ("ident32d", [128, 128], F32), ("tri32d", [128, 128], F32),
                            ("padbd", [128, 1], F32), ("bfd", [128, 4], F32),
                            ("mfoxd", [128, 4, 512], BF16), ("mmlad", [128, 4, 512], BF16),
                            ("relb", [4, 513], F32), ("mchkd", [128, 8, 512], F32)):
        ins[name] = k.dram(name, shape, dt, "ExternalInput")
    sp = {nm: k.dram("s5_" + nm, sh, F32, "ExternalInput") for nm, sh in S5_SHAPES.items()}
    Ed = k.dram("Ed", [4, 1536], F32, "Internal")
    yD = k.dram("yD", [256, T], BF16, "ExternalOutput")
    yA = k.dram("yA", [256, T], BF16, "ExternalOutput")
    C = common_B(k, ins)
    att_common(k, C, ins)
    with k.phase():
        chunk_phase(k, NG, C, ins["kfeat"], ins["ktok"], ins["dqT"], ins["relb"], ins["mchkd"], Ed, yD)
    with k.phase():
        s5_phase(k, NG, C, ins["kfeat"], sp, yA)
    return k.finish()


CV_F = 2048


def convert_weight(k, cv, name, src, shape):
    n = int(np.prod(shape))
    assert n % (128 * CV_F) == 0, (name, shape)
    dst = k.dram("wc_" + name, list(shape), BF16, "Internal")
    letters = "abcd"[:len(shape)]
    flat = "%s -> (%s)" % (" ".join(letters), " ".join(letters))
    sv = src.re(flat).re("(n p f) -> n p f", p=128, f=CV_F)
    dv = dst.re(flat).re("(n p f) -> n p f", p=128, f=CV_F)
    k.P.bg_mode = True
    try:
        _convert_tiles(k, cv, sv, dv, dst, n // (128 * CV_F))
    finally:
        k.P.bg_mode = False
    return dst


def _convert_tiles(k, cv, sv, dv, dst, ntiles):
    for i in range(ntiles):
        st, cb = cv["st"][cv["n"] % 2], cv["cb"][cv["n"] % 2]
        cv["n"] += 1
        k.dma(st, sv[i], q="pool")
        k.copy(cb, st, eng="pool")
        k.dma(dv[i], cb, q="pool", key=dst.tok)
    return dst


class WS:
    def __init__(self, k, nbf=5):
        self.k = k
        self.bf = [k.sb("w_bf%d" % i, [128, 8, 512], BF16) for i in range(nbf)]
        self.n = 0

    def load(self, src, KC, ncols):
        k = self.k
        bf = self.bf[self.n % len(self.bf)]
        self.n += 1
        k.dma(bf[:, 0:KC, 0:ncols], src.re("(kc p) c -> p kc c", p=128))
        return bf[:, 0:KC, 0:ncols]


def local_phase(k, NG, C, kind, final, x_in, p_in, yTs, W, x_out):
    T = NG * 512
    ws = WS(k)
    pss, pst = C.pss, C.pst
    ident = C.identb
    gts = {}
    for nm in ("g_mix", "g_ffn", "g_ple"):
        gts[nm] = k.sb("l_" + nm, [128, 8], F32)
        k.dma(gts[nm], W[nm])
    bg = k.sb("l_bg", [128, 4, 8], F32)
    k.dma(bg, W["b_gate"])
    scr = alloc_norm_scratch(k, "l")
    xgs = [k.sb("l_xg%d" % i, [128, 4, 1024], F32) for i in range(2)]
    hT = k.sb("l_hT", [128, 8, 512], BF16)
    YT = k.sb("l_YT", [128, 4, 2, 512], BF16)
    macc = k.sb("l_macc", [128, 4, 512], F32)
    mT = k.sb("l_mT", [128, 8, 512], BF16)
    gs = [k.sb("l_gs%d" % i, [128, 512], F32) for i in range(2)]
    tmp = [k.sb("l_tmp%d" % i, [128, 512], F32) for i in range(2)]
    nfc = 22 if kind == "ffn" else 11
    hid = k.sb("l_hid", [128, nfc, 512], BF16)
    pf = k.sb("l_pf", [128, 4, 256], F32)
    pb = k.sb("l_pb", [128, 4, 256], BF16)
    pT = k.sb("l_pT", [128, 2, 512], BF16)
    if final:
        gfin = k.sb("l_gfin", [128, 1024], F32)
        k.dma(gfin, W["g_final"])
    if kind == "moe":
        xhT = k.sb("l_xhT", [128, 8, 512], BF16)
        xlT = k.sb("l_xlT", [128, 8, 512], BF16)
        xn32 = k.sb("l_xn32", [128, 1024], F32)
        xnf = k.sb("l_xnf", [128, 1024], F32)
        xlo = k.sb("l_xlo", [128, 4, 1024], BF16)
        wr_s = k.sb("l_wr_s", [128, 8, 8], F32)
        wg32 = k.sb("l_wg32", [128, 8, 8], F32)
        wgf = k.sb("l_wgf", [128, 8, 8], F32)
        wg_hi = k.sb("l_wg_hi", [128, 8, 8], BF16)
        wg_lo = k.sb("l_wg_lo", [128, 8, 8], BF16)
        brt = k.sb("l_brt", [128, 8], F32)
        k.dma(wr_s, W["w_router"].re("(kc p) e -> p kc e", p=128))
        k.dma(brt, W["b_router"])
        for kc in range(8):
            k.ts(wg32[:, kc, :], wr_s[:, kc, :], gts["g_ffn"][:, kc:kc + 1], ALU.mult)
        k.copy(wg_hi, wg32)
        k.copy(wgf, wg_hi)
        k.tt(wg32, wg32, wgf, ALU.subtract)
        k.copy(wg_lo, wg32)
        lg = k.sb("l_lg", [128, 4, 8], F32)
        lg2 = k.sb("l_lg2", [128, 4, 8], F32)
        eq1 = k.sb("l_eq1", [128, 4, 8], F32)
        eq2 = k.sb("l_eq2", [128, 4, 8], F32)
        wts = k.sb("l_wts", [128, 4, 8], F32)
        m1 = k.sb("l_m1", [128, 4], F32)
        m2 = k.sb("l_m2", [128, 4], F32)
        g1 = k.sb("l_g1", [128, 4], F32)
        g2 = k.sb("l_g2", [128, 4], F32)

    def xadd(xg, tb, half, ps):
        xs = xg[:, tb, half * 512:(half + 1) * 512]
        k.tt(xs, xs, ps, ALU.add)

    k.dma(xgs[0], x_in[0:512, :].re("(b p) f -> p b f", p=128))
    for gi in range(NG):
        xg = xgs[gi % 2]
        cols = slice(gi * 512, (gi + 1) * 512)
        if gi + 1 < NG:
            k.dma(xgs[(gi + 1) % 2], x_in[(gi + 1) * 512:(gi + 2) * 512, :].re("(b p) f -> p b f", p=128), q="pool")
        for mi in range(4):
            k.dma(YT[:, mi, :, :].sub("S:l_YT%d" % mi), yTs[mi][:, cols].re("(c p) t -> p c t", p=128),
                  q="pool", key="l_YT%d" % mi)
        k.dma(pf, p_in[cols, :].re("(b p) c -> p b c", p=128), q="pool")
        norm_hT(k, xg, gts["g_mix"], hT, scr, pst, ident)
        for fc4 in range(2):
            for br in range(4):
                gtile = ws.load(W["w_gate"][br][:, fc4 * 512:(fc4 + 1) * 512], 8, 512)
                btile = ws.load(W["w_branch"][br][:, fc4 * 512:(fc4 + 1) * 512], 2, 512)
                for fl in range(4):
                    fc = fc4 * 4 + fl
                    p1, p2 = pss[(2 * fl) % 6], pss[(2 * fl + 1) % 6]
                    for kc in range(8):
                        k.mm(p1, gtile[:, kc, fl * 128:(fl + 1) * 128], hT[:, kc, :], start=(kc == 0), stop=(kc == 7))
                    g_ = gs[fl % 2]
                    k.act(g_, p1, AF.Sigmoid, bias=bg[:, br, fc:fc + 1])
                    for c in range(2):
                        k.mm(p2, btile[:, c, fl * 128:(fl + 1) * 128], YT[:, br, c, :].sub("S:l_YT%d" % br),
                             start=(c == 0), stop=(c == 1))
                    if br == 0:
                        k.tt(macc[:, fl, :], g_, p2, ALU.mult)
                    else:
                        t_ = tmp[fl % 2]
                        k.tt(t_, g_, p2, ALU.mult)
                        if br < 3:
                            k.tt(macc[:, fl, :], macc[:, fl, :], t_, ALU.add, eng="pool")
                        else:
                            k.tt(mT[:, fc, :], macc[:, fl, :], t_, ALU.add, eng="pool")
        for half in range(2):
            wt = ws.load(W["w_o"][:, half * 512:(half + 1) * 512], 8, 512)
            for tb in range(4):
                ps = pss[(half * 4 + tb) % 6]
                for kc in range(8):
                    k.mm(ps, mT[:, kc, tb * 128:(tb + 1) * 128], wt[:, kc, :], start=(kc == 0), stop=(kc == 7))
                xadd(xg, tb, half, ps)
        import os
        upto = int(os.environ.get("LOCAL_UPTO", "9"))
        if upto < 1:
            k.dma(x_out[cols, :].re("(b p) f -> p b f", p=128), xg, key=x_out.tok)
            continue
        if kind == "ffn":
            norm_hT(k, xg, gts["g_ffn"], hT, scr, pst, ident, flip=1)
            ffn_expert(k, ws, pss, hT, hid, gs, W["w1"], W["w3"], W["w2"], 2816,
                       lambda tb, half, ps: xadd(xg, tb, half, ps))
        else:
            norm_hT(k, xg, gts["g_ffn"], hT, scr, pst, ident, flip=1, raw_out=xhT)
            import os
            mstop = int(os.environ.get("MOE_STOP", "9"))
            for b in range(4 if mstop >= 1 else 0):
                k.ts(xn32, xg[:, b, :], scr.rstd[:, b:b + 1], ALU.mult)
                k.copy(xnf, scr.xn[:, b, :], eng="pool")
                k.tt(xlo[:, b, :], xn32, xnf, ALU.subtract)
            for c in range(8 if mstop >= 2 else 0):
                pt = pst[c % 2]
                for b in range(4):
                    k.tr(pt[:, b * 128:(b + 1) * 128], xlo[:, b, c * 128:(c + 1) * 128], ident)
                k.copy(xlT[:, c, :], pt[:, 0:512], eng=("act" if c % 2 else "dve"))
            for tb in range(4 if mstop >= 3 else 0):
                ps = pss[tb % 6]
                tsl = slice(tb * 128, (tb + 1) * 128)
                n = 0
                for (a_, w_) in ((xhT, wg_hi), (xlT, wg_hi), (xhT, wg_lo)):
                    for kc in range(8):
                        k.mm(ps[:, 0:8], a_[:, kc, tsl], w_[:, kc, :], start=(n == 0), stop=(n == 23))
                        n += 1
                k.tt(lg[:, tb, :], ps[:, 0:8], brt, ALU.add)
            if mstop < 4:
                k.dma(x_out[cols, :].re("(b p) f -> p b f", p=128), xg, key=x_out.tok)
                continue
            k.reduce(m1, lg, ALU.max)
            for tb in range(4):
                k.ts(eq1[:, tb, :], lg[:, tb, :], m1[:, tb:tb + 1], ALU.is_equal)
            k.stt(lg2, eq1, NEG, lg, ALU.mult, ALU.add)
            k.reduce(m2, lg2, ALU.max)
            for tb in range(4):
                k.ts(eq2[:, tb, :], lg2[:, tb, :], m2[:, tb:tb + 1], ALU.is_equal)
            k.tt(g1, m1, m2, ALU.subtract)
            k.act(g1, g1, AF.Sigmoid)
            k.ts(g2, g1, -1.0, ALU.mult, 1.0, ALU.add)
            for tb in range(4):
                k.ts(eq1[:, tb, :], eq1[:, tb, :], g1[:, tb:tb + 1], ALU.mult)
                k.stt(wts[:, tb, :], eq2[:, tb, :], g2[:, tb:tb + 1], eq1[:, tb, :], ALU.mult, ALU.add)
            import os
            for e in range(int(os.environ.get("MOE_NE", "8"))):
                def addw(tb, half, ps, e=e):
                    xs = xg[:, tb, half * 512:(half + 1) * 512]
                    k.stt(xs, ps, wts[:, tb, e:e + 1], xs, ALU.mult, ALU.add)
                ffn_expert(k, ws, pss, hT, hid, gs, W["w1"][e], W["w3"][e], W["w2"][e], 1408, addw)
        if upto < 2:
            k.dma(x_out[cols, :].re("(b p) f -> p b f", p=128), xg, key=x_out.tok)
            continue
        norm_hT(k, xg, gts["g_ple"], hT, scr, pst, ident)
        k.copy(pb, pf)
        for c in range(2):
            pt = pst[c % 2]
            for b in range(4):
                k.tr(pt[:, b * 128:(b + 1) * 128], pb[:, b, c * 128:(c + 1) * 128], ident)
            k.copy(pT[:, c, :], pt[:, 0:512], eng=("act" if c % 2 else "dve"))
        for half in range(2):
            wpg = ws.load(W["ple_w_gate"][:, half * 512:(half + 1) * 512], 8, 512)
            wup = ws.load(W["ple_w_up"][:, half * 512:(half + 1) * 512], 2, 512)
            for tb in range(4):
                p1, p2 = pss[(2 * tb) % 6], pss[(2 * tb + 1) % 6]
                tsl = slice(tb * 128, (tb + 1) * 128)
                for kc in range(8):
                    k.mm(p1, hT[:, kc, tsl], wpg[:, kc, :], start=(kc == 0), stop=(kc == 7))
                g_ = gs[tb % 2]
                k.act(g_, p1, AF.Sigmoid)
                for c in range(2):
                    k.mm(p2, pT[:, c, tsl], wup[:, c, :], start=(c == 0), stop=(c == 1))
                t_ = tmp[tb % 2]
                k.tt(t_, g_, p2, ALU.mult)
                xadd(xg, tb, half, t_)
        if final:
            for b in range(4):
                k.act(scr.junk.ap, xg[:, b, :], AF.Square, accum_out=scr.ssq[:, b:b + 1])
            k.ts(scr.rstd, scr.ssq, 1.0 / 1024, ALU.mult, RMS_EPS, ALU.add)
            k.act(scr.rstd, scr.rstd, AF.Sqrt)
            k.recip(scr.rstd, scr.rstd)
            for b in range(4):
                k.stt(xg[:, b, :], xg[:, b, :], scr.rstd[:, b:b + 1], gfin, ALU.mult, ALU.mult,
                      )
        k.dma(x_out[cols, :].re("(b p) f -> p b f", p=128), xg, key=x_out.tok)


def ffn_expert(k, ws, pss, hT, hid, gs, w1, w3, w2, dff, addfn):
    nfc = dff // 128
    c0 = 0
    fc = 0
    while c0 < dff:
        nc_ = min(512, dff - c0)
        t1 = ws.load(w1[:, c0:c0 + nc_], 8, nc_)
        t3 = ws.load(w3[:, c0:c0 + nc_], 8, nc_)
        for fl in range(nc_ // 128):
            p1, p3 = pss[(2 * fl) % 6], pss[(2 * fl + 1) % 6]
            for kc in range(8):
                k.mm(p1, t1[:, kc, fl * 128:(fl + 1) * 128], hT[:, kc, :], start=(kc == 0), stop=(kc == 7))
            for kc in range(8):
                k.mm(p3, t3[:, kc, fl * 128:(fl + 1) * 128], hT[:, kc, :], start=(kc == 0), stop=(kc == 7))
            g_ = gs[fl % 2]
            import os
            if os.environ.get("NOSILU"):
                k.act(g_, p1, AF.Sigmoid)
                k.tt(g_, g_, p1, ALU.mult)
            else:
                k.act(g_, p1, AF.Silu)
            k.tt(hid[:, fc, :], g_, p3, ALU.mult)
            fc += 1
        c0 += nc_
    for half in range(2):
        k0 = 0
        while k0 < nfc:
            kn = min(8, nfc - k0)
            wt = ws.load(w2[k0 * 128:(k0 + kn) * 128, half * 512:(half + 1) * 512], kn, 512)
            for tb in range(4):
                ps = pss[tb]
                for kc in range(kn):
                    k.mm(ps, hid[:, k0 + kc, tb * 128:(tb + 1) * 128], wt[:, kc, :],
                         start=(k0 + kc == 0), stop=(k0 + kc == nfc - 1))
            k0 += kn
        for tb in range(4):
            addfn(tb, half, pss[tb])


B_CONST_SPECS = (("identd", [128, 128], BF16), ("antid", [128, 128], BF16), ("ident32d", [128, 128], F32),
                 ("tri32d", [128, 128], F32), ("padbd", [128, 1], F32), ("bfd", [128, 4], F32),
                 ("mfoxd", [128, 4, 512], BF16), ("mmlad", [128, 4, 512], BF16),
                 ("relb", [4, 513], F32), ("mchkd", [128, 8, 512], F32))


def build_B(NG, kind, final, stages=("fox", "mla", "chunk", "s5", "local")):
    T, NK = NG * 512, 2 * NG * 512
    k = KB()
    ins = {}
    for name, shape, dt in (("kfeat", [NFR, NK], BF16), ("ktok", [NK, NTC], BF16), ("kff", [NK, 4], F32),
                            ("qfT", [256, T], BF16), ("dqT", [256, T], BF16), ("qmT", [384, T], BF16),
                            ("x", [T, D], F32), ("p", [T, 256], F32)) + B_CONST_SPECS:
        ins[name] = k.dram(name, shape, dt, "ExternalInput")
    sp = {nm: k.dram("s5_" + nm, sh, F32, "ExternalInput") for nm, sh in S5_SHAPES.items()}
    W = {}
    for nm in ("g_mix", "g_ffn", "g_ple"):
        W[nm] = k.dram(nm, [128, 8], F32, "ExternalInput")
    W["b_gate"] = k.dram("b_gate", [128, 4, 8], F32, "ExternalInput")
    wgate = k.dram("w_gate", [4, D, D], F32, "ExternalInput")
    wbr = k.dram("w_branch", [4, 256, D], F32, "ExternalInput")
    W["w_gate"] = [wgate[i] for i in range(4)]
    W["w_branch"] = [wbr[i] for i in range(4)]
    W["w_o"] = k.dram("w_o", [D, D], F32, "ExternalInput")
    if kind == "ffn":
        W["w1"] = k.dram("w1", [D, 2816], F32, "ExternalInput")
        W["w3"] = k.dram("w3", [D, 2816], F32, "ExternalInput")
        W["w2"] = k.dram("w2", [2816, D], F32, "ExternalInput")
    else:
        w1 = k.dram("w1", [8, D, 1408], F32, "ExternalInput")
        w3 = k.dram("w3", [8, D, 1408], F32, "ExternalInput")
        w2 = k.dram("w2", [8, 1408, D], F32, "ExternalInput")
        W["w1"] = [w1[e] for e in range(8)]
        W["w3"] = [w3[e] for e in range(8)]
        W["w2"] = [w2[e] for e in range(8)]
        W["w_router"] = k.dram("w_router", [D, 8], F32, "ExternalInput")
        W["b_router"] = k.dram("b_router", [128, 8], F32, "ExternalInput")
    W["ple_w_gate"] = k.dram("ple_w_gate", [D, D], F32, "ExternalInput")
    W["ple_w_up"] = k.dram("ple_w_up", [256, D], F32, "ExternalInput")
    if final:
        W["g_final"] = k.dram("g_final", [128, D], F32, "ExternalInput")
    Ed = k.dram("Ed", [4, 1536], F32, "Internal")
    dbg = len(stages) < 5
    yT = [k.dram("yT%d" % i, [256, T], BF16, "ExternalOutput" if dbg else "Internal") for i in range(4)]
    xo = k.dram("xo", [T, D], F32, "ExternalOutput")
    C = common_B(k, ins)
    if "local" in stages:
        cv = {"st": [k.sb("cv_st%d" % i, [128, CV_F], F32) for i in range(2)],
              "cb": [k.sb("cv_cb%d" % i, [128, CV_F], BF16) for i in range(2)], "n": 0}
        g_ = convert_weight(k, cv, "w_gate", wgate, [4, D, D])
        b_ = convert_weight(k, cv, "w_branch", wbr, [4, 256, D])
        W["w_gate"] = [g_[i] for i in range(4)]
        W["w_branch"] = [b_[i] for i in range(4)]
        W["w_o"] = convert_weight(k, cv, "w_o", W["w_o"], [D, D])
        if kind == "ffn":
            W["w1"] = convert_weight(k, cv, "w1", W["w1"], [D, 2816])
            W["w3"] = convert_weight(k, cv, "w3", W["w3"], [D, 2816])
            W["w2"] = convert_weight(k, cv, "w2", W["w2"], [2816, D])
        else:
            c1 = convert_weight(k, cv, "w1", w1, [8, D, 1408])
            c3 = convert_weight(k, cv, "w3", w3, [8, D, 1408])
            c2 = convert_weight(k, cv, "w2", w2, [8, 1408, D])
            W["w1"] = [c1[e] for e in range(8)]
            W["w3"] = [c3[e] for e in range(8)]
            W["w2"] = [c2[e] for e in range(8)]
        W["ple_w_gate"] = convert_weight(k, cv, "ple_w_gate", W["ple_w_gate"], [D, D])
        W["ple_w_up"] = convert_weight(k, cv, "ple_w_up", W["ple_w_up"], [256, D])
    with k.phase():
        att_common(k, C, ins)
        if "fox" in stages:
            with k.phase():
                fox_phase(k, NG, C, ins["kfeat"], ins["ktok"], ins["kff"], ins["qfT"], yT[1])
        if "mla" in stages:
            with k.phase():
                mla_phase(k, NG, C, ins["kfeat"], ins["ktok"], ins["qmT"], yT[2])
        if "chunk" in stages:
            with k.phase():
                chunk_phase(k, NG, C, ins["kfeat"], ins["ktok"], ins["dqT"], ins["relb"], ins["mchkd"], Ed, yT[3])
        if "s5" in stages:
            with k.phase():
                s5_phase(k, NG, C, ins["kfeat"], sp, yT[0])
    if "local" in stages:
        with k.phase():
            local_phase(k, NG, C, kind, final, ins["x"], ins["p"], yT, W, xo)
    return k.finish()


_PROGS = {}


def _prog(key, fn):
    if key not in _PROGS:
        _PROGS[key] = fn()
    return _PROGS[key]


def _own_idx(par, NG):
    return np.concatenate([np.arange((par + 2 * i) * 512, (par + 2 * i + 1) * 512) for i in range(NG)])


def _c(a, dt=np.float32):
    return np.ascontiguousarray(a, dtype=dt)


def run_model(inp, NG, n_cores=8, stages=None):
    f = np.float32
    nb = n_cores // 2
    S = 1024 * NG
    x = _c(inp["x"])
    p = _c(inp["p"])
    pos = np.asarray(inp["positions"]).astype(np.int32)
    own = [_own_idx(par, NG) for par in range(2)]
    xs = [x[c // 2][own[c % 2]] for c in range(n_cores)]
    cores = list(range(n_cores))
    for l in range(2):
        kind = "ffn" if l % 2 == 0 else "moe"
        final = (l == 1)
        ncA = _prog(("A", NG), lambda: build_A(NG))
        insA = [host_A_inputs(xs[c], np.asarray(inp["g_mix"][l]), np.asarray(inp["w_in"][l]),
                              np.asarray(inp["mla_w_uq"][l]), np.asarray(inp["mla_w_ukv"][l]),
                              np.asarray(inp["mla_g_q"][l]), np.asarray(inp["mla_g_kv"][l]),
                              pos[c // 2][own[c % 2]]) for c in cores]
        ra = run_bass_kernel_spmd(ncA, insA, core_ids=cores).results
        ncB = _prog(("B", NG, kind, final, stages), lambda: (build_B(NG, kind, final, stages) if stages else build_B(NG, kind, final)))
        spd = host_s5_params(*[np.asarray(inp[n][l]) for n in ("ssm_a_re", "ssm_a_im", "ssm_log_dt", "ssm_b_re",
                                                                 "ssm_b_im", "ssm_c_re", "ssm_c_im", "ssm_d",
                                                                 "ssm_w_glu")])
        wd = {"g_mix": _c(np.asarray(inp["g_mix"][l]).reshape(8, 128).T),
              "g_ffn": _c(np.asarray(inp["g_ffn"][l]).reshape(8, 128).T),
              "g_ple": _c(np.asarray(inp["g_ple"][l]).reshape(8, 128).T),
              "b_gate": _c(np.asarray(inp["b_gate"][l]).reshape(4, 8, 128).transpose(2, 0, 1)),
              "w_gate": _c(inp["w_gate"][l]), "w_branch": _c(inp["w_branch"][l]), "w_o": _c(inp["w_o"][l]),
              "ple_w_gate": _c(inp["ple_w_gate"][l]), "ple_w_up": _c(inp["ple_w_up"][l]),
              "identd": IDENT, "antid": ANTI, "ident32d": IDENT32, "tri32d": TRI32,
              "bfd": _c(np.tile(np.asarray(inp["fox_b_f"][l])[None], (128, 1))),
              "mfoxd": MFOX, "mmlad": MMLA, "relb": _c(inp["chk_rel_bias"][l]), "mchkd": MCHK}
        if kind == "ffn":
            wd.update({"w1": _c(inp["ffn_w1"][l // 2]), "w3": _c(inp["ffn_w3"][l // 2]), "w2": _c(inp["ffn_w2"][l // 2])})
        else:
            j = l // 2
            wd.update({"w1": _c(inp["moe_w1"][j]), "w3": _c(inp["moe_w3"][j]), "w2": _c(inp["moe_w2"][j]),
                       "w_router": _c(inp["moe_w_router"][j]),
                       "b_router": _c(np.tile(np.asarray(inp["moe_b_router"][j])[None], (128, 1)))})
        if final:
            wd["g_final"] = _c(np.tile(np.asarray(inp["g_final"])[None], (128, 1)))
        for nm, v in spd.items():
            wd["s5_" + nm] = v
        insB = []
        for c in cores:
            b, par = c // 2, c % 2
            ra0, ra1 = ra[2 * b], ra[2 * b + 1]

            def glob(nm, axis):
                a0, a1 = np.asarray(ra0[nm]), np.asarray(ra1[nm])
                a0 = np.moveaxis(a0, axis, 0).reshape((NG, 512) + tuple(np.delete(a0.shape, axis)))
                a1 = np.moveaxis(a1, axis, 0).reshape((NG, 512) + tuple(np.delete(a1.shape, axis)))
                g_ = np.stack([a0, a1], 1).reshape((2 * NG * 512,) + a0.shape[2:])
                return np.moveaxis(g_, 0, axis)
            d = dict(wd)
            d["kfeat"] = to_slots(glob("featT", 1), par, NG, 1)
            d["ktok"] = to_slots(glob("tokM", 0), par, NG, 0)
            d["kff"] = to_slots(glob("ffM", 0), par, NG, 0)
            me = ra[c]
            d["qfT"], d["dqT"], d["qmT"] = np.asarray(me["qfT"]), np.asarray(me["dqT"]), np.asarray(me["qmT"])
            d["x"] = _c(xs[c])
            d["p"] = _c(p[l][b][own[par]])
            d["padbd"] = np.full((128, 1), NEG if par == 0 else 0.0, f)
            insB.append(d)
        rb = run_bass_kernel_spmd(ncB, insB, core_ids=cores).results
        print('layer', l, 'B done', flush=True)
        xs = [np.asarray(rb[c]["xo"]) for c in cores]
    out = np.zeros((nb, S, D), f)
    for c in cores:
        out[c // 2][own[c % 2]] = xs[c]
    return out


def kernel(**inputs):
    return run_model(inputs, 8)
```
